# Optimizing a Trainium2 kernel written in Bass

```python
import math
import jax, jax.numpy as jnp
from jax import lax
import numpy as np

D_MODEL = 1024
BATCH = 8
SEQ = 4096
DEPTH = 4

CHUNK = 64
Q_BLOCK = 128
N_A_LAYERS = DEPTH // 2
N_B_LAYERS = DEPTH - N_A_LAYERS
RWKV_HEAD = 64
RWKV_HEADS = D_MODEL // RWKV_HEAD
DECAY_LORA = 64
ICLR_LORA = 64
GATE_LORA = 160
LNX_EPS = 64e-5
MLA_HEADS = 16
QK_NOPE = 64
QK_ROPE = 32
V_HEAD = 64
Q_LORA = 384
KV_LORA = 256
ROPE_THETA = 10000.0
NEG_INF = -1e30
D_FF = 2816
CONV_W = 3
NORM_EPS = 1e-6

kernel_name = "rwkv7_yoco_mla_convffn_trunk"


def rmsnorm(x, g):
    xf = x.astype(jnp.float32)
    y = xf * lax.rsqrt(jnp.mean(xf * xf, axis=-1, keepdims=True) + NORM_EPS)
    return (y * g.astype(jnp.float32)).astype(x.dtype)


def conv_ffn(x, w_in, conv_w, conv_b, w_out):
    T = x.shape[1]
    gate, up = jnp.split(x @ w_in, 2, axis=-1)
    gp = jnp.pad(gate, ((0, 0), (CONV_W - 1, 0), (0, 0)))
    gc = conv_b
    for j in range(CONV_W):
        gc = gc + gp[:, j:j + T, :] * conv_w[j]
    h = jax.nn.gelu(gc) * up
    return h @ w_out


def _rwkv7_step(S, inp):
    r, w, k, v, a, b = inp
    sa = jnp.einsum('bhvk,bhk->bhv', S, a)
    S = S * w[:, :, None, :] + sa[..., None] * b[:, :, None, :] + v[..., None] * k[:, :, None, :]
    y = jnp.einsum('bhvk,bhk->bhv', S, r)
    return S, y


def rwkv7_time_mix(x, mu, w_rkv, w0, w1, w2, a0, a1, a2, g1, g2, k_k, k_a, r_k, lnx_w, lnx_b, w_o):
    B, T, D = x.shape
    H, N = RWKV_HEADS, RWKV_HEAD
    f32 = jnp.float32
    x_prev = jnp.pad(x, ((0, 0), (1, 0), (0, 0)))[:, :T]
    xx = x_prev - x
    xs = x[None] + xx[None] * mu[:, None, None, :]
    rkv = jnp.einsum('nbtd,nde->nbte', xs[:3], w_rkv)
    r, k, v = rkv[0], rkv[1], rkv[2]
    xw, xa, xg = xs[3], xs[4], xs[5]
    w_log = -jax.nn.softplus(-(w0 + jnp.tanh(xw @ w1) @ w2)) - 0.5
    decay = jnp.exp(-jnp.exp(w_log.astype(f32)))
    a = jax.nn.sigmoid(a0 + (xa @ a1) @ a2)
    g = jax.nn.sigmoid(xg @ g1) @ g2
    kk = (k * k_k).reshape(B, T, H, N).astype(f32)
    kk = kk / jnp.maximum(jnp.sqrt(jnp.sum(kk * kk, axis=-1, keepdims=True)), 1e-12)
    k = k * (1.0 + (a - 1.0) * k_a)
    heads = lambda t: t.reshape(B, T, H, N).astype(f32)
    rh, kh, vh, wh, ah = heads(r), heads(k), heads(v), heads(decay), heads(a)
    seq = tuple(jnp.moveaxis(t, 1, 0) for t in (rh, wh, kh, vh, -kk, kk * ah))
    S0 = jnp.zeros((B, H, N, N), f32)
    _, y = lax.scan(_rwkv7_step, S0, seq)
    y = jnp.moveaxis(y, 0, 1)
    mean = jnp.mean(y, axis=-1, keepdims=True)
    var = jnp.mean(jnp.square(y - mean), axis=-1, keepdims=True)
    yn = ((y - mean) * lax.rsqrt(var + LNX_EPS)).reshape(B, T, D) * lnx_w + lnx_b
    bonus = (jnp.sum(rh * kh * r_k, axis=-1, keepdims=True) * vh).reshape(B, T, D)
    out = ((yn + bonus) * g).astype(x.dtype)
    return out @ w_o


def rope_tables(T):
    inv = 1.0 / (ROPE_THETA ** (jnp.arange(0, QK_ROPE, 2, dtype=jnp.float32) / QK_ROPE))
    ang = jnp.arange(T, dtype=jnp.float32)[:, None] * inv[None, :]
    return jnp.cos(ang), jnp.sin(ang)


def apply_rope(x, cos, sin):
    half = x.shape[-1] // 2
    xf = x.astype(jnp.float32)
    x1, x2 = xf[..., :half], xf[..., half:]
    return jnp.concatenate([x1 * cos - x2 * sin, x1 * sin + x2 * cos], axis=-1).astype(x.dtype)


def mla_attend(q_nope, q_rope, k_nope, k_rope, v):
    B, T, H, _ = q_nope.shape
    nb = T // Q_BLOCK
    scale = 1.0 / math.sqrt(QK_NOPE + QK_ROPE)
    k_chunk = jnp.arange(T) // CHUNK

    def blocks(t):
        return jnp.moveaxis(t.reshape((B, nb, Q_BLOCK) + t.shape[2:]), 1, 0)

    def one_block(args):
        qn, qr, bi = args
        s = jnp.einsum('bqhd,bkhd->bhqk', qn, k_nope) + jnp.einsum('bqhr,bkr->bhqk', qr, k_rope)
        s = s.astype(jnp.float32) * scale
        q_chunk = (bi * Q_BLOCK + jnp.arange(Q_BLOCK)) // CHUNK
        mask = k_chunk[None, :] <= q_chunk[:, None]
        s = jnp.where(mask[None, None], s, NEG_INF)
        p = jax.nn.softmax(s, axis=-1)
        return jnp.einsum('bhqk,bkhd->bqhd', p.astype(v.dtype), v)

    out = lax.map(one_block, (blocks(q_nope), blocks(q_rope), jnp.arange(nb)))
    return jnp.moveaxis(out, 0, 1).reshape(B, T, H, V_HEAD)


def setup_inputs(seed: int = 0) -> dict:
    key = jax.random.key(seed)
    ks = jax.random.split(key, 32)
    D, F, H, N = D_MODEL, D_FF, RWKV_HEADS, RWKV_HEAD
    nA, nB = N_A_LAYERS, N_B_LAYERS
    nrm = lambda k, shape, s: jax.random.normal(k, shape, jnp.float32) * s
    return {
        'x': nrm(ks[0], (BATCH, SEQ, D), 1.0),
        'norm_g': 1.0 + nrm(ks[1], (DEPTH, 4, D), 0.05),
        'ffn_w_in': nrm(ks[2], (DEPTH, D, 2 * F), D ** -0.5),
        'ffn_conv_w': nrm(ks[3], (DEPTH, CONV_W, F), CONV_W ** -0.5),
        'ffn_conv_b': nrm(ks[4], (DEPTH, F), 0.01),
        'ffn_w_out': nrm(ks[5], (DEPTH, F, D), F ** -0.5),
        'a_mu': jax.random.uniform(ks[6], (nA, 6, D), jnp.float32),
        'a_w_rkv': nrm(ks[7], (nA, 3, D, D), D ** -0.5),
        'a_w0': jax.random.uniform(ks[8], (nA, D), jnp.float32, minval=-6.5, maxval=-1.5),
        'a_w1': nrm(ks[9], (nA, D, DECAY_LORA), 0.1 * D ** -0.5),
        'a_w2': nrm(ks[10], (nA, DECAY_LORA, D), DECAY_LORA ** -0.5),
        'a_a0': nrm(ks[11], (nA, D), 0.1),
        'a_a1': nrm(ks[12], (nA, D, ICLR_LORA), 0.1 * D ** -0.5),
        'a_a2': nrm(ks[13], (nA, ICLR_LORA, D), ICLR_LORA ** -0.5),
        'a_g1': nrm(ks[14], (nA, D, GATE_LORA), D ** -0.5),
        'a_g2': nrm(ks[15], (nA, GATE_LORA, D), GATE_LORA ** -0.5),
        'a_k_k': 0.85 + nrm(ks[16], (nA, D), 0.05),
        'a_k_a': 1.0 + nrm(ks[17], (nA, D), 0.05),
        'a_r_k': nrm(ks[18], (nA, H, N), 0.1),
        'a_lnx_w': 1.0 + nrm(ks[19], (nA, D), 0.05),
        'a_lnx_b': nrm(ks[20], (nA, D), 0.01),
        'a_w_o': nrm(ks[21], (nA, D, D), D ** -0.5),
        'kv_norm_g': 1.0 + nrm(ks[22], (D,), 0.05),
        'kv_w_down': nrm(ks[23], (D, KV_LORA + QK_ROPE), D ** -0.5),
        'kv_a_norm_g': 1.0 + nrm(ks[24], (KV_LORA,), 0.05),
        'kv_w_up': nrm(ks[25], (KV_LORA, MLA_HEADS * (QK_NOPE + V_HEAD)), KV_LORA ** -0.5),
        'q_w_down': nrm(ks[26], (nB, D, Q_LORA), D ** -0.5),
        'q_norm_g': 1.0 + nrm(ks[27], (nB, Q_LORA), 0.05),
        'q_w_up': nrm(ks[28], (nB, Q_LORA, MLA_HEADS * (QK_NOPE + QK_ROPE)), Q_LORA ** -0.5),
        'o_w': nrm(ks[29], (nB, MLA_HEADS * V_HEAD, D), (MLA_HEADS * V_HEAD) ** -0.5),
    }


def reference(x, norm_g, ffn_w_in, ffn_conv_w, ffn_conv_b, ffn_w_out,
              a_mu, a_w_rkv, a_w0, a_w1, a_w2, a_a0, a_a1, a_a2, a_g1, a_g2,
              a_k_k, a_k_a, a_r_k, a_lnx_w, a_lnx_b, a_w_o,
              kv_norm_g, kv_w_down, kv_a_norm_g, kv_w_up,
              q_w_down, q_norm_g, q_w_up, o_w):
    B, T, _ = x.shape
    cos, sin = rope_tables(T)
    k_nope = k_rope = v_sh = None
    for layer in range(DEPTH):
        h = rmsnorm(x, norm_g[layer, 0])
        if layer < N_A_LAYERS:
            i = layer
            m = rwkv7_time_mix(h, a_mu[i], a_w_rkv[i], a_w0[i], a_w1[i], a_w2[i],
                               a_a0[i], a_a1[i], a_a2[i], a_g1[i], a_g2[i],
                               a_k_k[i], a_k_a[i], a_r_k[i], a_lnx_w[i], a_lnx_b[i], a_w_o[i])
        else:
            if layer == N_A_LAYERS:
                down = rmsnorm(x, kv_norm_g) @ kv_w_down
                c_kv = rmsnorm(down[..., :KV_LORA], kv_a_norm_g)
                k_rope = apply_rope(down[..., KV_LORA:], cos, sin)
                up = (c_kv @ kv_w_up).reshape(B, T, MLA_HEADS, QK_NOPE + V_HEAD)
                k_nope, v_sh = up[..., :QK_NOPE], up[..., QK_NOPE:]
            j = layer - N_A_LAYERS
            c_q = rmsnorm(h @ q_w_down[j], q_norm_g[j])
            q = (c_q @ q_w_up[j]).reshape(B, T, MLA_HEADS, QK_NOPE + QK_ROPE)
            q_nope = q[..., :QK_NOPE]
            q_rope = apply_rope(q[..., QK_NOPE:], cos[:, None, :], sin[:, None, :])
            o = mla_attend(q_nope, q_rope, k_nope, k_rope, v_sh)
            m = o.reshape(B, T, MLA_HEADS * V_HEAD) @ o_w[j]
        x = x + rmsnorm(m, norm_g[layer, 1])
        f = conv_ffn(rmsnorm(x, norm_g[layer, 2]), ffn_w_in[layer], ffn_conv_w[layer],
                     ffn_conv_b[layer], ffn_w_out[layer])
        x = x + rmsnorm(f, norm_g[layer, 3])
    return x
```

```python
import math
from contextlib import ExitStack
import numpy as np
import concourse.bass as bass
import concourse.mybir as mybir
from concourse.bass_utils import run_bass_kernel_spmd

F32 = mybir.dt.float32
BF16 = mybir.dt.bfloat16
AF = mybir.ActivationFunctionType
ALU = mybir.AluOpType

D = 1024
DEPTH = 4
NA = 2
H = 16
FF = 2816
NF = FF // 128
QL = 384
KVL = 256
LNX_EPS = 64e-5
NORM_EPS = 1e-6
SCALE = 1.0 / math.sqrt(96.0)

COMPUTE = ("pe", "act", "dve", "pool")
SEM_LIMIT = 30000
NDMA_SEM = 24


class Res:
    __slots__ = ("name", "w", "r")

    def __init__(self, name=""):
        self.name = name
        self.w = None
        self.r = {}


class Tl:
    __slots__ = ("t", "r")

    def __init__(self, t, r):
        self.t = t
        self.r = r


class Prog:
    def __init__(self, nc, stack):
        self.nc = nc
        self.stack = stack
        self.streams = {e: [] for e in COMPUTE + ("sp",)}
        self.sems = {}
        self.cnt = {}
        for e in COMPUTE:
            self.sems[e] = [self._newsem(e + "0")]
            self.cnt[e] = (0, 0)
        self.dma_sems = [self._newsem("dma%d" % i) for i in range(NDMA_SEM)]
        self.dma_cnt = [0] * NDMA_SEM
        self.dma_i = 0
        self.seen = {e: {} for e in self.streams}
        self.n = 0

    def _newsem(self, name):
        return self.stack.enter_context(self.nc.semaphore(name))

    def _semof(self, key, idx):
        if isinstance(key, tuple):
            return self.dma_sems[key[1]]
        return self.sems[key][idx]

    @staticmethod
    def _need(waits, tok):
        if tok is None:
            return
        key, idx, val = tok
        cur = waits.get(key)
        if cur is None or (idx, val) > cur:
            waits[key] = (idx, val)

    def _collect(self, q, reads, writes, eng):
        waits = {}
        for r in reads:
            self._need(waits, r.w)
        for w in writes:
            self._need(waits, w.w)
            for k, (i, v) in w.r.items():
                if k == eng:
                    continue
                self._need(waits, (k, i, v))
        if eng == "pe":
            waits.pop("pe", None)
        out = []
        for key, (idx, val) in waits.items():
            s = self.seen[q].get(key)
            if s is not None and s >= (idx, val):
                continue
            self.seen[q][key] = (idx, val)
            out.append((self._semof(key, idx), val))
        return out

    def op(self, eng, fn, reads=(), writes=()):
        waits = self._collect(eng, reads, writes, eng)
        idx, val = self.cnt[eng]
        if val >= SEM_LIMIT:
            idx += 1
            val = 0
            self.sems[eng].append(self._newsem("%s%d" % (eng, idx)))
        val += 1
        self.cnt[eng] = (idx, val)
        self.streams[eng].append((waits, fn, (self.sems[eng][idx], 1)))
        tok = (eng, idx, val)
        for r in reads:
            r.r[eng] = (idx, val)
        for w in writes:
            w.w = tok
            w.r = {}
        self.n += 1
        return tok

    def dma(self, out, in_, reads=(), writes=(), q="sp"):
        waits = self._collect(q, reads, writes, None)
        j = self.dma_i % NDMA_SEM
        self.dma_i += 1
        self.dma_cnt[j] += 16
        val = self.dma_cnt[j]
        key = ("dma", j)
        self.streams[q].append(
            (waits, lambda e: e.dma_start(out=out, in_=in_), (self.dma_sems[j], 16)))
        tok = (key, 0, val)
        for r in reads:
            r.r[key] = (0, val)
        for w in writes:
            w.w = tok
            w.r = {}
        self.n += 1
        return tok

    def barrier(self):
        for q in self.streams:
            waits = []
            for e in COMPUTE:
                if e == q:
                    continue
                idx, val = self.cnt[e]
                if val == 0:
                    continue
                sn = self.seen[q].get(e)
                if sn is not None and sn >= (idx, val):
                    continue
                self.seen[q][e] = (idx, val)
                waits.append((self.sems[e][idx], val))
            for j in range(NDMA_SEM):
                val = self.dma_cnt[j]
                if val == 0:
                    continue
                key = ("dma", j)
                sn = self.seen[q].get(key)
                if sn is not None and sn >= (0, val):
                    continue
                self.seen[q][key] = (0, val)
                waits.append((self.dma_sems[j], val))
            self.streams[q].append((waits, None, None))

    def emit(self):
        nc = self.nc
        with nc.Block() as block:
            def run(stream):
                def body(e):
                    for waits, fn, inc in stream:
                        for s, v in waits:
                            e.wait_ge(s, v)
                        if fn is not None:
                            ins = fn(e)
                            if inc is not None:
                                ins.then_inc(inc[0], inc[1])
                return body
            block.tensor(run(self.streams["pe"]))
            block.scalar(run(self.streams["act"]))
            block.vector(run(self.streams["dve"]))
            block.gpsimd(run(self.streams["pool"]))
            block.sync(run(self.streams["sp"]))


VD = {}


def _pack_tables(inp):
    vecs = []

    def add(name, v):
        VD[name] = len(vecs)
        vecs.append(np.asarray(v, np.float32).reshape(D))

    for l in range(DEPTH):
        for j in range(4):
            add("ng%d_%d" % (l, j), inp["norm_g"][l, j])
    for i in range(NA):
        for n in range(6):
            add("mu%d_%d" % (i, n), inp["a_mu"][i, n])
        for nm in ("w0", "a0", "k_k", "k_a", "r_k", "lnx_w", "lnx_b"):
            add("%s%d" % (nm, i), inp["a_" + nm][i])
    add("kvng", inp["kv_norm_g"])
    tabD = np.stack(vecs, 0).reshape(len(vecs), 8, 128).transpose(2, 0, 1).reshape(128, -1)
    fv = []
    for l in range(DEPTH):
        for j in range(3):
            fv.append(inp["ffn_conv_w"][l, j])
        fv.append(inp["ffn_conv_b"][l])
    tabF = np.stack(fv, 0).reshape(len(fv), NF, 128).transpose(2, 0, 1).reshape(128, -1)
    qg = np.asarray(inp["q_norm_g"]).reshape(2, 3, 128).transpose(2, 0, 1).reshape(128, 6)
    kg = np.asarray(inp["kv_a_norm_g"]).reshape(2, 128).T
    vt = np.ascontiguousarray(np.concatenate([tabD, tabF, qg, kg], 1).astype(np.float32))
    return vt, tabD.shape[1], tabF.shape[1]


def _consts():
    p = np.arange(128)
    ident = np.eye(128, dtype=np.float32)
    bd = (p[:, None] // 64 == p[None, :] // 64).astype(np.float32)
    su = (p[:, None] < p[None, :]).astype(np.float32)
    ui = (p[:, None] <= p[None, :]).astype(np.float32)
    low = (p[None, :] < p[:, None]).astype(np.float32)
    ones = np.ones((128, 128), np.float32)
    return np.ascontiguousarray(np.concatenate([ident, bd, su, ui, su, ui, low, ones], 1))


C_ID, C_BD, C_M4, C_LOW, C_ONE = 0, 128, 256, 768, 896
NCST = 1024


def _rope(T):
    inv = 1.0 / (10000.0 ** (np.arange(0, 32, 2, dtype=np.float32) / 32.0))
    ang = np.arange(T, dtype=np.float32)[:, None] * inv[None, :].astype(np.float32)
    cos = np.cos(ang).astype(np.float32).T
    sin = np.sin(ang).astype(np.float32).T
    tab = np.zeros((128, 2, T), np.float32)
    tab[64:80, 0] = cos
    tab[80:96, 0] = cos
    tab[64:80, 1] = -sin
    tab[80:96, 1] = sin
    return tab


def build(T, depth=DEPTH, dbg=False):
    nc = bass.Bass("TRN2", target_bir_lowering=False)
    NT5 = T // 512
    NT2 = T // 256

    def din(name, shape):
        return nc.dram_tensor(name, list(shape), F32, kind="ExternalInput").ap()

    xT = din("xT", [D, T])
    vt_d = din("vt", [128, NVT[0]])
    cst_d = din("cst", [128, NCST])
    rope_d = din("rope", [128, 2, T])
    w_in_d = din("ffn_w_in", [DEPTH, D, 2 * FF])
    w_out_d = din("ffn_w_out", [DEPTH, FF, D])
    wrkv_d = din("a_w_rkv", [NA, 3, D, D])
    w1_d = din("a_w1", [NA, D, 64])
    w2_d = din("a_w2", [NA, 64, D])
    a1_d = din("a_a1", [NA, D, 64])
    a2_d = din("a_a2", [NA, 64, D])
    g1_d = din("a_g1", [NA, D, 160])
    g2_d = din("a_g2", [NA, 160, D])
    wo_d = din("a_w_o", [NA, D, D])
    kd_d = din("kv_w_down", [D, 288])
    kds_d = din("kv_w_down_sw", [D, 32])
    kuk_d = din("kv_w_up_k", [KVL, H * 64])
    kuv_d = din("kv_w_up_v", [KVL, H * 64])
    qd_d = din("q_w_down", [2, D, QL])
    qu_d = din("q_w_up", [2, QL, H * 96])
    qus_d = din("q_w_up_sw", [2, QL, H * 32])
    ow_d = din("o_w", [2, D, D])
    y = nc.dram_tensor("y", [D, T], F32, kind="ExternalOutput").ap()
    xs = nc.dram_tensor("xs", [D, T], F32).ap()
    hscr = nc.dram_tensor("hscr", [FF, T], BF16).ap()
    kscr = nc.dram_tensor("kscr", [H, 96, T], BF16).ap()
    vscr = nc.dram_tensor("vscr", [H, T, 128], BF16).ap()

    Rx = [Res("x%d" % i) for i in range(NT2)]
    Rh = [Res("h%d" % i) for i in range(NT5)]
    Rkv = [Res("kv%d" % i) for i in range(NT5)]
    Rin = Res("in")

    with ExitStack() as top:
        P = Prog(nc, top)
        top.enter_context(nc.allow_low_precision("bf16 matmul operands, fp32 accumulate"))

        uid = [0]

        def sbt(st, name, shape, dt):
            uid[0] += 1
            nm = "sb%d_%s" % (uid[0], name)
            return Tl(st.enter_context(nc.sbuf_tensor(nm, list(shape), dt)), Res(nm))

        PS = [Tl(top.enter_context(nc.psum_tensor("ps%d" % i, [128, 512], F32)), Res("ps%d" % i))
              for i in range(8)]
        vt = sbt(top, "vt", [128, NVT[0]], F32)
        cst = sbt(top, "cst", [128, NCST], F32)
        onesb = sbt(top, "onesb", [128, 128], BF16)
        stg = [sbt(top, "stg%d" % i, [128, 1024], F32) for i in range(2)]
        stg_i = [0]
        P.dma(vt.t[:], vt_d, writes=[vt.r])
        P.dma(cst.t[:], cst_d, writes=[cst.r])
        P.op("dve", lambda e: e.tensor_copy(onesb.t[:], cst.t[:, C_ONE:C_ONE + 128]), [cst.r], [onesb.r])
        identb_t = sbt(top, "identb", [128, 128], BF16)
        P.op("dve", lambda e: e.tensor_copy(identb_t.t[:], cst.t[:, C_ID:C_ID + 128]), [cst.r], [identb_t.r])
        identb = identb_t.t[:, :]

        ident = cst.t[:, C_ID:C_ID + 128]
        bdones = cst.t[:, C_BD:C_BD + 128]
        mask4 = cst.t[:, C_M4:C_M4 + 512]
        lowm = cst.t[:, C_LOW:C_LOW + 128]
        ones = cst.t[:, C_ONE:C_ONE + 128]

        def vD(name, c):
            i = VD[name] * 8 + c
            return vt.t[:, i:i + 1]

        def vF(l, j, f):
            i = NVT[1] + (l * 4 + j) * NF + f
            return vt.t[:, i:i + 1]

        def vQ(j, c):
            i = NVT[1] + NVT[2] + j * 3 + c
            return vt.t[:, i:i + 1]

        def vK(c):
            i = NVT[1] + NVT[2] + 6 + c
            return vt.t[:, i:i + 1]

        def mm(out, lhsT, rhs, start, stop, reads, writes):
            P.op("pe", lambda e: e.matmul(out, lhsT, rhs, start=start, stop=stop), reads, writes)

        def act(out, in_, func, reads, writes, bias=None, scale=None):
            kw = {}
            if bias is not None:
                kw["bias"] = bias
            if scale is not None:
                kw["scale"] = scale
            P.op("act", lambda e: e.activation(out, in_, func, **kw), reads, writes)

        def tt(eng, out, in0, in1, op, reads, writes):
            P.op(eng, lambda e: e.tensor_tensor(out, in0, in1, op), reads, writes)

        def ts(eng, out, in0, s1, s2, op0, op1, reads, writes):
            if s2 is None:
                P.op(eng, lambda e: e.tensor_scalar(out, in0, s1, None, op0), reads, writes)
            else:
                P.op(eng, lambda e: e.tensor_scalar(out, in0, s1, s2, op0, op1), reads, writes)

        def stt(eng, out, in0, sc, in1, op0, op1, reads, writes):
            P.op("dve", lambda e: e.scalar_tensor_tensor(out, in0, sc, in1, op0, op1), reads, writes)

        def cp(eng, out, in_, reads, writes):
            if eng == "act":
                act(out, in_, AF.Copy, reads, writes)
            else:
                P.op(eng, lambda e: e.tensor_copy(out, in_), reads, writes)

        rr = [0]

        def ew():
            rr[0] += 1
            return "pool" if rr[0] % 3 == 0 else "dve"

        def load_w(view, res, src, rows, cols):
            c0 = 0
            while c0 < cols:
                cw = min(1024, cols - c0)
                s = stg[stg_i[0] % 2]
                stg_i[0] += 1
                P.dma(s.t[0:rows, 0:cw], src[:, c0:c0 + cw], reads=[Rin], writes=[s.r])
                eng = "pool" if stg_i[0] % 2 == 0 else "dve"
                cp(eng, view(rows, c0, cw), s.t[0:rows, 0:cw], [s.r], [res])
                c0 += cw

        def load_wk(tile, src, nk, cols, rows_last=128):
            for k in range(nk):
                rows = rows_last if k == nk - 1 else 128
                load_w(lambda r, c0, cw, k=k: tile.t[0:r, k, c0:c0 + cw], tile.r,
                       src[k * 128:k * 128 + rows, :], rows, cols)

        def rms_stats(st_sq, src_ap, nch, TT, Dn, eps, rstd, src_reads, bank):
            act(st_sq.t[:, 0:nch, 0:TT], src_ap, AF.Square, src_reads, [st_sq.r])
            for c in range(nch):
                mm(bank.t[:, 0:TT], onesb.t[:, :], st_sq.t[:, c, 0:TT], c == 0, c == nch - 1,
                   [st_sq.r, onesb.r], [bank.r])
            act(rstd.t[:, 0:TT], bank.t[:, 0:TT], AF.Sqrt, [], [bank.r, rstd.r], bias=eps, scale=1.0 / Dn)
            P.op("dve", lambda e: e.reciprocal(rstd.t[:, 0:TT], rstd.t[:, 0:TT]), [], [rstd.r])

        def xtile(ap, t0, TT):
            return ap.rearrange("(c p) t -> p c t", p=128)[:, :, t0:t0 + TT]

        def rx(t0, TT):
            return Rx[t0 // 256:(t0 + TT) // 256]

        def run_jobs(gens, width, stagger):
            active = []
            it = iter(gens)
            steps0 = 0
            done = False
            while True:
                while not done and len(active) < width and (len(active) == 0 or steps0 >= stagger):
                    g = next(it, None)
                    if g is None:
                        done = True
                        break
                    active.append(g)
                    if len(active) == 1:
                        steps0 = 0
                if not active:
                    break
                for g in list(active):
                    try:
                        next(g)
                    except StopIteration:
                        active.remove(g)
                        steps0 = stagger
                steps0 += 1

        def rwkv_layer(i, layer, src, dst):
            TT = 256
            with ExitStack() as st:
                wr = sbt(st, "wr", [128, 8, D], BF16)
                wk = sbt(st, "wk", [128, 8, D], BF16)
                wv = sbt(st, "wv", [128, 8, D], BF16)
                wo = sbt(st, "wo", [128, 8, D], BF16)
                w1 = sbt(st, "w1", [128, 8, 64], BF16)
                a1 = sbt(st, "a1", [128, 8, 64], BF16)
                g1 = sbt(st, "g1", [128, 8, 160], BF16)
                w2 = sbt(st, "w2", [128, 1, D], BF16)
                a2 = sbt(st, "a2", [128, 1, D], BF16)
                g2 = sbt(st, "g2", [128, 2, D], BF16)
                for tl, srcw in ((wr, wrkv_d[i, 0]), (wk, wrkv_d[i, 1]), (wv, wrkv_d[i, 2]), (wo, wo_d[i])):
                    load_wk(tl, srcw, 8, D)
                load_wk(w1, w1_d[i], 8, 64)
                load_wk(a1, a1_d[i], 8, 64)
                load_wk(g1, g1_d[i], 8, 160)
                load_wk(w2, w2_d[i], 1, D, rows_last=64)
                load_wk(a2, a2_d[i], 1, D, rows_last=64)
                load_wk(g2, g2_d[i], 2, D, rows_last=32)

                xt = sbt(st, "xt", [128, 8, TT], F32)
                hb = sbt(st, "hb", [128, 8, TT + 1], F32)
                xsn = [sbt(st, "xs%d" % n, [128, 8, TT], BF16) for n in range(6)]
                rstd = sbt(st, "rstd", [128, TT], F32)
                tw = sbt(st, "tw", [64, TT], BF16)
                ta = sbt(st, "ta", [64, TT], BF16)
                tg = sbt(st, "tg", [128, 2, TT], BF16)
                z = sbt(st, "z", [128, 8, TT], BF16)
                sq = z
                msb = sbt(st, "msb", [128, 8, TT], F32)
                xx = msb
                S = sbt(st, "S", [128, 8, 64], F32)
                Sb = sbt(st, "Sb", [128, 8, 64], BF16)
                names = "r k v kk a lw g sq2 rn kmod b cum cex ecum bonus d".split()
                sets = []
                for k in range(2):
                    J = dict(
                        cb={nm: sbt(st, "c%d_%s" % (k, nm), [128, TT], F32) for nm in names},
                        vb=sbt(st, "vb%d" % k, [128, TT], BF16),
                        AR=sbt(st, "AR%d" % k, [128, 2 * TT], BF16),
                        KB=sbt(st, "KB%d" % k, [128, 2 * TT], BF16),
                        TM=sbt(st, "TM%d" % k, [128, 2, 3, 128], BF16),
                        bA=PS[4 * k], bB=PS[4 * k + 1], hd=[])
                    for hh in range(2):
                        J["hd"].append(dict(
                            AMs=sbt(st, "AMs%d_%d" % (k, hh), [128, 512], BF16),
                            Ls=sbt(st, "Ls%d_%d" % (k, hh), [128, 128], BF16),
                            LM=[sbt(st, "LM%d_%d_%d" % (k, hh, q), [128, 256], BF16) for q in range(2)],
                            Tt=[sbt(st, "Tt%d_%d_%d" % (k, hh, q), [128, 128], BF16) for q in range(2)],
                            Xs=sbt(st, "Xs%d_%d" % (k, hh), [128, 64], BF16),
                            Us=sbt(st, "Us%d_%d" % (k, hh), [128, 64], BF16),
                            tmpS=sbt(st, "tmpS%d_%d" % (k, hh), [128, 64], F32),
                            WK=PS[4 * k + 2 + hh]))
                    sets.append(J)
                P.op("pool", lambda e: e.memset(S.t[:], 0.0), [], [S.r])
                P.op("pool", lambda e: e.memset(Sb.t[:], 0.0), [], [Sb.r])
                P.op("pool", lambda e: e.memset(hb.t[:, :, 0:1], 0.0), [], [hb.r])

                def job(c, J):
                    B = J["cb"]
                    vb, AR, KB, TM, bA, bB, hd = J["vb"], J["AR"], J["KB"], J["TM"], J["bA"], J["bB"], J["hd"]
                    cs = slice(c * 128, (c + 1) * 128)
                    for kc in range(8):
                        mm(bA.t[:, 0:TT], wr.t[:, kc, cs], xsn[0].t[:, kc, :], kc == 0, kc == 7, [wr.r, xsn[0].r], [bA.r])
                    for kc in range(8):
                        mm(bA.t[:, TT:2 * TT], wk.t[:, kc, cs], xsn[1].t[:, kc, :], kc == 0, kc == 7, [wk.r, xsn[1].r], [bA.r])
                    for kc in range(8):
                        mm(bB.t[:, 0:TT], wv.t[:, kc, cs], xsn[2].t[:, kc, :], kc == 0, kc == 7, [wv.r, xsn[2].r], [bB.r])
                    mm(bB.t[:, TT:2 * TT], w2.t[0:64, 0, cs], tw.t[:], True, True, [w2.r, tw.r], [bB.r])
                    yield
                    cp("act", B["r"].t[:], bA.t[:, 0:TT], [], [bA.r, B["r"].r])
                    ts("dve", B["kk"].t[:], bA.t[:, TT:2 * TT], vD("k_k%d" % i, c), None, ALU.mult, None, [vt.r], [bA.r, B["kk"].r])
                    cp("act", B["k"].t[:], bA.t[:, TT:2 * TT], [], [bA.r, B["k"].r])
                    yield
                    cp("dve", B["v"].t[:], bB.t[:, 0:TT], [], [bB.r, B["v"].r])
                    cp("act", vb.t[:], bB.t[:, 0:TT], [], [bB.r, vb.r])
                    act(B["lw"].t[:], bB.t[:, TT:2 * TT], AF.Sigmoid, [vt.r], [bB.r, B["lw"].r], bias=vD("w0%d" % i, c))
                    ts("pool", B["lw"].t[:], B["lw"].t[:], -math.exp(-0.5), None, ALU.mult, None, [], [B["lw"].r])
                    yield
                    mm(bA.t[:, 0:TT], a2.t[0:64, 0, cs], ta.t[:], True, True, [a2.r, ta.r], [bA.r])
                    mm(bA.t[:, TT:2 * TT], g2.t[:, 0, cs], tg.t[:, 0, :], True, False, [g2.r, tg.r], [bA.r])
                    mm(bA.t[:, TT:2 * TT], g2.t[0:32, 1, cs], tg.t[0:32, 1, :], False, True, [g2.r, tg.r], [bA.r])
                    act(B["a"].t[:], bA.t[:, 0:TT], AF.Sigmoid, [vt.r], [bA.r, B["a"].r], bias=vD("a0%d" % i, c))
                    cp("act", B["g"].t[:], bA.t[:, TT:2 * TT], [], [bA.r, B["g"].r])
                    yield
                    act(B["sq2"].t[:], B["kk"].t[:], AF.Square, [B["kk"].r], [B["sq2"].r])
                    mm(bB.t[:, 0:TT], bdones, B["sq2"].t[:], True, True, [cst.r, B["sq2"].r], [bB.r])
                    act(B["rn"].t[:], bB.t[:, 0:TT], AF.Sqrt, [], [bB.r, B["rn"].r])
                    ts("dve", B["rn"].t[:], B["rn"].t[:], 1e-12, None, ALU.max, None, [], [B["rn"].r])
                    P.op("dve", lambda e: e.reciprocal(B["rn"].t[:], B["rn"].t[:]), [], [B["rn"].r])
                    tt("dve", B["kk"].t[:], B["kk"].t[:], B["rn"].t[:], ALU.mult, [B["rn"].r], [B["kk"].r])
                    yield
                    ts("pool", B["kmod"].t[:], B["a"].t[:], -1.0, vD("k_a%d" % i, c), ALU.add, ALU.mult, [B["a"].r, vt.r], [B["kmod"].r])
                    stt("dve", B["kmod"].t[:], B["kmod"].t[:], 1.0, B["k"].t[:], ALU.add, ALU.mult, [B["k"].r], [B["kmod"].r])
                    tt("pool", B["b"].t[:], B["kk"].t[:], B["a"].t[:], ALU.mult, [B["kk"].r, B["a"].r], [B["b"].r])
                    for ch in range(2):
                        sl = slice(ch * 128, (ch + 1) * 128)
                        P.op("dve", lambda e, sl=sl: e.tensor_tensor_scan(
                            B["cum"].t[:, sl], ones, B["lw"].t[:, sl], 0.0, ALU.mult, ALU.add), [cst.r, B["lw"].r], [B["cum"].r])
                    tt("pool", B["cex"].t[:], B["cum"].t[:], B["lw"].t[:], ALU.subtract, [B["cum"].r, B["lw"].r], [B["cex"].r])
                    act(B["ecum"].t[:], B["cum"].t[:], AF.Exp, [B["cum"].r], [B["ecum"].r])
                    act(B["cum"].t[:], B["cum"].t[:], AF.Exp, [], [B["cum"].r], scale=-1.0)
                    act(B["cex"].t[:], B["cex"].t[:], AF.Exp, [], [B["cex"].r])
                    yield
                    for ch in range(2):
                        sl = slice(ch * 128, (ch + 1) * 128)
                        o = ch * 256
                        stt("dve", AR.t[:, o:o + 128], B["kk"].t[:, sl], -1.0, B["cex"].t[:, sl], ALU.mult, ALU.mult,
                            [B["kk"].r, B["cex"].r], [AR.r])
                        tt("pool", AR.t[:, o + 128:o + 256], B["r"].t[:, sl], B["ecum"].t[:, sl], ALU.mult, [B["r"].r, B["ecum"].r], [AR.r])
                        tt("dve", KB.t[:, o:o + 128], B["kmod"].t[:, sl], B["cum"].t[:, sl], ALU.mult, [B["kmod"].r, B["cum"].r], [KB.r])
                        tt("pool", KB.t[:, o + 128:o + 256], B["b"].t[:, sl], B["cum"].t[:, sl], ALU.mult, [B["b"].r, B["cum"].r], [KB.r])
                    yield
                    stt("dve", B["sq2"].t[:], B["r"].t[:], vD("r_k%d" % i, c), B["kmod"].t[:], ALU.mult, ALU.mult,
                        [B["r"].r, B["kmod"].r, vt.r], [B["sq2"].r])
                    mm(bB.t[:, TT:2 * TT], bdones, B["sq2"].t[:], True, True, [cst.r, B["sq2"].r], [bB.r])
                    tt("dve", B["bonus"].t[:], bB.t[:, TT:2 * TT], B["v"].t[:], ALU.mult, [B["v"].r], [bB.r, B["bonus"].r])
                    for ch in range(2):
                        sl = slice(ch * 128, (ch + 1) * 128)
                        o = ch * 256
                        mm(bA.t[:, ch * 128:ch * 128 + 128], vb.t[:, sl], identb, True, True, [vb.r, identb_t.r], [bA.r])
                        mm(bA.t[:, 256 + ch * 128:256 + ch * 128 + 128], KB.t[:, o:o + 128], identb, True, True, [KB.r, identb_t.r], [bA.r])
                        mm(bB.t[:, ch * 128:ch * 128 + 128], KB.t[:, o + 128:o + 256], identb, True, True, [KB.r, identb_t.r], [bB.r])
                    yield
                    for ch in range(2):
                        cp("act", TM.t[:, ch, 0, :], bA.t[:, ch * 128:ch * 128 + 128], [], [bA.r, TM.r])
                        cp("dve", TM.t[:, ch, 1, :], bA.t[:, 256 + ch * 128:256 + ch * 128 + 128], [], [bA.r, TM.r])
                        cp("act", TM.t[:, ch, 2, :], bB.t[:, ch * 128:ch * 128 + 128], [], [bB.r, TM.r])
                    yield
                    for ch in range(2):
                        o = ch * 256
                        for hh in range(2):
                            p = slice(hh * 64, hh * 64 + 64)
                            Hh = hd[hh]
                            WK = Hh["WK"]
                            mm(WK.t[:, 0:256], KB.t[p, o:o + 128], AR.t[p, o:o + 256], True, True, [KB.r, AR.r], [WK.r])
                            mm(WK.t[:, 256:512], KB.t[p, o + 128:o + 256], AR.t[p, o:o + 256], True, True, [KB.r, AR.r], [WK.r])
                            tt("dve", Hh["AMs"].t[:], WK.t[:], mask4, ALU.mult, [cst.r], [WK.r, Hh["AMs"].r])
                            mm(WK.t[:, 0:128], AR.t[p, o:o + 128], KB.t[p, o + 128:o + 256], True, True, [KB.r, AR.r], [WK.r])
                            tt("dve", Hh["Ls"].t[:], WK.t[:, 0:128], lowm, ALU.mult, [cst.r], [WK.r, Hh["Ls"].r])
                            tt("pool", Hh["Tt"][0].t[:], Hh["AMs"].t[:, 256:384], identb, ALU.add, [Hh["AMs"].r, identb_t.r], [Hh["Tt"][0].r])
                            yield
                        for lv in range(1, 7):
                            for hh in range(2):
                                Hh = hd[hh]
                                WK = Hh["WK"]
                                if lv == 1:
                                    Lp, Mp, rd = Hh["Ls"].t[:], Hh["AMs"].t[:, 256:384], [Hh["Ls"].r, Hh["AMs"].r]
                                else:
                                    pl = Hh["LM"][(lv - 1) % 2]
                                    Lp, Mp, rd = pl.t[:, 0:128], pl.t[:, 128:256], [pl.r]
                                nl = Hh["LM"][lv % 2]
                                mm(WK.t[:, 128:256], Mp, Lp, True, True, rd, [WK.r])
                                if lv < 6:
                                    mm(WK.t[:, 256:384], Lp, Mp, True, True, rd, [WK.r])
                                    cp("act", nl.t[:, 0:256], WK.t[:, 128:384], [], [WK.r, nl.r])
                                else:
                                    cp("act", nl.t[:, 0:128], WK.t[:, 128:256], [], [WK.r, nl.r])
                                Tc = Hh["Tt"][(lv - 1) % 2]
                                Tn = Hh["Tt"][lv % 2]
                                mm(WK.t[:, 384:512], nl.t[:, 0:128], Tc.t[:], True, True, [nl.r, Tc.r], [WK.r])
                                tt("dve", Tn.t[:], WK.t[:, 384:512], Tc.t[:], ALU.add, [Tc.r], [WK.r, Tn.r])
                            yield
                        for hh in range(2):
                            p = slice(hh * 64, hh * 64 + 64)
                            fs = slice(hh * 64, hh * 64 + 64)
                            Hh = hd[hh]
                            WK = Hh["WK"]
                            Tf = Hh["Tt"][0]
                            mm(WK.t[:, 0:64], AR.t[p, o:o + 128], Sb.t[p, c, :], True, False, [AR.r, Sb.r], [WK.r])
                            mm(WK.t[:, 0:64], Hh["AMs"].t[:, 0:128], TM.t[:, ch, 0, fs], False, True, [Hh["AMs"].r, TM.r], [WK.r])
                            cp("act", Hh["Xs"].t[:], WK.t[:, 0:64], [], [WK.r, Hh["Xs"].r])
                            mm(WK.t[:, 64:128], Tf.t[:], Hh["Xs"].t[:], True, True, [Tf.r, Hh["Xs"].r], [WK.r])
                            cp("act", Hh["Us"].t[:], WK.t[:, 64:128], [], [WK.r, Hh["Us"].r])
                        yield
                        for hh in range(2):
                            p = slice(hh * 64, hh * 64 + 64)
                            fs = slice(hh * 64, hh * 64 + 64)
                            Hh = hd[hh]
                            WK = Hh["WK"]
                            yo = WK.t[p, 128:256]
                            mm(yo, Sb.t[p, c, :], AR.t[p, o + 128:o + 256], True, False, [Sb.r, AR.r], [WK.r])
                            mm(yo, Hh["Us"].t[:], Hh["AMs"].t[:, 384:512], False, False, [Hh["Us"].r, Hh["AMs"].r], [WK.r])
                            mm(yo, TM.t[:, ch, 0, fs], Hh["AMs"].t[:, 128:256], False, True, [TM.r, Hh["AMs"].r], [WK.r])
                            so = WK.t[p, 256:320]
                            mm(so, TM.t[:, ch, 2, fs], Hh["Us"].t[:], True, False, [TM.r, Hh["Us"].r], [WK.r])
                            mm(so, TM.t[:, ch, 1, fs], TM.t[:, ch, 0, fs], False, True, [TM.r], [WK.r])
                            cp("act", B["d"].t[p, ch * 128:ch * 128 + 128], yo, [], [WK.r, B["d"].r])
                            tt("dve", Hh["tmpS"].t[p, :], so, S.t[p, c, :], ALU.add, [S.r], [WK.r, Hh["tmpS"].r])
                            wc = B["ecum"].t[p, ch * 128 + 127:ch * 128 + 128]
                            ts("dve", S.t[p, c, :], Hh["tmpS"].t[p, :], wc, None, ALU.mult, None, [Hh["tmpS"].r, B["ecum"].r], [S.r])
                            ts("pool", Sb.t[p, c, :], Hh["tmpS"].t[p, :], wc, None, ALU.mult, None, [Hh["tmpS"].r, B["ecum"].r], [Sb.r])
                        yield
                    mm(bB.t[:, 0:TT], bdones, B["d"].t[:], True, True, [cst.r, B["d"].r], [bB.r])
                    stt("dve", B["d"].t[:], bB.t[:, 0:TT], -1.0 / 64, B["d"].t[:], ALU.mult, ALU.add, [], [bB.r, B["d"].r])
                    act(B["sq2"].t[:], B["d"].t[:], AF.Square, [B["d"].r], [B["sq2"].r])
                    mm(bB.t[:, TT:2 * TT], bdones, B["sq2"].t[:], True, True, [cst.r, B["sq2"].r], [bB.r])
                    act(B["rn"].t[:], bB.t[:, TT:2 * TT], AF.Sqrt, [], [bB.r, B["rn"].r], bias=LNX_EPS, scale=1.0 / 64)
                    P.op("dve", lambda e: e.reciprocal(B["rn"].t[:], B["rn"].t[:]), [], [B["rn"].r])
                    yield
                    stt("dve", B["d"].t[:], B["d"].t[:], vD("lnx_w%d" % i, c), B["rn"].t[:], ALU.mult, ALU.mult, [B["rn"].r, vt.r], [B["d"].r])
                    stt("dve", B["d"].t[:], B["d"].t[:], vD("lnx_b%d" % i, c), B["bonus"].t[:], ALU.add, ALU.add, [B["bonus"].r, vt.r], [B["d"].r])
                    tt("pool", z.t[:, c, :], B["d"].t[:], B["g"].t[:], ALU.mult, [B["d"].r, B["g"].r], [z.r])
                    yield

                for it in range(T // TT):
                    t0 = it * TT
                    P.dma(xt.t[:], xtile(src, t0, TT), reads=rx(t0, TT), writes=[xt.r])
                    rms_stats(sq, xt.t[:], 8, TT, D, NORM_EPS, rstd, [xt.r], PS[3])
                    for c in range(8):
                        stt("dve", hb.t[:, c, 1:TT + 1], xt.t[:, c, :], vD("ng%d_0" % layer, c), rstd.t[:, :],
                            ALU.mult, ALU.mult, [xt.r, rstd.r, vt.r], [hb.r])
                    tt("pool", xx.t[:], hb.t[:, :, 0:TT], hb.t[:, :, 1:TT + 1], ALU.subtract, [hb.r], [xx.r])
                    for n in range(6):
                        for c in range(8):
                            stt("dve", xsn[n].t[:, c, :], xx.t[:, c, :], vD("mu%d_%d" % (i, n), c),
                                hb.t[:, c, 1:TT + 1], ALU.mult, ALU.add, [xx.r, hb.r, vt.r], [xsn[n].r])
                    for kc in range(8):
                        mm(PS[0].t[0:64, 0:TT], w1.t[:, kc, :], xsn[3].t[:, kc, :], kc == 0, kc == 7, [w1.r, xsn[3].r], [PS[0].r])
                    act(tw.t[:], PS[0].t[0:64, 0:TT], AF.Tanh, [], [PS[0].r, tw.r])
                    for kc in range(8):
                        mm(PS[1].t[0:64, 0:TT], a1.t[:, kc, :], xsn[4].t[:, kc, :], kc == 0, kc == 7, [a1.r, xsn[4].r], [PS[1].r])
                    cp("act", ta.t[:], PS[1].t[0:64, 0:TT], [], [PS[1].r, ta.r])
                    for kc in range(8):
                        mm(PS[2].t[:, 0:TT], g1.t[:, kc, 0:128], xsn[5].t[:, kc, :], kc == 0, kc == 7, [g1.r, xsn[5].r], [PS[2].r])
                    for kc in range(8):
                        mm(PS[2].t[0:32, TT:2 * TT], g1.t[:, kc, 128:160], xsn[5].t[:, kc, :], kc == 0, kc == 7, [g1.r, xsn[5].r], [PS[2].r])
                    act(tg.t[:, 0, :], PS[2].t[:, 0:TT], AF.Sigmoid, [], [PS[2].r, tg.r])
                    act(tg.t[0:32, 1, :], PS[2].t[0:32, TT:2 * TT], AF.Sigmoid, [], [PS[2].r, tg.r])

                    run_jobs((job(c, sets[c % 2]) for c in range(8)), 2, 12)

                    for m in range(8):
                        bk = PS[m % 4]
                        for kc in range(8):
                            mm(bk.t[:, 0:TT], wo.t[:, kc, m * 128:(m + 1) * 128], z.t[:, kc, :], kc == 0, kc == 7, [wo.r, z.r], [bk.r])
                        cp("act", msb.t[:, m, :], bk.t[:, 0:TT], [], [bk.r, msb.r])
                    rms_stats(sq, msb.t[:], 8, TT, D, NORM_EPS, rstd, [msb.r], PS[4])
                    for c in range(8):
                        stt("dve", msb.t[:, c, :], msb.t[:, c, :], vD("ng%d_1" % layer, c), rstd.t[:, :], ALU.mult, ALU.mult,
                            [rstd.r, vt.r], [msb.r])
                    tt("pool", msb.t[:], msb.t[:], xt.t[:], ALU.add, [xt.r], [msb.r])
                    P.dma(xtile(dst, t0, TT), msb.t[:], reads=[msb.r], writes=rx(t0, TT))
                    cp("pool", hb.t[:, :, 0:1], hb.t[:, :, TT:TT + 1], [], [hb.r])
                P.barrier()

        def ffn_layer(layer, src, dst):
            TT = 512
            with ExitStack() as st:
                win = sbt(st, "win", [128, 8, 2 * FF], BF16)
                load_wk(win, w_in_d[layer], 8, 2 * FF)
                xt = sbt(st, "xt", [128, 8, TT], F32)
                xn = sbt(st, "xn", [128, 8, TT], BF16)
                sq = sbt(st, "sq", [128, 8, TT], BF16)
                rstd = sbt(st, "rstd", [128, TT], F32)
                carry = sbt(st, "carry", [128, NF, 2], F32)
                gbuf = [sbt(st, "gbuf%d" % k, [128, TT + 2], F32) for k in range(2)]
                acc = [sbt(st, "acc%d" % k, [128, TT], F32) for k in range(2)]
                gl = [sbt(st, "gl%d" % k, [128, TT], F32) for k in range(2)]
                hT = [sbt(st, "hT%d" % k, [128, TT], BF16) for k in range(2)]
                P.op("pool", lambda e: e.memset(carry.t[:], 0.0), [], [carry.r])
                for it in range(T // TT):
                    t0 = it * TT
                    P.dma(xt.t[:], xtile(src, t0, TT), reads=rx(t0, TT), writes=[xt.r])
                    rms_stats(sq, xt.t[:], 8, TT, D, NORM_EPS, rstd, [xt.r], PS[7])
                    for c in range(8):
                        stt(ew(), xn.t[:, c, :], xt.t[:, c, :], vD("ng%d_2" % layer, c), rstd.t[:, :], ALU.mult, ALU.mult,
                            [xt.r, rstd.r, vt.r], [xn.r])
                    for f in range(NF):
                        k2 = f % 2
                        gb, ub = PS[k2], PS[2 + k2]
                        for kc in range(8):
                            mm(gb.t[:, :], win.t[:, kc, f * 128:(f + 1) * 128], xn.t[:, kc, :], kc == 0, kc == 7,
                               [win.r, xn.r], [gb.r])
                        for kc in range(8):
                            mm(ub.t[:, :], win.t[:, kc, FF + f * 128:FF + (f + 1) * 128], xn.t[:, kc, :], kc == 0, kc == 7,
                               [win.r, xn.r], [ub.r])
                        G = gbuf[k2]
                        cp("pool", G.t[:, 0:2], carry.t[:, f, :], [carry.r], [G.r])
                        cp("act", G.t[:, 2:TT + 2], gb.t[:, :], [], [gb.r, G.r])
                        cp("pool", carry.t[:, f, :], G.t[:, TT:TT + 2], [G.r], [carry.r])
                        A = acc[k2]
                        ts("dve", A.t[:], G.t[:, 2:TT + 2], vF(layer, 2, f), vF(layer, 3, f), ALU.mult, ALU.add,
                           [G.r, vt.r], [A.r])
                        stt("dve", A.t[:], G.t[:, 1:TT + 1], vF(layer, 1, f), A.t[:], ALU.mult, ALU.add, [G.r, vt.r], [A.r])
                        stt("dve", A.t[:], G.t[:, 0:TT], vF(layer, 0, f), A.t[:], ALU.mult, ALU.add, [G.r, vt.r], [A.r])
                        act(gl[k2].t[:], A.t[:], AF.Gelu_apprx_tanh, [A.r], [gl[k2].r])
                        tt("dve", hT[k2].t[:], ub.t[:, :], gl[k2].t[:], ALU.mult, [gl[k2].r], [ub.r, hT[k2].r])
                        P.dma(hscr[f * 128:(f + 1) * 128, t0:t0 + TT], hT[k2].t[:], reads=[hT[k2].r], writes=[Rh[it]])
                P.barrier()
            with ExitStack() as st:
                wout = sbt(st, "wout", [128, NF, D], BF16)
                load_wk(wout, w_out_d[layer], NF, D)
                xt = sbt(st, "xt", [128, 8, TT], F32)
                ht = [sbt(st, "ht%d" % k, [128, NF, TT], BF16) for k in range(2)]
                msb = sbt(st, "msb", [128, 8, TT], F32)
                sq = sbt(st, "sq", [128, 8, TT], BF16)
                rstd = sbt(st, "rstd", [128, TT], F32)
                for it in range(T // TT):
                    t0 = it * TT
                    hh = ht[it % 2]
                    P.dma(hh.t[:], hscr.rearrange("(f p) t -> p f t", p=128)[:, :, t0:t0 + TT], reads=[Rh[it]], writes=[hh.r])
                    P.dma(xt.t[:], xtile(src, t0, TT), reads=rx(t0, TT), writes=[xt.r])
                    for m in range(8):
                        bk = PS[m % 4]
                        for f in range(NF):
                            mm(bk.t[:, :], wout.t[:, f, m * 128:(m + 1) * 128], hh.t[:, f, :], f == 0, f == NF - 1,
                               [wout.r, hh.r], [bk.r])
                        cp("act", msb.t[:, m, :], bk.t[:, :], [], [bk.r, msb.r])
                    rms_stats(sq, msb.t[:], 8, TT, D, NORM_EPS, rstd, [msb.r], PS[7])
                    for c in range(8):
                        stt("dve", msb.t[:, c, :], msb.t[:, c, :], vD("ng%d_3" % layer, c), rstd.t[:, :], ALU.mult, ALU.mult,
                            [rstd.r, vt.r], [msb.r])
                    tt("pool", msb.t[:], msb.t[:], xt.t[:], ALU.add, [xt.r], [msb.r])
                    P.dma(xtile(dst, t0, TT), msb.t[:], reads=[msb.r], writes=rx(t0, TT))
                P.barrier()

        def kv_prep(src):
            TT = 512
            with ExitStack() as st:
                kd = sbt(st, "kd", [128, 8, 288], BF16)
                kds = sbt(st, "kds", [128, 8, 32], BF16)
                kuk = sbt(st, "kuk", [128, 2, H * 64], BF16)
                kuv = sbt(st, "kuv", [128, 2, H * 64], BF16)
                load_wk(kd, kd_d, 8, 288)
                load_wk(kds, kds_d, 8, 32)
                load_wk(kuk, kuk_d, 2, H * 64)
                load_wk(kuv, kuv_d, 2, H * 64)
                xt = sbt(st, "xt", [128, 8, TT], F32)
                xn = sbt(st, "xn", [128, 8, TT], BF16)
                sq = sbt(st, "sq", [128, 8, TT], BF16)
                rstd = sbt(st, "rstd", [128, TT], F32)
                ckv = sbt(st, "ckv", [128, 2, TT], F32)
                ckvn = sbt(st, "ckvn", [128, 2, TT], BF16)
                rp = sbt(st, "rp", [128, 2, TT], F32)
                t1 = sbt(st, "t1", [128, TT], F32)
                t2 = sbt(st, "t2", [128, TT], F32)
                kr = sbt(st, "kr", [128, TT], BF16)
                KT = [sbt(st, "KT%d" % k, [128, TT], BF16) for k in range(2)]
                Vt = [sbt(st, "Vt%d" % k, [128, H, 128], BF16) for k in range(2)]
                for k in range(2):
                    P.op("pool", lambda e, k=k: e.memset(Vt[k].t[:, :, 64:128], 1.0), [], [Vt[k].r])
                R = slice(64, 96)
                for it in range(T // TT):
                    t0 = it * TT
                    P.dma(xt.t[:], xtile(src, t0, TT), reads=rx(t0, TT), writes=[xt.r])
                    P.dma(rp.t[R, :, :], rope_d[R, :, t0:t0 + TT], reads=[Rin], writes=[rp.r])
                    rms_stats(sq, xt.t[:], 8, TT, D, NORM_EPS, rstd, [xt.r], PS[7])
                    for c in range(8):
                        stt(ew(), xn.t[:, c, :], xt.t[:, c, :], vD("kvng", c), rstd.t[:, :], ALU.mult, ALU.mult,
                            [xt.r, rstd.r, vt.r], [xn.r])
                    for j in range(2):
                        for kc in range(8):
                            mm(PS[j].t[:, :], kd.t[:, kc, j * 128:(j + 1) * 128], xn.t[:, kc, :], kc == 0, kc == 7,
                               [kd.r, xn.r], [PS[j].r])
                        cp("act", ckv.t[:, j, :], PS[j].t[:, :], [], [PS[j].r, ckv.r])
                    for kc in range(8):
                        mm(PS[2].t[R, :], kd.t[:, kc, 256:288], xn.t[:, kc, :], kc == 0, kc == 7, [kd.r, xn.r], [PS[2].r])
                    for kc in range(8):
                        mm(PS[3].t[R, :], kds.t[:, kc, :], xn.t[:, kc, :], kc == 0, kc == 7, [kds.r, xn.r], [PS[3].r])
                    tt("dve", t1.t[R, :], PS[2].t[R, :], rp.t[R, 0, :], ALU.mult, [rp.r], [PS[2].r, t1.r])
                    tt("dve", t2.t[R, :], PS[3].t[R, :], rp.t[R, 1, :], ALU.mult, [rp.r], [PS[3].r, t2.r])
                    tt("dve", kr.t[R, :], t1.t[R, :], t2.t[R, :], ALU.add, [t1.r, t2.r], [kr.r])
                    rms_stats(sq, ckv.t[:], 2, TT, KVL, NORM_EPS, rstd, [ckv.r], PS[7])
                    for c in range(2):
                        stt("dve", ckvn.t[:, c, :], ckv.t[:, c, :], vK(c), rstd.t[:, :], ALU.mult, ALU.mult,
                            [ckv.r, rstd.r, vt.r], [ckvn.r])
                    for h in range(H):
                        K = KT[h % 2]
                        bk = PS[h % 2]
                        for kc in range(2):
                            mm(bk.t[0:64, :], kuk.t[:, kc, h * 64:(h + 1) * 64], ckvn.t[:, kc, :], kc == 0, kc == 1,
                               [kuk.r, ckvn.r], [bk.r])
                        cp("act", K.t[0:64, :], bk.t[0:64, :], [], [bk.r, K.r])
                        cp("pool", K.t[R, :], kr.t[R, :], [kr.r], [K.r])
                        P.dma(kscr[h, :, t0:t0 + TT], K.t[0:96, :], reads=[K.r], writes=[Rkv[it]])
                    for tb in range(TT // 128):
                        V = Vt[tb % 2]
                        for hf in range(2):
                            bk = PS[4 + hf]
                            for kc in range(2):
                                mm(bk.t[:, :], ckvn.t[:, kc, tb * 128:(tb + 1) * 128], kuv.t[:, kc, hf * 512:(hf + 1) * 512],
                                   kc == 0, kc == 1, [kuv.r, ckvn.r], [bk.r])
                            cp("act" if hf == 0 else "dve", V.t[:, hf * 8:(hf + 1) * 8, 0:64],
                               bk.t[:, :].rearrange("p (h d) -> p h d", d=64), [], [bk.r, V.r])
                        P.dma(vscr[:, t0 + tb * 128:t0 + (tb + 1) * 128, :].rearrange("h t d -> t h d"), V.t[:],
                              reads=[V.r], writes=[Rkv[it]])
                P.barrier()

        def mla_layer(j, layer, src, dst):
            TT = 512
            with ExitStack() as st:
                qd = sbt(st, "qd", [128, 8, QL], BF16)
                qu = sbt(st, "qu", [128, 3, H * 96], BF16)
                qus = sbt(st, "qus", [128, 3, H * 32], BF16)
                ow = sbt(st, "ow", [64, H, D], BF16)
                load_wk(qd, qd_d[j], 8, QL)
                load_wk(qu, qu_d[j], 3, H * 96)
                load_wk(qus, qus_d[j], 3, H * 32)
                for h in range(H):
                    load_w(lambda r, c0, cw, h=h: ow.t[0:r, h, c0:c0 + cw], ow.r, ow_d[j, h * 64:(h + 1) * 64, :], 64, D)
                xt = sbt(st, "xt", [128, 8, TT], F32)
                xn = sbt(st, "xn", [128, 8, TT], BF16)
                sq = sbt(st, "sq", [128, 8, TT], BF16)
                rstd = sbt(st, "rstd", [128, TT], F32)
                cq = sbt(st, "cq", [128, 3, TT], F32)
                cqn = sbt(st, "cqn", [128, 3, TT], BF16)
                rp = sbt(st, "rp", [128, 2, TT], F32)
                t1 = sbt(st, "t1", [128, TT], F32)
                t2 = sbt(st, "t2", [128, TT], F32)
                QT = sbt(st, "QT", [128, H, TT], BF16)
                OT = sbt(st, "OT", [64, H, TT], BF16)
                KT = [sbt(st, "KT%d" % k, [128, T], BF16) for k in range(2)]
                VT = [sbt(st, "VT%d" % k, [128, T // 128, 128], BF16) for k in range(2)]
                PT = [sbt(st, "PT%d" % k, [128, TT], BF16) for k in range(3)]
                rec = sbt(st, "rec", [64, TT], F32)
                msb = sbt(st, "msb", [128, 8, TT], F32)
                R = slice(64, 96)
                pi = 0
                for it in range(T // TT):
                    t0 = it * TT
                    P.dma(xt.t[:], xtile(src, t0, TT), reads=rx(t0, TT), writes=[xt.r])
                    P.dma(rp.t[R, :, :], rope_d[R, :, t0:t0 + TT], reads=[Rin], writes=[rp.r])
                    rms_stats(sq, xt.t[:], 8, TT, D, NORM_EPS, rstd, [xt.r], PS[2])
                    for c in range(8):
                        stt(ew(), xn.t[:, c, :], xt.t[:, c, :], vD("ng%d_0" % layer, c), rstd.t[:, :], ALU.mult, ALU.mult,
                            [xt.r, rstd.r, vt.r], [xn.r])
                    for m in range(3):
                        bk = PS[m % 2]
                        for kc in range(8):
                            mm(bk.t[:, :], qd.t[:, kc, m * 128:(m + 1) * 128], xn.t[:, kc, :], kc == 0, kc == 7,
                               [qd.r, xn.r], [bk.r])
                        cp("act", cq.t[:, m, :], bk.t[:, :], [], [bk.r, cq.r])
                    rms_stats(sq, cq.t[:], 3, TT, QL, NORM_EPS, rstd, [cq.r], PS[2])
                    for c in range(3):
                        stt("dve", cqn.t[:, c, :], cq.t[:, c, :], vQ(j, c), rstd.t[:, :], ALU.mult, ALU.mult,
                            [cq.r, rstd.r, vt.r], [cqn.r])
                    for h in range(H):
                        A = PS[h % 2]
                        Bk = PS[2]
                        for kc in range(3):
                            mm(A.t[0:96, :], qu.t[:, kc, h * 96:(h + 1) * 96], cqn.t[:, kc, :], kc == 0, kc == 2,
                               [qu.r, cqn.r], [A.r])
                        for kc in range(3):
                            mm(Bk.t[R, :], qus.t[:, kc, h * 32:(h + 1) * 32], cqn.t[:, kc, :], kc == 0, kc == 2,
                               [qus.r, cqn.r], [Bk.r])
                        act(QT.t[0:64, h, :], A.t[0:64, :], AF.Copy, [], [A.r, QT.r], scale=SCALE)
                        stt("dve", t1.t[R, :], A.t[R, :], SCALE, rp.t[R, 0, :], ALU.mult, ALU.mult, [rp.r], [A.r, t1.r])
                        stt("dve", t2.t[R, :], Bk.t[R, :], SCALE, rp.t[R, 1, :], ALU.mult, ALU.mult, [rp.r], [Bk.r, t2.r])
                        tt("dve", QT.t[R, h, :], t1.t[R, :], t2.t[R, :], ALU.add, [t1.r, t2.r], [QT.r])
                    nkb = (it + 1) * 4
                    nk = nkb * 128
                    for h in range(H):
                        K = KT[h % 2]
                        V = VT[h % 2]
                        P.dma(K.t[0:96, 0:nk], kscr[h, :, 0:nk], reads=Rkv[0:it + 1], writes=[K.r])
                        P.dma(V.t[:, 0:nkb, :], vscr[h, 0:nk, :].rearrange("(kb p) d -> p kb d", p=128),
                              reads=Rkv[0:it + 1], writes=[V.r])
                        Ob = PS[6 + h % 2]
                        pts = {}

                        def qk(kb, h=h, K=K):
                            nonlocal pi
                            jd = kb - it * 4
                            c0 = max(jd, 0) * 128
                            Sb_ = PS[3 + pi % 3]
                            Pt = PT[pi % 3]
                            pi += 1
                            mm(Sb_.t[:, c0:TT], K.t[0:96, kb * 128:(kb + 1) * 128], QT.t[0:96, h, c0:TT], True, True,
                               [K.r, QT.r], [Sb_.r])
                            act(Pt.t[:, c0:TT], Sb_.t[:, c0:TT], AF.Exp, [], [Sb_.r, Pt.r])
                            if jd >= 0:
                                P.op("pool", lambda e, Pt=Pt, c0=c0: e.memset(Pt.t[64:128, c0:c0 + 64], 0.0), [], [Pt.r])
                            pts[kb] = (Pt, c0)

                        def pv(kb, V=V, Ob=Ob):
                            Pt, c0 = pts.pop(kb)
                            mm(Ob.t[:, c0:TT], V.t[:, kb, :], Pt.t[:, c0:TT], kb == 0, kb == nkb - 1, [V.r, Pt.r], [Ob.r])

                        LA = 2
                        for kb in range(min(LA, nkb)):
                            qk(kb)
                        for kb in range(nkb):
                            if kb + LA < nkb:
                                qk(kb + LA)
                            pv(kb)
                        P.op("dve", lambda e, Ob=Ob: e.reciprocal(rec.t[:, :], Ob.t[64:128, :]), [], [Ob.r, rec.r])
                        tt("dve", OT.t[:, h, :], Ob.t[0:64, :], rec.t[:, :], ALU.mult, [rec.r], [Ob.r, OT.r])
                    for m in range(8):
                        bk = PS[m % 2]
                        for h in range(H):
                            mm(bk.t[:, :], ow.t[0:64, h, m * 128:(m + 1) * 128], OT.t[0:64, h, :], h == 0, h == H - 1,
                               [ow.r, OT.r], [bk.r])
                        cp("act", msb.t[:, m, :], bk.t[:, :], [], [bk.r, msb.r])
                    rms_stats(sq, msb.t[:], 8, TT, D, NORM_EPS, rstd, [msb.r], PS[2])
                    for c in range(8):
                        stt("dve", msb.t[:, c, :], msb.t[:, c, :], vD("ng%d_1" % layer, c), rstd.t[:, :], ALU.mult, ALU.mult,
                            [rstd.r, vt.r], [msb.r])
                    tt("pool", msb.t[:], msb.t[:], xt.t[:], ALU.add, [xt.r], [msb.r])
                    P.dma(xtile(dst, t0, TT), msb.t[:], reads=[msb.r], writes=rx(t0, TT))
                P.barrier()

        P.barrier()
        cur = xT
        for layer in range(depth):
            if layer < NA:
                rwkv_layer(layer, layer, cur, xs)
            else:
                if layer == NA:
                    kv_prep(xs)
                mla_layer(layer - NA, layer, xs, xs)
            cur = xs
            ffn_layer(layer, xs, y if layer == depth - 1 else xs)
        P.barrier()
        P.emit()
        nc._n_ops = P.n
    return nc


NVT = [0, 0, 0]
_CACHE = {}


def prep_inputs(inputs):
    inp = {k: np.asarray(v) for k, v in inputs.items()}
    B, T, _ = inp["x"].shape
    vt, nd, nf = _pack_tables(inp)
    NVT[0], NVT[1], NVT[2] = vt.shape[1], nd, nf
    f32 = lambda a: np.ascontiguousarray(a, dtype=np.float32)
    kd = inp["kv_w_down"]
    kds = np.concatenate([kd[:, 272:288], kd[:, 256:272]], 1)
    ku = inp["kv_w_up"].reshape(KVL, H, 128)
    qu = inp["q_w_up"].reshape(2, QL, H, 96)
    qus = np.concatenate([qu[..., 80:96], qu[..., 64:80]], -1).reshape(2, QL, H * 32)
    shared = {
        "vt": vt, "cst": _consts(), "rope": _rope(T),
        "ffn_w_in": f32(inp["ffn_w_in"]), "ffn_w_out": f32(inp["ffn_w_out"]),
        "a_w_rkv": f32(inp["a_w_rkv"]), "a_w1": f32(inp["a_w1"]), "a_w2": f32(inp["a_w2"]),
        "a_a1": f32(inp["a_a1"]), "a_a2": f32(inp["a_a2"]), "a_g1": f32(inp["a_g1"]), "a_g2": f32(inp["a_g2"]),
        "a_w_o": f32(inp["a_w_o"]), "kv_w_down": f32(kd), "kv_w_down_sw": f32(kds),
        "kv_w_up_k": f32(ku[:, :, 0:64].reshape(KVL, H * 64)), "kv_w_up_v": f32(ku[:, :, 64:128].reshape(KVL, H * 64)),
        "q_w_down": f32(inp["q_w_down"]), "q_w_up": f32(inp["q_w_up"]), "q_w_up_sw": f32(qus),
        "o_w": f32(inp["o_w"]),
    }
    maps = []
    for b in range(B):
        m = dict(shared)
        m["xT"] = f32(inp["x"][b].T)
        maps.append(m)
    return maps, B, T


def kernel(**inputs):
    maps, B, T = prep_inputs(inputs)
    key = (T, DEPTH)
    if key not in _CACHE:
        _CACHE[key] = build(T)
    nc = _CACHE[key]
    res = run_bass_kernel_spmd(nc, maps, core_ids=list(range(B)))
    out = np.stack([np.asarray(r["y"]).T for r in res.results], 0)
    return np.ascontiguousarray(out.astype(np.float32))
```

```python
import math
from contextlib import ExitStack
import numpy as np
import concourse.bass as bass
import concourse.mybir as mybir
from concourse.bass_utils import run_bass_kernel_spmd

F32 = mybir.dt.float32
BF16 = mybir.dt.bfloat16
AF = mybir.ActivationFunctionType
ALU = mybir.AluOpType

D = 1024
DEPTH = 4
NA = 2
H = 16
FF = 2816
NF = FF // 128
QL = 384
KVL = 256
LNX_EPS = 64e-5
NORM_EPS = 1e-6
SCALE = 1.0 / math.sqrt(96.0)

COMPUTE = ("pe", "act", "dve", "pool")
SEM_LIMIT = 30000
NDMA_SEM = 24


class Res:
    __slots__ = ("name", "w", "r")

    def __init__(self, name=""):
        self.name = name
        self.w = None
        self.r = {}


class Tl:
    __slots__ = ("t", "r")

    def __init__(self, t, r):
        self.t = t
        self.r = r


class Prog:
    def __init__(self, nc, stack):
        self.nc = nc
        self.stack = stack
        self.streams = {e: [] for e in COMPUTE + ("sp",)}
        self.sems = {}
        self.cnt = {}
        for e in COMPUTE:
            self.sems[e] = [self._newsem(e + "0")]
            self.cnt[e] = (0, 0)
        self.dma_sems = [self._newsem("dma%d" % i) for i in range(NDMA_SEM)]
        self.dma_cnt = [0] * NDMA_SEM
        self.dma_i = 0
        self.seen = {e: {} for e in self.streams}
        self.n = 0

    def _newsem(self, name):
        return self.stack.enter_context(self.nc.semaphore(name))

    def _semof(self, key, idx):
        if isinstance(key, tuple):
            return self.dma_sems[key[1]]
        return self.sems[key][idx]

    @staticmethod
    def _need(waits, tok):
        if tok is None:
            return
        key, idx, val = tok
        cur = waits.get(key)
        if cur is None or (idx, val) > cur:
            waits[key] = (idx, val)

    def _collect(self, q, reads, writes, eng):
        waits = {}
        for r in reads:
            self._need(waits, r.w)
        for w in writes:
            self._need(waits, w.w)
            for k, (i, v) in w.r.items():
                if k == eng:
                    continue
                self._need(waits, (k, i, v))
        if eng == "pe":
            waits.pop("pe", None)
        out = []
        for key, (idx, val) in waits.items():
            s = self.seen[q].get(key)
            if s is not None and s >= (idx, val):
                continue
            self.seen[q][key] = (idx, val)
            out.append((self._semof(key, idx), val))
        return out

    def op(self, eng, fn, reads=(), writes=()):
        waits = self._collect(eng, reads, writes, eng)
        idx, val = self.cnt[eng]
        if val >= SEM_LIMIT:
            idx += 1
            val = 0
            self.sems[eng].append(self._newsem("%s%d" % (eng, idx)))
        val += 1
        self.cnt[eng] = (idx, val)
        self.streams[eng].append((waits, fn, (self.sems[eng][idx], 1)))
        tok = (eng, idx, val)
        for r in reads:
            r.r[eng] = (idx, val)
        for w in writes:
            w.w = tok
            w.r = {}
        self.n += 1
        return tok

    def dma(self, out, in_, reads=(), writes=(), q="sp"):
        waits = self._collect(q, reads, writes, None)
        j = self.dma_i % NDMA_SEM
        self.dma_i += 1
        self.dma_cnt[j] += 16
        val = self.dma_cnt[j]
        key = ("dma", j)
        self.streams[q].append(
            (waits, lambda e: e.dma_start(out=out, in_=in_), (self.dma_sems[j], 16)))
        tok = (key, 0, val)
        for r in reads:
            r.r[key] = (0, val)
        for w in writes:
            w.w = tok
            w.r = {}
        self.n += 1
        return tok

    def barrier(self):
        for q in self.streams:
            waits = []
            for e in COMPUTE:
                if e == q:
                    continue
                idx, val = self.cnt[e]
                if val == 0:
                    continue
                sn = self.seen[q].get(e)
                if sn is not None and sn >= (idx, val):
                    continue
                self.seen[q][e] = (idx, val)
                waits.append((self.sems[e][idx], val))
            for j in range(NDMA_SEM):
                val = self.dma_cnt[j]
                if val == 0:
                    continue
                key = ("dma", j)
                sn = self.seen[q].get(key)
                if sn is not None and sn >= (0, val):
                    continue
                self.seen[q][key] = (0, val)
                waits.append((self.dma_sems[j], val))
            self.streams[q].append((waits, None, None))

    def emit(self):
        nc = self.nc
        with nc.Block() as block:
            def run(stream):
                def body(e):
                    for waits, fn, inc in stream:
                        for s, v in waits:
                            e.wait_ge(s, v)
                        if fn is not None:
                            ins = fn(e)
                            if inc is not None:
                                ins.then_inc(inc[0], inc[1])
                return body
            block.tensor(run(self.streams["pe"]))
            block.scalar(run(self.streams["act"]))
            block.vector(run(self.streams["dve"]))
            block.gpsimd(run(self.streams["pool"]))
            block.sync(run(self.streams["sp"]))


VD = {}


def _pack_tables(inp):
    vecs = []

    def add(name, v):
        VD[name] = len(vecs)
        vecs.append(np.asarray(v, np.float32).reshape(D))

    for l in range(DEPTH):
        for j in range(4):
            add("ng%d_%d" % (l, j), inp["norm_g"][l, j])
    for i in range(NA):
        for n in range(6):
            add("mu%d_%d" % (i, n), inp["a_mu"][i, n])
        for nm in ("w0", "a0", "k_k", "k_a", "r_k", "lnx_w", "lnx_b"):
            add("%s%d" % (nm, i), inp["a_" + nm][i])
    add("kvng", inp["kv_norm_g"])
    tabD = np.stack(vecs, 0).reshape(len(vecs), 8, 128).transpose(2, 0, 1).reshape(128, -1)
    fv = []
    for l in range(DEPTH):
        for j in range(3):
            fv.append(inp["ffn_conv_w"][l, j])
        fv.append(inp["ffn_conv_b"][l])
    tabF = np.stack(fv, 0).reshape(len(fv), NF, 128).transpose(2, 0, 1).reshape(128, -1)
    qg = np.asarray(inp["q_norm_g"]).reshape(2, 3, 128).transpose(2, 0, 1).reshape(128, 6)
    kg = np.asarray(inp["kv_a_norm_g"]).reshape(2, 128).T
    vt = np.ascontiguousarray(np.concatenate([tabD, tabF, qg, kg], 1).astype(np.float32))
    return vt, tabD.shape[1], tabF.shape[1]


def _consts():
    p = np.arange(128)
    ident = np.eye(128, dtype=np.float32)
    bd = (p[:, None] // 64 == p[None, :] // 64).astype(np.float32)
    su = (p[:, None] < p[None, :]).astype(np.float32)
    ui = (p[:, None] <= p[None, :]).astype(np.float32)
    low = (p[None, :] < p[:, None]).astype(np.float32)
    ones = np.ones((128, 128), np.float32)
    return np.ascontiguousarray(np.concatenate([ident, bd, su, ui, su, ui, low, ones], 1))


C_ID, C_BD, C_M4, C_LOW, C_ONE = 0, 128, 256, 768, 896
NCST = 1024


def _rope(T):
    inv = 1.0 / (10000.0 ** (np.arange(0, 32, 2, dtype=np.float32) / 32.0))
    ang = np.arange(T, dtype=np.float32)[:, None] * inv[None, :].astype(np.float32)
    cos = np.cos(ang).astype(np.float32).T
    sin = np.sin(ang).astype(np.float32).T
    tab = np.zeros((128, 2, T), np.float32)
    tab[64:80, 0] = cos
    tab[80:96, 0] = cos
    tab[64:80, 1] = -sin
    tab[80:96, 1] = sin
    return tab


def build(T, depth=DEPTH, dbg=False):
    nc = bass.Bass("TRN2", target_bir_lowering=False)
    NT5 = T // 512
    NT2 = T // 256

    def din(name, shape):
        return nc.dram_tensor(name, list(shape), F32, kind="ExternalInput").ap()

    xT = din("xT", [D, T])
    vt_d = din("vt", [128, NVT[0]])
    cst_d = din("cst", [128, NCST])
    rope_d = din("rope", [128, 2, T])
    w_in_d = din("ffn_w_in", [DEPTH, D, 2 * FF])
    w_out_d = din("ffn_w_out", [DEPTH, FF, D])
    wrkv_d = din("a_w_rkv", [NA, 3, D, D])
    w1_d = din("a_w1", [NA, D, 64])
    w2_d = din("a_w2", [NA, 64, D])
    a1_d = din("a_a1", [NA, D, 64])
    a2_d = din("a_a2", [NA, 64, D])
    g1_d = din("a_g1", [NA, D, 160])
    g2_d = din("a_g2", [NA, 160, D])
    wo_d = din("a_w_o", [NA, D, D])
    kd_d = din("kv_w_down", [D, 288])
    kds_d = din("kv_w_down_sw", [D, 32])
    kuk_d = din("kv_w_up_k", [KVL, H * 64])
    kuv_d = din("kv_w_up_v", [KVL, H * 64])
    qd_d = din("q_w_down", [2, D, QL])
    qu_d = din("q_w_up", [2, QL, H * 96])
    qus_d = din("q_w_up_sw", [2, QL, H * 32])
    ow_d = din("o_w", [2, D, D])
    y = nc.dram_tensor("y", [D, T], F32, kind="ExternalOutput").ap()
    xs = nc.dram_tensor("xs", [D, T], F32).ap()
    hscr = nc.dram_tensor("hscr", [FF, T], BF16).ap()
    kscr = nc.dram_tensor("kscr", [H, 96, T], BF16).ap()
    vscr = nc.dram_tensor("vscr", [H, T, 128], BF16).ap()

    Rx = [Res("x%d" % i) for i in range(NT2)]
    Rh = [Res("h%d" % i) for i in range(NT5)]
    Rkv = [Res("kv%d" % i) for i in range(NT5)]
    Rin = Res("in")

    with ExitStack() as top:
        P = Prog(nc, top)
        top.enter_context(nc.allow_low_precision("bf16 matmul operands, fp32 accumulate"))

        uid = [0]

        def sbt(st, name, shape, dt):
            uid[0] += 1
            nm = "sb%d_%s" % (uid[0], name)
            return Tl(st.enter_context(nc.sbuf_tensor(nm, list(shape), dt)), Res(nm))

        PS = [Tl(top.enter_context(nc.psum_tensor("ps%d" % i, [128, 512], F32)), Res("ps%d" % i))
              for i in range(8)]
        vt = sbt(top, "vt", [128, NVT[0]], F32)
        cst = sbt(top, "cst", [128, NCST], F32)
        onesb = sbt(top, "onesb", [128, 128], BF16)
        stg = [sbt(top, "stg%d" % i, [128, 1024], F32) for i in range(2)]
        stg_i = [0]
        P.dma(vt.t[:], vt_d, writes=[vt.r])
        P.dma(cst.t[:], cst_d, writes=[cst.r])
        P.op("dve", lambda e: e.tensor_copy(onesb.t[:], cst.t[:, C_ONE:C_ONE + 128]), [cst.r], [onesb.r])
        identb_t = sbt(top, "identb", [128, 128], BF16)
        P.op("dve", lambda e: e.tensor_copy(identb_t.t[:], cst.t[:, C_ID:C_ID + 128]), [cst.r], [identb_t.r])
        identb = identb_t.t[:, :]

        ident = cst.t[:, C_ID:C_ID + 128]
        bdones = cst.t[:, C_BD:C_BD + 128]
        mask4 = cst.t[:, C_M4:C_M4 + 512]
        lowm = cst.t[:, C_LOW:C_LOW + 128]
        ones = cst.t[:, C_ONE:C_ONE + 128]

        def vD(name, c):
            i = VD[name] * 8 + c
            return vt.t[:, i:i + 1]

        def vF(l, j, f):
            i = NVT[1] + (l * 4 + j) * NF + f
            return vt.t[:, i:i + 1]

        def vQ(j, c):
            i = NVT[1] + NVT[2] + j * 3 + c
            return vt.t[:, i:i + 1]

        def vK(c):
            i = NVT[1] + NVT[2] + 6 + c
            return vt.t[:, i:i + 1]

        def mm(out, lhsT, rhs, start, stop, reads, writes):
            P.op("pe", lambda e: e.matmul(out, lhsT, rhs, start=start, stop=stop), reads, writes)

        def act(out, in_, func, reads, writes, bias=None, scale=None):
            kw = {}
            if bias is not None:
                kw["bias"] = bias
            if scale is not None:
                kw["scale"] = scale
            P.op("act", lambda e: e.activation(out, in_, func, **kw), reads, writes)

        def tt(eng, out, in0, in1, op, reads, writes):
            P.op(eng, lambda e: e.tensor_tensor(out, in0, in1, op), reads, writes)

        def ts(eng, out, in0, s1, s2, op0, op1, reads, writes):
            if s2 is None:
                P.op(eng, lambda e: e.tensor_scalar(out, in0, s1, None, op0), reads, writes)
            else:
                P.op(eng, lambda e: e.tensor_scalar(out, in0, s1, s2, op0, op1), reads, writes)

        def stt(eng, out, in0, sc, in1, op0, op1, reads, writes):
            P.op("dve", lambda e: e.scalar_tensor_tensor(out, in0, sc, in1, op0, op1), reads, writes)

        def cp(eng, out, in_, reads, writes):
            if eng == "act":
                act(out, in_, AF.Copy, reads, writes)
            else:
                P.op(eng, lambda e: e.tensor_copy(out, in_), reads, writes)

        rr = [0]

        def ew():
            rr[0] += 1
            return "pool" if rr[0] % 3 == 0 else "dve"

        def load_w(view, res, src, rows, cols):
            c0 = 0
            while c0 < cols:
                cw = min(1024, cols - c0)
                s = stg[stg_i[0] % 2]
                stg_i[0] += 1
                P.dma(s.t[0:rows, 0:cw], src[:, c0:c0 + cw], reads=[Rin], writes=[s.r])
                eng = "pool" if stg_i[0] % 2 == 0 else "dve"
                cp(eng, view(rows, c0, cw), s.t[0:rows, 0:cw], [s.r], [res])
                c0 += cw

        def load_wk(tile, src, nk, cols, rows_last=128):
            for k in range(nk):
                rows = rows_last if k == nk - 1 else 128
                load_w(lambda r, c0, cw, k=k: tile.t[0:r, k, c0:c0 + cw], tile.r,
                       src[k * 128:k * 128 + rows, :], rows, cols)

        def rms_stats(st_sq, src_ap, nch, TT, Dn, eps, rstd, src_reads, bank):
            act(st_sq.t[:, 0:nch, 0:TT], src_ap, AF.Square, src_reads, [st_sq.r])
            for c in range(nch):
                mm(bank.t[:, 0:TT], onesb.t[:, :], st_sq.t[:, c, 0:TT], c == 0, c == nch - 1,
                   [st_sq.r, onesb.r], [bank.r])
            act(rstd.t[:, 0:TT], bank.t[:, 0:TT], AF.Sqrt, [], [bank.r, rstd.r], bias=eps, scale=1.0 / Dn)
            P.op("dve", lambda e: e.reciprocal(rstd.t[:, 0:TT], rstd.t[:, 0:TT]), [], [rstd.r])

        def xtile(ap, t0, TT):
            return ap.rearrange("(c p) t -> p c t", p=128)[:, :, t0:t0 + TT]

        def rx(t0, TT):
            return Rx[t0 // 256:(t0 + TT) // 256]

        def run_jobs(gens, width, stagger):
            active = []
            it = iter(gens)
            steps0 = 0
            done = False
            while True:
                while not done and len(active) < width and (len(active) == 0 or steps0 >= stagger):
                    g = next(it, None)
                    if g is None:
                        done = True
                        break
                    active.append(g)
                    if len(active) == 1:
                        steps0 = 0
                if not active:
                    break
                for g in list(active):
                    try:
                        next(g)
                    except StopIteration:
                        active.remove(g)
                        steps0 = stagger
                steps0 += 1

        def rwkv_layer(i, layer, src, dst):
            TT = 256
            with ExitStack() as st:
                wr = sbt(st, "wr", [128, 8, D], BF16)
                wk = sbt(st, "wk", [128, 8, D], BF16)
                wv = sbt(st, "wv", [128, 8, D], BF16)
                wo = sbt(st, "wo", [128, 8, D], BF16)
                w1 = sbt(st, "w1", [128, 8, 64], BF16)
                a1 = sbt(st, "a1", [128, 8, 64], BF16)
                g1 = sbt(st, "g1", [128, 8, 160], BF16)
                w2 = sbt(st, "w2", [128, 1, D], BF16)
                a2 = sbt(st, "a2", [128, 1, D], BF16)
                g2 = sbt(st, "g2", [128, 2, D], BF16)
                for tl, srcw in ((wr, wrkv_d[i, 0]), (wk, wrkv_d[i, 1]), (wv, wrkv_d[i, 2]), (wo, wo_d[i])):
                    load_wk(tl, srcw, 8, D)
                load_wk(w1, w1_d[i], 8, 64)
                load_wk(a1, a1_d[i], 8, 64)
                load_wk(g1, g1_d[i], 8, 160)
                load_wk(w2, w2_d[i], 1, D, rows_last=64)
                load_wk(a2, a2_d[i], 1, D, rows_last=64)
                load_wk(g2, g2_d[i], 2, D, rows_last=32)

                xt = sbt(st, "xt", [128, 8, TT], F32)
                hb = sbt(st, "hb", [128, 8, TT + 1], F32)
                xsn = [sbt(st, "xs%d" % n, [128, 8, TT], BF16) for n in range(6)]
                rstd = sbt(st, "rstd", [128, TT], F32)
                tw = sbt(st, "tw", [64, TT], BF16)
                ta = sbt(st, "ta", [64, TT], BF16)
                tg = sbt(st, "tg", [128, 2, TT], BF16)
                z = sbt(st, "z", [128, 8, TT], BF16)
                sq = z
                msb = sbt(st, "msb", [128, 8, TT], F32)
                xx = msb
                S = sbt(st, "S", [128, 8, 64], F32)
                Sb = sbt(st, "Sb", [128, 8, 64], BF16)
                names = "r k v kk a lw g sq2 rn kmod b cum cex ecum bonus d".split()
                sets = []
                for k in range(2):
                    J = dict(
                        cb={nm: sbt(st, "c%d_%s" % (k, nm), [128, TT], F32) for nm in names},
                        vb=sbt(st, "vb%d" % k, [128, TT], BF16),
                        AR=sbt(st, "AR%d" % k, [128, 2 * TT], BF16),
                        KB=sbt(st, "KB%d" % k, [128, 2 * TT], BF16),
                        TM=sbt(st, "TM%d" % k, [128, 2, 3, 128], BF16),
                        bA=PS[4 * k], bB=PS[4 * k + 1], hd=[])
                    for hh in range(2):
                        J["hd"].append(dict(
                            AMs=sbt(st, "AMs%d_%d" % (k, hh), [128, 512], BF16),
                            Ls=sbt(st, "Ls%d_%d" % (k, hh), [128, 128], BF16),
                            LM=[sbt(st, "LM%d_%d_%d" % (k, hh, q), [128, 256], BF16) for q in range(2)],
                            Tt=[sbt(st, "Tt%d_%d_%d" % (k, hh, q), [128, 128], BF16) for q in range(2)],
                            Xs=sbt(st, "Xs%d_%d" % (k, hh), [128, 64], BF16),
                            Us=sbt(st, "Us%d_%d" % (k, hh), [128, 64], BF16),
                            tmpS=sbt(st, "tmpS%d_%d" % (k, hh), [128, 64], F32),
                            WK=PS[4 * k + 2 + hh]))
                    sets.append(J)
                SR = [Res("S%d" % c) for c in range(8)]
                SbR = [Res("Sb%d" % c) for c in range(8)]
                P.op("pool", lambda e: e.memset(S.t[:], 0.0), [], SR)
                P.op("pool", lambda e: e.memset(Sb.t[:], 0.0), [], SbR)
                P.op("pool", lambda e: e.memset(hb.t[:, :, 0:1], 0.0), [], [hb.r])

                def job(c, J):
                    B = J["cb"]
                    vb, AR, KB, TM, bA, bB, hd = J["vb"], J["AR"], J["KB"], J["TM"], J["bA"], J["bB"], J["hd"]
                    cs = slice(c * 128, (c + 1) * 128)
                    for kc in range(8):
                        mm(bA.t[:, 0:TT], wr.t[:, kc, cs], xsn[0].t[:, kc, :], kc == 0, kc == 7, [wr.r, xsn[0].r], [bA.r])
                    for kc in range(8):
                        mm(bA.t[:, TT:2 * TT], wk.t[:, kc, cs], xsn[1].t[:, kc, :], kc == 0, kc == 7, [wk.r, xsn[1].r], [bA.r])
                    for kc in range(8):
                        mm(bB.t[:, 0:TT], wv.t[:, kc, cs], xsn[2].t[:, kc, :], kc == 0, kc == 7, [wv.r, xsn[2].r], [bB.r])
                    mm(bB.t[:, TT:2 * TT], w2.t[0:64, 0, cs], tw.t[:], True, True, [w2.r, tw.r], [bB.r])
                    yield
                    cp("act", B["r"].t[:], bA.t[:, 0:TT], [], [bA.r, B["r"].r])
                    ts("dve", B["kk"].t[:], bA.t[:, TT:2 * TT], vD("k_k%d" % i, c), None, ALU.mult, None, [vt.r], [bA.r, B["kk"].r])
                    cp("act", B["k"].t[:], bA.t[:, TT:2 * TT], [], [bA.r, B["k"].r])
                    yield
                    cp("dve", B["v"].t[:], bB.t[:, 0:TT], [], [bB.r, B["v"].r])
                    cp("act", vb.t[:], bB.t[:, 0:TT], [], [bB.r, vb.r])
                    act(B["lw"].t[:], bB.t[:, TT:2 * TT], AF.Sigmoid, [vt.r], [bB.r, B["lw"].r], bias=vD("w0%d" % i, c))
                    ts("pool", B["lw"].t[:], B["lw"].t[:], -math.exp(-0.5), None, ALU.mult, None, [], [B["lw"].r])
                    yield
                    mm(bA.t[:, 0:TT], a2.t[0:64, 0, cs], ta.t[:], True, True, [a2.r, ta.r], [bA.r])
                    mm(bA.t[:, TT:2 * TT], g2.t[:, 0, cs], tg.t[:, 0, :], True, False, [g2.r, tg.r], [bA.r])
                    mm(bA.t[:, TT:2 * TT], g2.t[0:32, 1, cs], tg.t[0:32, 1, :], False, True, [g2.r, tg.r], [bA.r])
                    act(B["a"].t[:], bA.t[:, 0:TT], AF.Sigmoid, [vt.r], [bA.r, B["a"].r], bias=vD("a0%d" % i, c))
                    cp("act", B["g"].t[:], bA.t[:, TT:2 * TT], [], [bA.r, B["g"].r])
                    yield
                    act(B["sq2"].t[:], B["kk"].t[:], AF.Square, [B["kk"].r], [B["sq2"].r])
                    mm(bB.t[:, 0:TT], bdones, B["sq2"].t[:], True, True, [cst.r, B["sq2"].r], [bB.r])
                    act(B["rn"].t[:], bB.t[:, 0:TT], AF.Sqrt, [], [bB.r, B["rn"].r])
                    ts("dve", B["rn"].t[:], B["rn"].t[:], 1e-12, None, ALU.max, None, [], [B["rn"].r])
                    P.op("dve", lambda e: e.reciprocal(B["rn"].t[:], B["rn"].t[:]), [], [B["rn"].r])
                    tt("dve", B["kk"].t[:], B["kk"].t[:], B["rn"].t[:], ALU.mult, [B["rn"].r], [B["kk"].r])
                    yield
                    ts("pool", B["kmod"].t[:], B["a"].t[:], -1.0, vD("k_a%d" % i, c), ALU.add, ALU.mult, [B["a"].r, vt.r], [B["kmod"].r])
                    stt("dve", B["kmod"].t[:], B["kmod"].t[:], 1.0, B["k"].t[:], ALU.add, ALU.mult, [B["k"].r], [B["kmod"].r])
                    tt("pool", B["b"].t[:], B["kk"].t[:], B["a"].t[:], ALU.mult, [B["kk"].r, B["a"].r], [B["b"].r])
                    for ch in range(2):
                        sl = slice(ch * 128, (ch + 1) * 128)
                        P.op("dve", lambda e, sl=sl: e.tensor_tensor_scan(
                            B["cum"].t[:, sl], ones, B["lw"].t[:, sl], 0.0, ALU.mult, ALU.add), [cst.r, B["lw"].r], [B["cum"].r])
                    tt("pool", B["cex"].t[:], B["cum"].t[:], B["lw"].t[:], ALU.subtract, [B["cum"].r, B["lw"].r], [B["cex"].r])
                    act(B["ecum"].t[:], B["cum"].t[:], AF.Exp, [B["cum"].r], [B["ecum"].r])
                    act(B["cum"].t[:], B["cum"].t[:], AF.Exp, [], [B["cum"].r], scale=-1.0)
                    act(B["cex"].t[:], B["cex"].t[:], AF.Exp, [], [B["cex"].r])
                    yield
                    for ch in range(2):
                        sl = slice(ch * 128, (ch + 1) * 128)
                        o = ch * 256
                        stt("dve", AR.t[:, o:o + 128], B["kk"].t[:, sl], -1.0, B["cex"].t[:, sl], ALU.mult, ALU.mult,
                            [B["kk"].r, B["cex"].r], [AR.r])
                        tt("pool", AR.t[:, o + 128:o + 256], B["r"].t[:, sl], B["ecum"].t[:, sl], ALU.mult, [B["r"].r, B["ecum"].r], [AR.r])
                        tt("dve", KB.t[:, o:o + 128], B["kmod"].t[:, sl], B["cum"].t[:, sl], ALU.mult, [B["kmod"].r, B["cum"].r], [KB.r])
                        tt("pool", KB.t[:, o + 128:o + 256], B["b"].t[:, sl], B["cum"].t[:, sl], ALU.mult, [B["b"].r, B["cum"].r], [KB.r])
                    yield
                    stt("dve", B["sq2"].t[:], B["r"].t[:], vD("r_k%d" % i, c), B["kmod"].t[:], ALU.mult, ALU.mult,
                        [B["r"].r, B["kmod"].r, vt.r], [B["sq2"].r])
                    mm(bB.t[:, TT:2 * TT], bdones, B["sq2"].t[:], True, True, [cst.r, B["sq2"].r], [bB.r])
                    tt("dve", B["bonus"].t[:], bB.t[:, TT:2 * TT], B["v"].t[:], ALU.mult, [B["v"].r], [bB.r, B["bonus"].r])
                    for ch in range(2):
                        sl = slice(ch * 128, (ch + 1) * 128)
                        o = ch * 256
                        mm(bA.t[:, ch * 128:ch * 128 + 128], vb.t[:, sl], identb, True, True, [vb.r, identb_t.r], [bA.r])
                        mm(bA.t[:, 256 + ch * 128:256 + ch * 128 + 128], KB.t[:, o:o + 128], identb, True, True, [KB.r, identb_t.r], [bA.r])
                        mm(bB.t[:, ch * 128:ch * 128 + 128], KB.t[:, o + 128:o + 256], identb, True, True, [KB.r, identb_t.r], [bB.r])
                    yield
                    for ch in range(2):
                        cp("act", TM.t[:, ch, 0, :], bA.t[:, ch * 128:ch * 128 + 128], [], [bA.r, TM.r])
                        cp("dve", TM.t[:, ch, 1, :], bA.t[:, 256 + ch * 128:256 + ch * 128 + 128], [], [bA.r, TM.r])
                        cp("act", TM.t[:, ch, 2, :], bB.t[:, ch * 128:ch * 128 + 128], [], [bB.r, TM.r])
                    yield
                    for ch in range(2):
                        o = ch * 256
                        for hh in range(2):
                            p = slice(hh * 64, hh * 64 + 64)
                            Hh = hd[hh]
                            WK = Hh["WK"]
                            mm(WK.t[:, 0:256], KB.t[p, o:o + 128], AR.t[p, o:o + 256], True, True, [KB.r, AR.r], [WK.r])
                            mm(WK.t[:, 256:512], KB.t[p, o + 128:o + 256], AR.t[p, o:o + 256], True, True, [KB.r, AR.r], [WK.r])
                            TB = bA if hh == 0 else bB
                            mm(TB.t[:, 128:256], AR.t[p, o:o + 128], KB.t[p, o + 128:o + 256], True, True, [KB.r, AR.r], [TB.r])
                        yield
                        for hh in range(2):
                            Hh = hd[hh]
                            WK = Hh["WK"]
                            TB = bA if hh == 0 else bB
                            tt("dve", Hh["AMs"].t[:], WK.t[:], mask4, ALU.mult, [cst.r], [WK.r, Hh["AMs"].r])
                            tt("dve", Hh["Ls"].t[:], TB.t[:, 128:256], lowm, ALU.mult, [cst.r], [TB.r, Hh["Ls"].r])
                            tt("pool", Hh["Tt"][0].t[:], Hh["AMs"].t[:, 256:384], identb, ALU.add, [Hh["AMs"].r, identb_t.r], [Hh["Tt"][0].r])
                        yield
                        for lv in range(1, 7):
                            for hh in range(2):
                                Hh = hd[hh]
                                WK = Hh["WK"]
                                if lv == 1:
                                    Lp, Mp, rd = Hh["Ls"].t[:], Hh["AMs"].t[:, 256:384], [Hh["Ls"].r, Hh["AMs"].r]
                                else:
                                    pl = Hh["LM"][(lv - 1) % 2]
                                    Lp, Mp, rd = pl.t[:, 0:128], pl.t[:, 128:256], [pl.r]
                                nl = Hh["LM"][lv % 2]
                                mm(WK.t[:, 128:256], Mp, Lp, True, True, rd, [WK.r])
                                if lv < 6:
                                    mm(WK.t[:, 256:384], Lp, Mp, True, True, rd, [WK.r])
                                    cp("act", nl.t[:, 0:256], WK.t[:, 128:384], [], [WK.r, nl.r])
                                else:
                                    cp("act", nl.t[:, 0:128], WK.t[:, 128:256], [], [WK.r, nl.r])
                            yield
                            for hh in range(2):
                                Hh = hd[hh]
                                WK = Hh["WK"]
                                nl = Hh["LM"][lv % 2]
                                Tc = Hh["Tt"][(lv - 1) % 2]
                                Tn = Hh["Tt"][lv % 2]
                                TB = bA if hh == 0 else bB
                                mm(TB.t[:, 0:128], nl.t[:, 0:128], Tc.t[:], True, True, [nl.r, Tc.r], [TB.r])
                                tt("dve", Tn.t[:], TB.t[:, 0:128], Tc.t[:], ALU.add, [Tc.r], [TB.r, Tn.r])
                            yield
                        for hh in range(2):
                            p = slice(hh * 64, hh * 64 + 64)
                            fs = slice(hh * 64, hh * 64 + 64)
                            Hh = hd[hh]
                            WK = Hh["WK"]
                            mm(WK.t[:, 0:64], AR.t[p, o:o + 128], Sb.t[p, c, :], True, False, [AR.r, SbR[c]], [WK.r])
                            mm(WK.t[:, 0:64], Hh["AMs"].t[:, 0:128], TM.t[:, ch, 0, fs], False, True, [Hh["AMs"].r, TM.r], [WK.r])
                            cp("act", Hh["Xs"].t[:], WK.t[:, 0:64], [], [WK.r, Hh["Xs"].r])
                        yield
                        for hh in range(2):
                            Hh = hd[hh]
                            WK = Hh["WK"]
                            Tf = Hh["Tt"][0]
                            mm(WK.t[:, 64:128], Tf.t[:], Hh["Xs"].t[:], True, True, [Tf.r, Hh["Xs"].r], [WK.r])
                            cp("act", Hh["Us"].t[:], WK.t[:, 64:128], [], [WK.r, Hh["Us"].r])
                        yield
                        for hh in range(2):
                            p = slice(hh * 64, hh * 64 + 64)
                            fs = slice(hh * 64, hh * 64 + 64)
                            Hh = hd[hh]
                            WK = Hh["WK"]
                            yo = WK.t[p, 128:256]
                            mm(yo, Sb.t[p, c, :], AR.t[p, o + 128:o + 256], True, False, [SbR[c], AR.r], [WK.r])
                            mm(yo, Hh["Us"].t[:], Hh["AMs"].t[:, 384:512], False, False, [Hh["Us"].r, Hh["AMs"].r], [WK.r])
                            mm(yo, TM.t[:, ch, 0, fs], Hh["AMs"].t[:, 128:256], False, True, [TM.r, Hh["AMs"].r], [WK.r])
                            so = WK.t[p, 256:320]
                            mm(so, TM.t[:, ch, 2, fs], Hh["Us"].t[:], True, False, [TM.r, Hh["Us"].r], [WK.r])
                            mm(so, TM.t[:, ch, 1, fs], TM.t[:, ch, 0, fs], False, True, [TM.r], [WK.r])
                        yield
                        for hh in range(2):
                            p = slice(hh * 64, hh * 64 + 64)
                            Hh = hd[hh]
                            WK = Hh["WK"]
                            yo = WK.t[p, 128:256]
                            so = WK.t[p, 256:320]
                            cp("act", B["d"].t[p, ch * 128:ch * 128 + 128], yo, [], [WK.r, B["d"].r])
                            tt("dve", Hh["tmpS"].t[p, :], so, S.t[p, c, :], ALU.add, [SR[c]], [WK.r, Hh["tmpS"].r])
                            wc = B["ecum"].t[p, ch * 128 + 127:ch * 128 + 128]
                            ts("dve", S.t[p, c, :], Hh["tmpS"].t[p, :], wc, None, ALU.mult, None, [Hh["tmpS"].r, B["ecum"].r], [SR[c]])
                            ts("pool", Sb.t[p, c, :], Hh["tmpS"].t[p, :], wc, None, ALU.mult, None, [Hh["tmpS"].r, B["ecum"].r], [SbR[c]])
                        yield
                    mm(bB.t[:, 0:TT], bdones, B["d"].t[:], True, True, [cst.r, B["d"].r], [bB.r])
                    stt("dve", B["d"].t[:], bB.t[:, 0:TT], -1.0 / 64, B["d"].t[:], ALU.mult, ALU.add, [], [bB.r, B["d"].r])
                    act(B["sq2"].t[:], B["d"].t[:], AF.Square, [B["d"].r], [B["sq2"].r])
                    mm(bB.t[:, TT:2 * TT], bdones, B["sq2"].t[:], True, True, [cst.r, B["sq2"].r], [bB.r])
                    act(B["rn"].t[:], bB.t[:, TT:2 * TT], AF.Sqrt, [], [bB.r, B["rn"].r], bias=LNX_EPS, scale=1.0 / 64)
                    P.op("dve", lambda e: e.reciprocal(B["rn"].t[:], B["rn"].t[:]), [], [B["rn"].r])
                    yield
                    stt("dve", B["d"].t[:], B["d"].t[:], vD("lnx_w%d" % i, c), B["rn"].t[:], ALU.mult, ALU.mult, [B["rn"].r, vt.r], [B["d"].r])
                    stt("dve", B["d"].t[:], B["d"].t[:], vD("lnx_b%d" % i, c), B["bonus"].t[:], ALU.add, ALU.add, [B["bonus"].r, vt.r], [B["d"].r])
                    tt("pool", z.t[:, c, :], B["d"].t[:], B["g"].t[:], ALU.mult, [B["d"].r, B["g"].r], [z.r])
                    yield

                for it in range(T // TT):
                    t0 = it * TT
                    P.dma(xt.t[:], xtile(src, t0, TT), reads=rx(t0, TT), writes=[xt.r])
                    rms_stats(sq, xt.t[:], 8, TT, D, NORM_EPS, rstd, [xt.r], PS[3])
                    for c in range(8):
                        stt("dve", hb.t[:, c, 1:TT + 1], xt.t[:, c, :], vD("ng%d_0" % layer, c), rstd.t[:, :],
                            ALU.mult, ALU.mult, [xt.r, rstd.r, vt.r], [hb.r])
                    tt("pool", xx.t[:], hb.t[:, :, 0:TT], hb.t[:, :, 1:TT + 1], ALU.subtract, [hb.r], [xx.r])
                    for n in range(6):
                        for c in range(8):
                            stt("dve", xsn[n].t[:, c, :], xx.t[:, c, :], vD("mu%d_%d" % (i, n), c),
                                hb.t[:, c, 1:TT + 1], ALU.mult, ALU.add, [xx.r, hb.r, vt.r], [xsn[n].r])
                    for kc in range(8):
                        mm(PS[0].t[0:64, 0:TT], w1.t[:, kc, :], xsn[3].t[:, kc, :], kc == 0, kc == 7, [w1.r, xsn[3].r], [PS[0].r])
                    act(tw.t[:], PS[0].t[0:64, 0:TT], AF.Tanh, [], [PS[0].r, tw.r])
                    for kc in range(8):
                        mm(PS[1].t[0:64, 0:TT], a1.t[:, kc, :], xsn[4].t[:, kc, :], kc == 0, kc == 7, [a1.r, xsn[4].r], [PS[1].r])
                    cp("act", ta.t[:], PS[1].t[0:64, 0:TT], [], [PS[1].r, ta.r])
                    for kc in range(8):
                        mm(PS[2].t[:, 0:TT], g1.t[:, kc, 0:128], xsn[5].t[:, kc, :], kc == 0, kc == 7, [g1.r, xsn[5].r], [PS[2].r])
                    for kc in range(8):
                        mm(PS[2].t[0:32, TT:2 * TT], g1.t[:, kc, 128:160], xsn[5].t[:, kc, :], kc == 0, kc == 7, [g1.r, xsn[5].r], [PS[2].r])
                    act(tg.t[:, 0, :], PS[2].t[:, 0:TT], AF.Sigmoid, [], [PS[2].r, tg.r])
                    act(tg.t[0:32, 1, :], PS[2].t[0:32, TT:2 * TT], AF.Sigmoid, [], [PS[2].r, tg.r])

                    run_jobs((job(c, sets[c % 2]) for c in range(8)), 2, 20)

                    for m in range(8):
                        bk = PS[m % 4]
                        for kc in range(8):
                            mm(bk.t[:, 0:TT], wo.t[:, kc, m * 128:(m + 1) * 128], z.t[:, kc, :], kc == 0, kc == 7, [wo.r, z.r], [bk.r])
                        cp("act", msb.t[:, m, :], bk.t[:, 0:TT], [], [bk.r, msb.r])
                    rms_stats(sq, msb.t[:], 8, TT, D, NORM_EPS, rstd, [msb.r], PS[4])
                    for c in range(8):
                        stt("dve", msb.t[:, c, :], msb.t[:, c, :], vD("ng%d_1" % layer, c), rstd.t[:, :], ALU.mult, ALU.mult,
                            [rstd.r, vt.r], [msb.r])
                    tt("pool", msb.t[:], msb.t[:], xt.t[:], ALU.add, [xt.r], [msb.r])
                    P.dma(xtile(dst, t0, TT), msb.t[:], reads=[msb.r], writes=rx(t0, TT))
                    cp("pool", hb.t[:, :, 0:1], hb.t[:, :, TT:TT + 1], [], [hb.r])
                P.barrier()

        def ffn_layer(layer, src, dst):
            TT = 512
            with ExitStack() as st:
                win = sbt(st, "win", [128, 8, 2 * FF], BF16)
                load_wk(win, w_in_d[layer], 8, 2 * FF)
                xt = sbt(st, "xt", [128, 8, TT], F32)
                xn = sbt(st, "xn", [128, 8, TT], BF16)
                sq = sbt(st, "sq", [128, 8, TT], BF16)
                rstd = sbt(st, "rstd", [128, TT], F32)
                carry = sbt(st, "carry", [128, NF, 2], F32)
                gbuf = [sbt(st, "gbuf%d" % k, [128, TT + 2], F32) for k in range(2)]
                acc = [sbt(st, "acc%d" % k, [128, TT], F32) for k in range(2)]
                gl = [sbt(st, "gl%d" % k, [128, TT], F32) for k in range(2)]
                hT = [sbt(st, "hT%d" % k, [128, TT], BF16) for k in range(2)]
                P.op("pool", lambda e: e.memset(carry.t[:], 0.0), [], [carry.r])
                for it in range(T // TT):
                    t0 = it * TT
                    P.dma(xt.t[:], xtile(src, t0, TT), reads=rx(t0, TT), writes=[xt.r])
                    rms_stats(sq, xt.t[:], 8, TT, D, NORM_EPS, rstd, [xt.r], PS[7])
                    for c in range(8):
                        stt(ew(), xn.t[:, c, :], xt.t[:, c, :], vD("ng%d_2" % layer, c), rstd.t[:, :], ALU.mult, ALU.mult,
                            [xt.r, rstd.r, vt.r], [xn.r])
                    for f in range(NF):
                        k2 = f % 2
                        gb, ub = PS[k2], PS[2 + k2]
                        for kc in range(8):
                            mm(gb.t[:, :], win.t[:, kc, f * 128:(f + 1) * 128], xn.t[:, kc, :], kc == 0, kc == 7,
                               [win.r, xn.r], [gb.r])
                        for kc in range(8):
                            mm(ub.t[:, :], win.t[:, kc, FF + f * 128:FF + (f + 1) * 128], xn.t[:, kc, :], kc == 0, kc == 7,
                               [win.r, xn.r], [ub.r])
                        G = gbuf[k2]
                        cp("pool", G.t[:, 0:2], carry.t[:, f, :], [carry.r], [G.r])
                        cp("act", G.t[:, 2:TT + 2], gb.t[:, :], [], [gb.r, G.r])
                        cp("pool", carry.t[:, f, :], G.t[:, TT:TT + 2], [G.r], [carry.r])
                        A = acc[k2]
                        ts("dve", A.t[:], G.t[:, 2:TT + 2], vF(layer, 2, f), vF(layer, 3, f), ALU.mult, ALU.add,
                           [G.r, vt.r], [A.r])
                        stt("dve", A.t[:], G.t[:, 1:TT + 1], vF(layer, 1, f), A.t[:], ALU.mult, ALU.add, [G.r, vt.r], [A.r])
                        stt("dve", A.t[:], G.t[:, 0:TT], vF(layer, 0, f), A.t[:], ALU.mult, ALU.add, [G.r, vt.r], [A.r])
                        act(gl[k2].t[:], A.t[:], AF.Gelu_apprx_tanh, [A.r], [gl[k2].r])
                        tt("dve", hT[k2].t[:], ub.t[:, :], gl[k2].t[:], ALU.mult, [gl[k2].r], [ub.r, hT[k2].r])
                        P.dma(hscr[f * 128:(f + 1) * 128, t0:t0 + TT], hT[k2].t[:], reads=[hT[k2].r], writes=[Rh[it]])
                P.barrier()
            with ExitStack() as st:
                wout = sbt(st, "wout", [128, NF, D], BF16)
                load_wk(wout, w_out_d[layer], NF, D)
                xt = sbt(st, "xt", [128, 8, TT], F32)
                ht = [sbt(st, "ht%d" % k, [128, NF, TT], BF16) for k in range(2)]
                msb = sbt(st, "msb", [128, 8, TT], F32)
                sq = sbt(st, "sq", [128, 8, TT], BF16)
                rstd = sbt(st, "rstd", [128, TT], F32)
                for it in range(T // TT):
                    t0 = it * TT
                    hh = ht[it % 2]
                    P.dma(hh.t[:], hscr.rearrange("(f p) t -> p f t", p=128)[:, :, t0:t0 + TT], reads=[Rh[it]], writes=[hh.r])
                    P.dma(xt.t[:], xtile(src, t0, TT), reads=rx(t0, TT), writes=[xt.r])
                    for m in range(8):
                        bk = PS[m % 4]
                        for f in range(NF):
                            mm(bk.t[:, :], wout.t[:, f, m * 128:(m + 1) * 128], hh.t[:, f, :], f == 0, f == NF - 1,
                               [wout.r, hh.r], [bk.r])
                        cp("act", msb.t[:, m, :], bk.t[:, :], [], [bk.r, msb.r])
                    rms_stats(sq, msb.t[:], 8, TT, D, NORM_EPS, rstd, [msb.r], PS[7])
                    for c in range(8):
                        stt("dve", msb.t[:, c, :], msb.t[:, c, :], vD("ng%d_3" % layer, c), rstd.t[:, :], ALU.mult, ALU.mult,
                            [rstd.r, vt.r], [msb.r])
                    tt("pool", msb.t[:], msb.t[:], xt.t[:], ALU.add, [xt.r], [msb.r])
                    P.dma(xtile(dst, t0, TT), msb.t[:], reads=[msb.r], writes=rx(t0, TT))
                P.barrier()

        def kv_prep(src):
            TT = 512
            with ExitStack() as st:
                kd = sbt(st, "kd", [128, 8, 288], BF16)
                kds = sbt(st, "kds", [128, 8, 32], BF16)
                kuk = sbt(st, "kuk", [128, 2, H * 64], BF16)
                kuv = sbt(st, "kuv", [128, 2, H * 64], BF16)
                load_wk(kd, kd_d, 8, 288)
                load_wk(kds, kds_d, 8, 32)
                load_wk(kuk, kuk_d, 2, H * 64)
                load_wk(kuv, kuv_d, 2, H * 64)
                xt = sbt(st, "xt", [128, 8, TT], F32)
                xn = sbt(st, "xn", [128, 8, TT], BF16)
                sq = sbt(st, "sq", [128, 8, TT], BF16)
                rstd = sbt(st, "rstd", [128, TT], F32)
                ckv = sbt(st, "ckv", [128, 2, TT], F32)
                ckvn = sbt(st, "ckvn", [128, 2, TT], BF16)
                rp = sbt(st, "rp", [128, 2, TT], F32)
                t1 = sbt(st, "t1", [128, TT], F32)
                t2 = sbt(st, "t2", [128, TT], F32)
                kr = sbt(st, "kr", [128, TT], BF16)
                KT = [sbt(st, "KT%d" % k, [128, TT], BF16) for k in range(2)]
                Vt = [sbt(st, "Vt%d" % k, [128, H, 128], BF16) for k in range(2)]
                for k in range(2):
                    P.op("pool", lambda e, k=k: e.memset(Vt[k].t[:, :, 64:128], 1.0), [], [Vt[k].r])
                R = slice(64, 96)
                for it in range(T // TT):
                    t0 = it * TT
                    P.dma(xt.t[:], xtile(src, t0, TT), reads=rx(t0, TT), writes=[xt.r])
                    P.dma(rp.t[R, :, :], rope_d[R, :, t0:t0 + TT], reads=[Rin], writes=[rp.r])
                    rms_stats(sq, xt.t[:], 8, TT, D, NORM_EPS, rstd, [xt.r], PS[7])
                    for c in range(8):
                        stt(ew(), xn.t[:, c, :], xt.t[:, c, :], vD("kvng", c), rstd.t[:, :], ALU.mult, ALU.mult,
                            [xt.r, rstd.r, vt.r], [xn.r])
                    for j in range(2):
                        for kc in range(8):
                            mm(PS[j].t[:, :], kd.t[:, kc, j * 128:(j + 1) * 128], xn.t[:, kc, :], kc == 0, kc == 7,
                               [kd.r, xn.r], [PS[j].r])
                        cp("act", ckv.t[:, j, :], PS[j].t[:, :], [], [PS[j].r, ckv.r])
                    for kc in range(8):
                        mm(PS[2].t[R, :], kd.t[:, kc, 256:288], xn.t[:, kc, :], kc == 0, kc == 7, [kd.r, xn.r], [PS[2].r])
                    for kc in range(8):
                        mm(PS[3].t[R, :], kds.t[:, kc, :], xn.t[:, kc, :], kc == 0, kc == 7, [kds.r, xn.r], [PS[3].r])
                    tt("dve", t1.t[R, :], PS[2].t[R, :], rp.t[R, 0, :], ALU.mult, [rp.r], [PS[2].r, t1.r])
                    tt("dve", t2.t[R, :], PS[3].t[R, :], rp.t[R, 1, :], ALU.mult, [rp.r], [PS[3].r, t2.r])
                    tt("dve", kr.t[R, :], t1.t[R, :], t2.t[R, :], ALU.add, [t1.r, t2.r], [kr.r])
                    rms_stats(sq, ckv.t[:], 2, TT, KVL, NORM_EPS, rstd, [ckv.r], PS[7])
                    for c in range(2):
                        stt("dve", ckvn.t[:, c, :], ckv.t[:, c, :], vK(c), rstd.t[:, :], ALU.mult, ALU.mult,
                            [ckv.r, rstd.r, vt.r], [ckvn.r])
                    for h in range(H):
                        K = KT[h % 2]
                        bk = PS[h % 2]
                        for kc in range(2):
                            mm(bk.t[0:64, :], kuk.t[:, kc, h * 64:(h + 1) * 64], ckvn.t[:, kc, :], kc == 0, kc == 1,
                               [kuk.r, ckvn.r], [bk.r])
                        cp("act", K.t[0:64, :], bk.t[0:64, :], [], [bk.r, K.r])
                        cp("pool", K.t[R, :], kr.t[R, :], [kr.r], [K.r])
                        P.dma(kscr[h, :, t0:t0 + TT], K.t[0:96, :], reads=[K.r], writes=[Rkv[it]])
                    for tb in range(TT // 128):
                        V = Vt[tb % 2]
                        for hf in range(2):
                            bk = PS[4 + hf]
                            for kc in range(2):
                                mm(bk.t[:, :], ckvn.t[:, kc, tb * 128:(tb + 1) * 128], kuv.t[:, kc, hf * 512:(hf + 1) * 512],
                                   kc == 0, kc == 1, [kuv.r, ckvn.r], [bk.r])
                            cp("act" if hf == 0 else "dve", V.t[:, hf * 8:(hf + 1) * 8, 0:64],
                               bk.t[:, :].rearrange("p (h d) -> p h d", d=64), [], [bk.r, V.r])
                        P.dma(vscr[:, t0 + tb * 128:t0 + (tb + 1) * 128, :].rearrange("h t d -> t h d"), V.t[:],
                              reads=[V.r], writes=[Rkv[it]])
                P.barrier()

        def mla_layer(j, layer, src, dst):
            TT = 512
            with ExitStack() as st:
                qd = sbt(st, "qd", [128, 8, QL], BF16)
                qu = sbt(st, "qu", [128, 3, H * 96], BF16)
                qus = sbt(st, "qus", [128, 3, H * 32], BF16)
                ow = sbt(st, "ow", [64, H, D], BF16)
                load_wk(qd, qd_d[j], 8, QL)
                load_wk(qu, qu_d[j], 3, H * 96)
                load_wk(qus, qus_d[j], 3, H * 32)
                for h in range(H):
                    load_w(lambda r, c0, cw, h=h: ow.t[0:r, h, c0:c0 + cw], ow.r, ow_d[j, h * 64:(h + 1) * 64, :], 64, D)
                xt = sbt(st, "xt", [128, 8, TT], F32)
                xn = sbt(st, "xn", [128, 8, TT], BF16)
                sq = sbt(st, "sq", [128, 8, TT], BF16)
                rstd = sbt(st, "rstd", [128, TT], F32)
                cq = sbt(st, "cq", [128, 3, TT], F32)
                cqn = sbt(st, "cqn", [128, 3, TT], BF16)
                rp = sbt(st, "rp", [128, 2, TT], F32)
                t1 = sbt(st, "t1", [128, TT], F32)
                t2 = sbt(st, "t2", [128, TT], F32)
                QT = sbt(st, "QT", [128, H, TT], BF16)
                OT = sbt(st, "OT", [64, H, TT], BF16)
                KT = [sbt(st, "KT%d" % k, [128, T], BF16) for k in range(2)]
                VT = [sbt(st, "VT%d" % k, [128, T // 128, 128], BF16) for k in range(2)]
                PT = [sbt(st, "PT%d" % k, [128, TT], BF16) for k in range(3)]
                rec = sbt(st, "rec", [64, TT], F32)
                msb = sbt(st, "msb", [128, 8, TT], F32)
                R = slice(64, 96)
                pi = 0
                for it in range(T // TT):
                    t0 = it * TT
                    P.dma(xt.t[:], xtile(src, t0, TT), reads=rx(t0, TT), writes=[xt.r])
                    P.dma(rp.t[R, :, :], rope_d[R, :, t0:t0 + TT], reads=[Rin], writes=[rp.r])
                    rms_stats(sq, xt.t[:], 8, TT, D, NORM_EPS, rstd, [xt.r], PS[2])
                    for c in range(8):
                        stt(ew(), xn.t[:, c, :], xt.t[:, c, :], vD("ng%d_0" % layer, c), rstd.t[:, :], ALU.mult, ALU.mult,
                            [xt.r, rstd.r, vt.r], [xn.r])
                    for m in range(3):
                        bk = PS[m % 2]
                        for kc in range(8):
                            mm(bk.t[:, :], qd.t[:, kc, m * 128:(m + 1) * 128], xn.t[:, kc, :], kc == 0, kc == 7,
                               [qd.r, xn.r], [bk.r])
                        cp("act", cq.t[:, m, :], bk.t[:, :], [], [bk.r, cq.r])
                    rms_stats(sq, cq.t[:], 3, TT, QL, NORM_EPS, rstd, [cq.r], PS[2])
                    for c in range(3):
                        stt("dve", cqn.t[:, c, :], cq.t[:, c, :], vQ(j, c), rstd.t[:, :], ALU.mult, ALU.mult,
                            [cq.r, rstd.r, vt.r], [cqn.r])
                    for h in range(H):
                        A = PS[h % 2]
                        Bk = PS[2]
                        for kc in range(3):
                            mm(A.t[0:96, :], qu.t[:, kc, h * 96:(h + 1) * 96], cqn.t[:, kc, :], kc == 0, kc == 2,
                               [qu.r, cqn.r], [A.r])
                        for kc in range(3):
                            mm(Bk.t[R, :], qus.t[:, kc, h * 32:(h + 1) * 32], cqn.t[:, kc, :], kc == 0, kc == 2,
                               [qus.r, cqn.r], [Bk.r])
                        act(QT.t[0:64, h, :], A.t[0:64, :], AF.Copy, [], [A.r, QT.r], scale=SCALE)
                        stt("dve", t1.t[R, :], A.t[R, :], SCALE, rp.t[R, 0, :], ALU.mult, ALU.mult, [rp.r], [A.r, t1.r])
                        stt("dve", t2.t[R, :], Bk.t[R, :], SCALE, rp.t[R, 1, :], ALU.mult, ALU.mult, [rp.r], [Bk.r, t2.r])
                        tt("dve", QT.t[R, h, :], t1.t[R, :], t2.t[R, :], ALU.add, [t1.r, t2.r], [QT.r])
                    nkb = (it + 1) * 4
                    nk = nkb * 128
                    for h in range(H):
                        K = KT[h % 2]
                        V = VT[h % 2]
                        P.dma(K.t[0:96, 0:nk], kscr[h, :, 0:nk], reads=Rkv[0:it + 1], writes=[K.r])
                        P.dma(V.t[:, 0:nkb, :], vscr[h, 0:nk, :].rearrange("(kb p) d -> p kb d", p=128),
                              reads=Rkv[0:it + 1], writes=[V.r])
                        Ob = PS[6 + h % 2]
                        pts = {}

                        def qk(kb, h=h, K=K):
                            nonlocal pi
                            jd = kb - it * 4
                            c0 = max(jd, 0) * 128
                            Sb_ = PS[3 + pi % 3]
                            Pt = PT[pi % 3]
                            pi += 1
                            mm(Sb_.t[:, c0:TT], K.t[0:96, kb * 128:(kb + 1) * 128], QT.t[0:96, h, c0:TT], True, True,
                               [K.r, QT.r], [Sb_.r])
                            act(Pt.t[:, c0:TT], Sb_.t[:, c0:TT], AF.Exp, [], [Sb_.r, Pt.r])
                            if jd >= 0:
                                P.op("pool", lambda e, Pt=Pt, c0=c0: e.memset(Pt.t[64:128, c0:c0 + 64], 0.0), [], [Pt.r])
                            pts[kb] = (Pt, c0)

                        def pv(kb, V=V, Ob=Ob):
                            Pt, c0 = pts.pop(kb)
                            mm(Ob.t[:, c0:TT], V.t[:, kb, :], Pt.t[:, c0:TT], kb == 0, kb == nkb - 1, [V.r, Pt.r], [Ob.r])

                        LA = 2
                        for kb in range(min(LA, nkb)):
                            qk(kb)
                        for kb in range(nkb):
                            if kb + LA < nkb:
                                qk(kb + LA)
                            pv(kb)
                        P.op("dve", lambda e, Ob=Ob: e.reciprocal(rec.t[:, :], Ob.t[64:128, :]), [], [Ob.r, rec.r])
                        tt("dve", OT.t[:, h, :], Ob.t[0:64, :], rec.t[:, :], ALU.mult, [rec.r], [Ob.r, OT.r])
                    for m in range(8):
                        bk = PS[m % 2]
                        for h in range(H):
                            mm(bk.t[:, :], ow.t[0:64, h, m * 128:(m + 1) * 128], OT.t[0:64, h, :], h == 0, h == H - 1,
                               [ow.r, OT.r], [bk.r])
                        cp("act", msb.t[:, m, :], bk.t[:, :], [], [bk.r, msb.r])
                    rms_stats(sq, msb.t[:], 8, TT, D, NORM_EPS, rstd, [msb.r], PS[2])
                    for c in range(8):
                        stt("dve", msb.t[:, c, :], msb.t[:, c, :], vD("ng%d_1" % layer, c), rstd.t[:, :], ALU.mult, ALU.mult,
                            [rstd.r, vt.r], [msb.r])
                    tt("pool", msb.t[:], msb.t[:], xt.t[:], ALU.add, [xt.r], [msb.r])
                    P.dma(xtile(dst, t0, TT), msb.t[:], reads=[msb.r], writes=rx(t0, TT))
                P.barrier()

        P.barrier()
        cur = xT
        for layer in range(depth):
            if layer < NA:
                rwkv_layer(layer, layer, cur, xs)
            else:
                if layer == NA:
                    kv_prep(xs)
                mla_layer(layer - NA, layer, xs, xs)
            cur = xs
            ffn_layer(layer, xs, y if layer == depth - 1 else xs)
        P.barrier()
        P.emit()
        nc._n_ops = P.n
    return nc


NVT = [0, 0, 0]
_CACHE = {}


def prep_inputs(inputs):
    inp = {k: np.asarray(v) for k, v in inputs.items()}
    B, T, _ = inp["x"].shape
    vt, nd, nf = _pack_tables(inp)
    NVT[0], NVT[1], NVT[2] = vt.shape[1], nd, nf
    f32 = lambda a: np.ascontiguousarray(a, dtype=np.float32)
    kd = inp["kv_w_down"]
    kds = np.concatenate([kd[:, 272:288], kd[:, 256:272]], 1)
    ku = inp["kv_w_up"].reshape(KVL, H, 128)
    qu = inp["q_w_up"].reshape(2, QL, H, 96)
    qus = np.concatenate([qu[..., 80:96], qu[..., 64:80]], -1).reshape(2, QL, H * 32)
    shared = {
        "vt": vt, "cst": _consts(), "rope": _rope(T),
        "ffn_w_in": f32(inp["ffn_w_in"]), "ffn_w_out": f32(inp["ffn_w_out"]),
        "a_w_rkv": f32(inp["a_w_rkv"]), "a_w1": f32(inp["a_w1"]), "a_w2": f32(inp["a_w2"]),
        "a_a1": f32(inp["a_a1"]), "a_a2": f32(inp["a_a2"]), "a_g1": f32(inp["a_g1"]), "a_g2": f32(inp["a_g2"]),
        "a_w_o": f32(inp["a_w_o"]), "kv_w_down": f32(kd), "kv_w_down_sw": f32(kds),
        "kv_w_up_k": f32(ku[:, :, 0:64].reshape(KVL, H * 64)), "kv_w_up_v": f32(ku[:, :, 64:128].reshape(KVL, H * 64)),
        "q_w_down": f32(inp["q_w_down"]), "q_w_up": f32(inp["q_w_up"]), "q_w_up_sw": f32(qus),
        "o_w": f32(inp["o_w"]),
    }
    maps = []
    for b in range(B):
        m = dict(shared)
        m["xT"] = f32(inp["x"][b].T)
        maps.append(m)
    return maps, B, T


def kernel(**inputs):
    maps, B, T = prep_inputs(inputs)
    key = (T, DEPTH)
    if key not in _CACHE:
        _CACHE[key] = build(T)
    nc = _CACHE[key]
    res = run_bass_kernel_spmd(nc, maps, core_ids=list(range(B)))
    out = np.stack([np.asarray(r["y"]).T for r in res.results], 0)
    return np.ascontiguousarray(out.astype(np.float32))
```

```python
import math
from contextlib import ExitStack
import numpy as np
import concourse.bass as bass
import concourse.mybir as mybir
from concourse.bass_utils import run_bass_kernel_spmd

F32 = mybir.dt.float32
BF16 = mybir.dt.bfloat16
AF = mybir.ActivationFunctionType
ALU = mybir.AluOpType

D = 1024
DEPTH = 4
NA = 2
H = 16
FF = 2816
NF = FF // 128
QL = 384
KVL = 256
LNX_EPS = 64e-5
NORM_EPS = 1e-6
SCALE = 1.0 / math.sqrt(96.0)

COMPUTE = ("pe", "act", "dve", "pool")
SEM_LIMIT = 30000
NDMA_SEM = 24


class Res:
    __slots__ = ("name", "w", "r")

    def __init__(self, name=""):
        self.name = name
        self.w = None
        self.r = {}


class Tl:
    __slots__ = ("t", "r")

    def __init__(self, t, r):
        self.t = t
        self.r = r


class Prog:
    def __init__(self, nc, stack):
        self.nc = nc
        self.stack = stack
        self.streams = {e: [] for e in COMPUTE + ("sp",)}
        self.sems = {}
        self.cnt = {}
        for e in COMPUTE:
            self.sems[e] = [self._newsem(e + "0")]
            self.cnt[e] = (0, 0)
        self.dma_sems = [self._newsem("dma%d" % i) for i in range(NDMA_SEM)]
        self.dma_cnt = [0] * NDMA_SEM
        self.dma_i = 0
        self.seen = {e: {} for e in self.streams}
        self.n = 0

    def _newsem(self, name):
        return self.stack.enter_context(self.nc.semaphore(name))

    def _semof(self, key, idx):
        if isinstance(key, tuple):
            return self.dma_sems[key[1]]
        return self.sems[key][idx]

    @staticmethod
    def _need(waits, tok):
        if tok is None:
            return
        key, idx, val = tok
        cur = waits.get(key)
        if cur is None or (idx, val) > cur:
            waits[key] = (idx, val)

    def _collect(self, q, reads, writes, eng):
        waits = {}
        for r in reads:
            self._need(waits, r.w)
        for w in writes:
            self._need(waits, w.w)
            for k, (i, v) in w.r.items():
                if k == eng:
                    continue
                self._need(waits, (k, i, v))
        if eng == "pe":
            waits.pop("pe", None)
        out = []
        for key, (idx, val) in waits.items():
            s = self.seen[q].get(key)
            if s is not None and s >= (idx, val):
                continue
            self.seen[q][key] = (idx, val)
            out.append((self._semof(key, idx), val))
        return out

    def op(self, eng, fn, reads=(), writes=()):
        waits = self._collect(eng, reads, writes, eng)
        idx, val = self.cnt[eng]
        if val >= SEM_LIMIT:
            idx += 1
            val = 0
            self.sems[eng].append(self._newsem("%s%d" % (eng, idx)))
        val += 1
        self.cnt[eng] = (idx, val)
        self.streams[eng].append((waits, fn, (self.sems[eng][idx], 1)))
        tok = (eng, idx, val)
        for r in reads:
            r.r[eng] = (idx, val)
        for w in writes:
            w.w = tok
            w.r = {}
        self.n += 1
        return tok

    def dma(self, out, in_, reads=(), writes=(), q="sp"):
        waits = self._collect(q, reads, writes, None)
        j = self.dma_i % NDMA_SEM
        self.dma_i += 1
        self.dma_cnt[j] += 16
        val = self.dma_cnt[j]
        key = ("dma", j)
        self.streams[q].append(
            (waits, lambda e: e.dma_start(out=out, in_=in_), (self.dma_sems[j], 16)))
        tok = (key, 0, val)
        for r in reads:
            r.r[key] = (0, val)
        for w in writes:
            w.w = tok
            w.r = {}
        self.n += 1
        return tok

    def barrier(self):
        for q in self.streams:
            waits = []
            for e in COMPUTE:
                if e == q:
                    continue
                idx, val = self.cnt[e]
                if val == 0:
                    continue
                sn = self.seen[q].get(e)
                if sn is not None and sn >= (idx, val):
                    continue
                self.seen[q][e] = (idx, val)
                waits.append((self.sems[e][idx], val))
            for j in range(NDMA_SEM):
                val = self.dma_cnt[j]
                if val == 0:
                    continue
                key = ("dma", j)
                sn = self.seen[q].get(key)
                if sn is not None and sn >= (0, val):
                    continue
                self.seen[q][key] = (0, val)
                waits.append((self.dma_sems[j], val))
            self.streams[q].append((waits, None, None))

    def emit(self):
        nc = self.nc
        with nc.Block() as block:
            def run(stream):
                def body(e):
                    for waits, fn, inc in stream:
                        for s, v in waits:
                            e.wait_ge(s, v)
                        if fn is not None:
                            ins = fn(e)
                            if inc is not None:
                                ins.then_inc(inc[0], inc[1])
                return body
            block.tensor(run(self.streams["pe"]))
            block.scalar(run(self.streams["act"]))
            block.vector(run(self.streams["dve"]))
            block.gpsimd(run(self.streams["pool"]))
            block.sync(run(self.streams["sp"]))


VD = {}


def _pack_tables(inp):
    vecs = []

    def add(name, v):
        VD[name] = len(vecs)
        vecs.append(np.asarray(v, np.float32).reshape(D))

    for l in range(DEPTH):
        for j in range(4):
            add("ng%d_%d" % (l, j), inp["norm_g"][l, j])
    for i in range(NA):
        for n in range(6):
            add("mu%d_%d" % (i, n), inp["a_mu"][i, n])
        for nm in ("w0", "a0", "k_k", "k_a", "r_k", "lnx_w", "lnx_b"):
            add("%s%d" % (nm, i), inp["a_" + nm][i])
    add("kvng", inp["kv_norm_g"])
    tabD = np.stack(vecs, 0).reshape(len(vecs), 8, 128).transpose(2, 0, 1).reshape(128, -1)
    fv = []
    for l in range(DEPTH):
        for j in range(3):
            fv.append(inp["ffn_conv_w"][l, j])
        fv.append(inp["ffn_conv_b"][l])
    tabF = np.stack(fv, 0).reshape(len(fv), NF, 128).transpose(2, 0, 1).reshape(128, -1)
    qg = np.asarray(inp["q_norm_g"]).reshape(2, 3, 128).transpose(2, 0, 1).reshape(128, 6)
    kg = np.asarray(inp["kv_a_norm_g"]).reshape(2, 128).T
    vt = np.ascontiguousarray(np.concatenate([tabD, tabF, qg, kg], 1).astype(np.float32))
    return vt, tabD.shape[1], tabF.shape[1]


def _consts():
    p = np.arange(128)
    ident = np.eye(128, dtype=np.float32)
    bd = (p[:, None] // 64 == p[None, :] // 64).astype(np.float32)
    su = (p[:, None] < p[None, :]).astype(np.float32)
    ui = (p[:, None] <= p[None, :]).astype(np.float32)
    low = (p[None, :] < p[:, None]).astype(np.float32)
    ones = np.ones((128, 128), np.float32)
    return np.ascontiguousarray(np.concatenate([ident, bd, su, ui, su, ui, low, ones], 1))


C_ID, C_BD, C_M4, C_LOW, C_ONE = 0, 128, 256, 768, 896
NCST = 1024


def _rope(T):
    inv = 1.0 / (10000.0 ** (np.arange(0, 32, 2, dtype=np.float32) / 32.0))
    ang = np.arange(T, dtype=np.float32)[:, None] * inv[None, :].astype(np.float32)
    cos = np.cos(ang).astype(np.float32).T
    sin = np.sin(ang).astype(np.float32).T
    tab = np.zeros((128, 2, T), np.float32)
    tab[64:80, 0] = cos
    tab[80:96, 0] = cos
    tab[64:80, 1] = -sin
    tab[80:96, 1] = sin
    return tab


def build(T, depth=DEPTH, dbg=False):
    nc = bass.Bass("TRN2", target_bir_lowering=False)
    NT5 = T // 512
    NT2 = T // 256

    def din(name, shape):
        return nc.dram_tensor(name, list(shape), F32, kind="ExternalInput").ap()

    xT = din("xT", [D, T])
    vt_d = din("vt", [128, NVT[0]])
    cst_d = din("cst", [128, NCST])
    rope_d = din("rope", [128, 2, T])
    w_in_d = din("ffn_w_in", [DEPTH, D, 2 * FF])
    w_out_d = din("ffn_w_out", [DEPTH, FF, D])
    wrkv_d = din("a_w_rkv", [NA, 3, D, D])
    w1_d = din("a_w1", [NA, D, 64])
    w2_d = din("a_w2", [NA, 64, D])
    a1_d = din("a_a1", [NA, D, 64])
    a2_d = din("a_a2", [NA, 64, D])
    g1_d = din("a_g1", [NA, D, 160])
    g2_d = din("a_g2", [NA, 160, D])
    wo_d = din("a_w_o", [NA, D, D])
    kd_d = din("kv_w_down", [D, 288])
    kds_d = din("kv_w_down_sw", [D, 32])
    kuk_d = din("kv_w_up_k", [KVL, H * 64])
    kuv_d = din("kv_w_up_v", [KVL, H * 64])
    qd_d = din("q_w_down", [2, D, QL])
    qu_d = din("q_w_up", [2, QL, H * 96])
    qus_d = din("q_w_up_sw", [2, QL, H * 32])
    ow_d = din("o_w", [2, D, D])
    y = nc.dram_tensor("y", [D, T], F32, kind="ExternalOutput").ap()
    xs = nc.dram_tensor("xs", [D, T], F32).ap()
    hscr = nc.dram_tensor("hscr", [FF, T], BF16).ap()
    kscr = nc.dram_tensor("kscr", [H, 96, T], BF16).ap()
    vscr = nc.dram_tensor("vscr", [H, T, 128], BF16).ap()

    Rx = [Res("x%d" % i) for i in range(NT2)]
    Rh = [Res("h%d" % i) for i in range(NT5)]
    Rkv = [Res("kv%d" % i) for i in range(NT5)]
    Rin = Res("in")

    with ExitStack() as top:
        P = Prog(nc, top)
        top.enter_context(nc.allow_low_precision("bf16 matmul operands, fp32 accumulate"))

        uid = [0]

        def sbt(st, name, shape, dt):
            uid[0] += 1
            nm = "sb%d_%s" % (uid[0], name)
            return Tl(st.enter_context(nc.sbuf_tensor(nm, list(shape), dt)), Res(nm))

        PS = [Tl(top.enter_context(nc.psum_tensor("ps%d" % i, [128, 512], F32)), Res("ps%d" % i))
              for i in range(8)]
        vt = sbt(top, "vt", [128, NVT[0]], F32)
        cst = sbt(top, "cst", [128, NCST], F32)
        onesb = sbt(top, "onesb", [128, 128], BF16)
        stg = [sbt(top, "stg%d" % i, [128, 1024], F32) for i in range(2)]
        stg_i = [0]
        P.dma(vt.t[:], vt_d, writes=[vt.r])
        P.dma(cst.t[:], cst_d, writes=[cst.r])
        P.op("dve", lambda e: e.tensor_copy(onesb.t[:], cst.t[:, C_ONE:C_ONE + 128]), [cst.r], [onesb.r])
        identb_t = sbt(top, "identb", [128, 128], BF16)
        P.op("dve", lambda e: e.tensor_copy(identb_t.t[:], cst.t[:, C_ID:C_ID + 128]), [cst.r], [identb_t.r])
        identb = identb_t.t[:, :]
        bdb_t = sbt(top, "bdb", [128, 128], BF16)
        P.op("dve", lambda e: e.tensor_copy(bdb_t.t[:], cst.t[:, C_BD:C_BD + 128]), [cst.r], [bdb_t.r])
        bdb = bdb_t.t[:, :]

        ident = cst.t[:, C_ID:C_ID + 128]
        bdones = cst.t[:, C_BD:C_BD + 128]
        mask4 = cst.t[:, C_M4:C_M4 + 512]
        lowm = cst.t[:, C_LOW:C_LOW + 128]
        ones = cst.t[:, C_ONE:C_ONE + 128]

        def vD(name, c):
            i = VD[name] * 8 + c
            return vt.t[:, i:i + 1]

        def vF(l, j, f):
            i = NVT[1] + (l * 4 + j) * NF + f
            return vt.t[:, i:i + 1]

        def vQ(j, c):
            i = NVT[1] + NVT[2] + j * 3 + c
            return vt.t[:, i:i + 1]

        def vK(c):
            i = NVT[1] + NVT[2] + 6 + c
            return vt.t[:, i:i + 1]

        def mm(out, lhsT, rhs, start, stop, reads, writes):
            P.op("pe", lambda e: e.matmul(out, lhsT, rhs, start=start, stop=stop), reads, writes)

        def act(out, in_, func, reads, writes, bias=None, scale=None):
            kw = {}
            if bias is not None:
                kw["bias"] = bias
            if scale is not None:
                kw["scale"] = scale
            P.op("act", lambda e: e.activation(out, in_, func, **kw), reads, writes)

        def tt(eng, out, in0, in1, op, reads, writes):
            P.op(eng, lambda e: e.tensor_tensor(out, in0, in1, op), reads, writes)

        def ts(eng, out, in0, s1, s2, op0, op1, reads, writes):
            if s2 is None:
                P.op(eng, lambda e: e.tensor_scalar(out, in0, s1, None, op0), reads, writes)
            else:
                P.op(eng, lambda e: e.tensor_scalar(out, in0, s1, s2, op0, op1), reads, writes)

        def stt(eng, out, in0, sc, in1, op0, op1, reads, writes):
            P.op("dve", lambda e: e.scalar_tensor_tensor(out, in0, sc, in1, op0, op1), reads, writes)

        def cp(eng, out, in_, reads, writes):
            if eng == "act":
                act(out, in_, AF.Copy, reads, writes)
            else:
                P.op(eng, lambda e: e.tensor_copy(out, in_), reads, writes)

        rr = [0]

        def ew():
            rr[0] += 1
            return "pool" if rr[0] % 3 == 0 else "dve"

        def load_w(view, res, src, rows, cols):
            c0 = 0
            while c0 < cols:
                cw = min(1024, cols - c0)
                s = stg[stg_i[0] % len(stg)]
                stg_i[0] += 1
                P.dma(s.t[0:rows, 0:cw], src[:, c0:c0 + cw], reads=[Rin], writes=[s.r])
                eng = "pool" if stg_i[0] % 2 == 0 else "dve"
                cp(eng, view(rows, c0, cw), s.t[0:rows, 0:cw], [s.r], [res])
                c0 += cw

        def load_wk(tile, src, nk, cols, rows_last=128):
            for k in range(nk):
                rows = rows_last if k == nk - 1 else 128
                load_w(lambda r, c0, cw, k=k: tile.t[0:r, k, c0:c0 + cw], tile.r,
                       src[k * 128:k * 128 + rows, :], rows, cols)

        def rms_stats(st_sq, src_ap, nch, TT, Dn, eps, rstd, src_reads, bank):
            act(st_sq.t[:, 0:nch, 0:TT], src_ap, AF.Square, src_reads, [st_sq.r])
            for c in range(nch):
                mm(bank.t[:, 0:TT], onesb.t[:, :], st_sq.t[:, c, 0:TT], c == 0, c == nch - 1,
                   [st_sq.r, onesb.r], [bank.r])
            act(rstd.t[:, 0:TT], bank.t[:, 0:TT], AF.Sqrt, [], [bank.r, rstd.r], bias=eps, scale=1.0 / Dn)
            P.op("dve", lambda e: e.reciprocal(rstd.t[:, 0:TT], rstd.t[:, 0:TT]), [], [rstd.r])

        def xtile(ap, t0, TT):
            return ap.rearrange("(c p) t -> p c t", p=128)[:, :, t0:t0 + TT]

        def rx(t0, TT):
            return Rx[t0 // 256:(t0 + TT) // 256]

        def run_jobs(gens, width, stagger):
            active = []
            it = iter(gens)
            steps0 = 0
            done = False
            while True:
                while not done and len(active) < width and (len(active) == 0 or steps0 >= stagger):
                    g = next(it, None)
                    if g is None:
                        done = True
                        break
                    active.append(g)
                    if len(active) == 1:
                        steps0 = 0
                if not active:
                    break
                for g in list(active):
                    try:
                        next(g)
                    except StopIteration:
                        active.remove(g)
                        steps0 = stagger
                steps0 += 1

        def rwkv_layer(i, layer, src, dst):
            TT = 256
            with ExitStack() as st:
                wr = sbt(st, "wr", [128, 8, D], BF16)
                wk = sbt(st, "wk", [128, 8, D], BF16)
                wv = sbt(st, "wv", [128, 8, D], BF16)
                wo = sbt(st, "wo", [128, 8, D], BF16)
                w1 = sbt(st, "w1", [128, 8, 64], BF16)
                a1 = sbt(st, "a1", [128, 8, 64], BF16)
                g1 = sbt(st, "g1", [128, 8, 160], BF16)
                w2 = sbt(st, "w2", [128, 1, D], BF16)
                a2 = sbt(st, "a2", [128, 1, D], BF16)
                g2 = sbt(st, "g2", [128, 2, D], BF16)
                for tl, srcw in ((wr, wrkv_d[i, 0]), (wk, wrkv_d[i, 1]), (wv, wrkv_d[i, 2]), (wo, wo_d[i])):
                    load_wk(tl, srcw, 8, D)
                load_wk(w1, w1_d[i], 8, 64)
                load_wk(a1, a1_d[i], 8, 64)
                load_wk(g1, g1_d[i], 8, 160)
                load_wk(w2, w2_d[i], 1, D, rows_last=64)
                load_wk(a2, a2_d[i], 1, D, rows_last=64)
                load_wk(g2, g2_d[i], 2, D, rows_last=32)

                xt = sbt(st, "xt", [128, 8, TT], F32)
                hb = sbt(st, "hb", [128, 8, TT + 1], F32)
                xsn = [sbt(st, "xs%d" % n, [128, 8, TT], BF16) for n in range(6)]
                rstd = sbt(st, "rstd", [128, TT], F32)
                tw = sbt(st, "tw", [64, TT], BF16)
                ta = sbt(st, "ta", [64, TT], BF16)
                tg = sbt(st, "tg", [128, 2, TT], BF16)
                z = sbt(st, "z", [128, 8, TT], BF16)
                sq = z
                msb = sbt(st, "msb", [128, 8, TT], F32)
                xx = msb
                S = sbt(st, "S", [128, 8, 64], F32)
                Sb = sbt(st, "Sb", [128, 8, 64], BF16)
                names = "r k v kk a lw g rn kmod b cum cex ecum bonus d".split()
                sets = []
                for k in range(2):
                    J = dict(
                        cb={nm: sbt(st, "c%d_%s" % (k, nm), [128, TT], F32) for nm in names},
                        vb=sbt(st, "vb%d" % k, [128, TT], BF16),
                        sqb=sbt(st, "sqb%d" % k, [128, TT], BF16),
                        AR=sbt(st, "AR%d" % k, [128, 2 * TT], BF16),
                        KB=sbt(st, "KB%d" % k, [128, 2 * TT], BF16),
                        TM=sbt(st, "TM%d" % k, [128, 2, 3, 128], BF16),
                        bA=PS[4 * k], bB=PS[4 * k + 1], hd=[])
                    for hh in range(2):
                        J["hd"].append(dict(
                            AMs=sbt(st, "AMs%d_%d" % (k, hh), [128, 512], BF16),
                            Ls=sbt(st, "Ls%d_%d" % (k, hh), [128, 128], BF16),
                            LM=[sbt(st, "LM%d_%d_%d" % (k, hh, q), [128, 256], BF16) for q in range(2)],
                            Tt=[sbt(st, "Tt%d_%d_%d" % (k, hh, q), [128, 128], BF16) for q in range(2)],
                            Xs=sbt(st, "Xs%d_%d" % (k, hh), [128, 64], BF16),
                            Us=sbt(st, "Us%d_%d" % (k, hh), [128, 64], BF16),
                            tmpS=sbt(st, "tmpS%d_%d" % (k, hh), [128, 64], F32),
                            WK=PS[4 * k + 2 + hh]))
                    sets.append(J)
                SR = [Res("S%d" % c) for c in range(8)]
                SbR = [Res("Sb%d" % c) for c in range(8)]
                P.op("pool", lambda e: e.memset(S.t[:], 0.0), [], SR)
                P.op("pool", lambda e: e.memset(Sb.t[:], 0.0), [], SbR)
                P.op("pool", lambda e: e.memset(hb.t[:, :, 0:1], 0.0), [], [hb.r])

                def job(c, J):
                    B = J["cb"]
                    vb, AR, KB, TM, bA, bB, hd = J["vb"], J["AR"], J["KB"], J["TM"], J["bA"], J["bB"], J["hd"]
                    sqb = J["sqb"]
                    cs = slice(c * 128, (c + 1) * 128)
                    for kc in range(8):
                        mm(bA.t[:, 0:TT], wr.t[:, kc, cs], xsn[0].t[:, kc, :], kc == 0, kc == 7, [wr.r, xsn[0].r], [bA.r])
                    for kc in range(8):
                        mm(bA.t[:, TT:2 * TT], wk.t[:, kc, cs], xsn[1].t[:, kc, :], kc == 0, kc == 7, [wk.r, xsn[1].r], [bA.r])
                    for kc in range(8):
                        mm(bB.t[:, 0:TT], wv.t[:, kc, cs], xsn[2].t[:, kc, :], kc == 0, kc == 7, [wv.r, xsn[2].r], [bB.r])
                    mm(bB.t[:, TT:2 * TT], w2.t[0:64, 0, cs], tw.t[:], True, True, [w2.r, tw.r], [bB.r])
                    yield
                    cp("act", B["r"].t[:], bA.t[:, 0:TT], [], [bA.r, B["r"].r])
                    ts("dve", B["kk"].t[:], bA.t[:, TT:2 * TT], vD("k_k%d" % i, c), None, ALU.mult, None, [vt.r], [bA.r, B["kk"].r])
                    cp("act", B["k"].t[:], bA.t[:, TT:2 * TT], [], [bA.r, B["k"].r])
                    yield
                    cp("dve", B["v"].t[:], bB.t[:, 0:TT], [], [bB.r, B["v"].r])
                    cp("act", vb.t[:], bB.t[:, 0:TT], [], [bB.r, vb.r])
                    act(B["lw"].t[:], bB.t[:, TT:2 * TT], AF.Sigmoid, [vt.r], [bB.r, B["lw"].r], bias=vD("w0%d" % i, c))
                    ts("pool", B["lw"].t[:], B["lw"].t[:], -math.exp(-0.5), None, ALU.mult, None, [], [B["lw"].r])
                    yield
                    mm(bA.t[:, 0:TT], a2.t[0:64, 0, cs], ta.t[:], True, True, [a2.r, ta.r], [bA.r])
                    mm(bA.t[:, TT:2 * TT], g2.t[:, 0, cs], tg.t[:, 0, :], True, False, [g2.r, tg.r], [bA.r])
                    mm(bA.t[:, TT:2 * TT], g2.t[0:32, 1, cs], tg.t[0:32, 1, :], False, True, [g2.r, tg.r], [bA.r])
                    act(B["a"].t[:], bA.t[:, 0:TT], AF.Sigmoid, [vt.r], [bA.r, B["a"].r], bias=vD("a0%d" % i, c))
                    cp("act", B["g"].t[:], bA.t[:, TT:2 * TT], [], [bA.r, B["g"].r])
                    yield
                    act(sqb.t[:], B["kk"].t[:], AF.Square, [B["kk"].r], [sqb.r])
                    mm(bB.t[:, 0:TT], bdb, sqb.t[:], True, True, [bdb_t.r, sqb.r], [bB.r])
                    act(B["rn"].t[:], bB.t[:, 0:TT], AF.Sqrt, [], [bB.r, B["rn"].r])
                    ts("dve", B["rn"].t[:], B["rn"].t[:], 1e-12, None, ALU.max, None, [], [B["rn"].r])
                    P.op("dve", lambda e: e.reciprocal(B["rn"].t[:], B["rn"].t[:]), [], [B["rn"].r])
                    tt("dve", B["kk"].t[:], B["kk"].t[:], B["rn"].t[:], ALU.mult, [B["rn"].r], [B["kk"].r])
                    yield
                    ts("pool", B["kmod"].t[:], B["a"].t[:], -1.0, vD("k_a%d" % i, c), ALU.add, ALU.mult, [B["a"].r, vt.r], [B["kmod"].r])
                    stt("dve", B["kmod"].t[:], B["kmod"].t[:], 1.0, B["k"].t[:], ALU.add, ALU.mult, [B["k"].r], [B["kmod"].r])
                    tt("pool", B["b"].t[:], B["kk"].t[:], B["a"].t[:], ALU.mult, [B["kk"].r, B["a"].r], [B["b"].r])
                    for ch in range(2):
                        sl = slice(ch * 128, (ch + 1) * 128)
                        P.op("dve", lambda e, sl=sl: e.tensor_tensor_scan(
                            B["cum"].t[:, sl], ones, B["lw"].t[:, sl], 0.0, ALU.mult, ALU.add), [cst.r, B["lw"].r], [B["cum"].r])
                    tt("pool", B["cex"].t[:], B["cum"].t[:], B["lw"].t[:], ALU.subtract, [B["cum"].r, B["lw"].r], [B["cex"].r])
                    act(B["ecum"].t[:], B["cum"].t[:], AF.Exp, [B["cum"].r], [B["ecum"].r])
                    act(B["cum"].t[:], B["cum"].t[:], AF.Exp, [], [B["cum"].r], scale=-1.0)
                    act(B["cex"].t[:], B["cex"].t[:], AF.Exp, [], [B["cex"].r])
                    yield
                    for ch in range(2):
                        sl = slice(ch * 128, (ch + 1) * 128)
                        o = ch * 256
                        stt("dve", AR.t[:, o:o + 128], B["kk"].t[:, sl], -1.0, B["cex"].t[:, sl], ALU.mult, ALU.mult,
                            [B["kk"].r, B["cex"].r], [AR.r])
                        tt("pool", AR.t[:, o + 128:o + 256], B["r"].t[:, sl], B["ecum"].t[:, sl], ALU.mult, [B["r"].r, B["ecum"].r], [AR.r])
                        tt("dve", KB.t[:, o:o + 128], B["kmod"].t[:, sl], B["cum"].t[:, sl], ALU.mult, [B["kmod"].r, B["cum"].r], [KB.r])
                        tt("pool", KB.t[:, o + 128:o + 256], B["b"].t[:, sl], B["cum"].t[:, sl], ALU.mult, [B["b"].r, B["cum"].r], [KB.r])
                    yield
                    stt("dve", sqb.t[:], B["r"].t[:], vD("r_k%d" % i, c), B["kmod"].t[:], ALU.mult, ALU.mult,
                        [B["r"].r, B["kmod"].r, vt.r], [sqb.r])
                    mm(bB.t[:, TT:2 * TT], bdb, sqb.t[:], True, True, [bdb_t.r, sqb.r], [bB.r])
                    tt("dve", B["bonus"].t[:], bB.t[:, TT:2 * TT], B["v"].t[:], ALU.mult, [B["v"].r], [bB.r, B["bonus"].r])
                    for ch in range(2):
                        sl = slice(ch * 128, (ch + 1) * 128)
                        o = ch * 256
                        mm(bA.t[:, ch * 128:ch * 128 + 128], vb.t[:, sl], identb, True, True, [vb.r, identb_t.r], [bA.r])
                        mm(bA.t[:, 256 + ch * 128:256 + ch * 128 + 128], KB.t[:, o:o + 128], identb, True, True, [KB.r, identb_t.r], [bA.r])
                        mm(bB.t[:, ch * 128:ch * 128 + 128], KB.t[:, o + 128:o + 256], identb, True, True, [KB.r, identb_t.r], [bB.r])
                    yield
                    for ch in range(2):
                        cp("act", TM.t[:, ch, 0, :], bA.t[:, ch * 128:ch * 128 + 128], [], [bA.r, TM.r])
                        cp("dve", TM.t[:, ch, 1, :], bA.t[:, 256 + ch * 128:256 + ch * 128 + 128], [], [bA.r, TM.r])
                        cp("act", TM.t[:, ch, 2, :], bB.t[:, ch * 128:ch * 128 + 128], [], [bB.r, TM.r])
                    yield
                    for ch in range(2):
                        o = ch * 256
                        for hh in range(2):
                            p = slice(hh * 64, hh * 64 + 64)
                            Hh = hd[hh]
                            WK = Hh["WK"]
                            mm(WK.t[:, 0:256], KB.t[p, o:o + 128], AR.t[p, o:o + 256], True, True, [KB.r, AR.r], [WK.r])
                            mm(WK.t[:, 256:512], KB.t[p, o + 128:o + 256], AR.t[p, o:o + 256], True, True, [KB.r, AR.r], [WK.r])
                            TB = bA if hh == 0 else bB
                            mm(TB.t[:, 128:256], AR.t[p, o:o + 128], KB.t[p, o + 128:o + 256], True, True, [KB.r, AR.r], [TB.r])
                        yield
                        for hh in range(2):
                            Hh = hd[hh]
                            WK = Hh["WK"]
                            TB = bA if hh == 0 else bB
                            tt("dve", Hh["AMs"].t[:], WK.t[:], mask4, ALU.mult, [cst.r], [WK.r, Hh["AMs"].r])
                            tt("dve", Hh["Ls"].t[:], TB.t[:, 128:256], lowm, ALU.mult, [cst.r], [TB.r, Hh["Ls"].r])
                            tt("pool", Hh["Tt"][0].t[:], Hh["AMs"].t[:, 256:384], identb, ALU.add, [Hh["AMs"].r, identb_t.r], [Hh["Tt"][0].r])
                        yield
                        for lv in range(1, 7):
                            for hh in range(2):
                                Hh = hd[hh]
                                WK = Hh["WK"]
                                if lv == 1:
                                    Lp, Mp, rd = Hh["Ls"].t[:], Hh["AMs"].t[:, 256:384], [Hh["Ls"].r, Hh["AMs"].r]
                                else:
                                    pl = Hh["LM"][(lv - 1) % 2]
                                    Lp, Mp, rd = pl.t[:, 0:128], pl.t[:, 128:256], [pl.r]
                                nl = Hh["LM"][lv % 2]
                                mm(WK.t[:, 128:256], Mp, Lp, True, True, rd, [WK.r])
                                if lv < 6:
                                    mm(WK.t[:, 256:384], Lp, Mp, True, True, rd, [WK.r])
                                    cp("act", nl.t[:, 0:256], WK.t[:, 128:384], [], [WK.r, nl.r])
                                else:
                                    cp("act", nl.t[:, 0:128], WK.t[:, 128:256], [], [WK.r, nl.r])
                            yield
                            for hh in range(2):
                                Hh = hd[hh]
                                WK = Hh["WK"]
                                nl = Hh["LM"][lv % 2]
                                Tc = Hh["Tt"][(lv - 1) % 2]
                                Tn = Hh["Tt"][lv % 2]
                                TB = bA if hh == 0 else bB
                                mm(TB.t[:, 0:128], nl.t[:, 0:128], Tc.t[:], True, True, [nl.r, Tc.r], [TB.r])
                                tt("dve", Tn.t[:], TB.t[:, 0:128], Tc.t[:], ALU.add, [Tc.r], [TB.r, Tn.r])
                            yield
                        for hh in range(2):
                            p = slice(hh * 64, hh * 64 + 64)
                            fs = slice(hh * 64, hh * 64 + 64)
                            Hh = hd[hh]
                            WK = Hh["WK"]
                            mm(WK.t[:, 0:64], AR.t[p, o:o + 128], Sb.t[p, c, :], True, False, [AR.r, SbR[c]], [WK.r])
                            mm(WK.t[:, 0:64], Hh["AMs"].t[:, 0:128], TM.t[:, ch, 0, fs], False, True, [Hh["AMs"].r, TM.r], [WK.r])
                            cp("act", Hh["Xs"].t[:], WK.t[:, 0:64], [], [WK.r, Hh["Xs"].r])
                        yield
                        for hh in range(2):
                            Hh = hd[hh]
                            WK = Hh["WK"]
                            Tf = Hh["Tt"][0]
                            mm(WK.t[:, 64:128], Tf.t[:], Hh["Xs"].t[:], True, True, [Tf.r, Hh["Xs"].r], [WK.r])
                            cp("act", Hh["Us"].t[:], WK.t[:, 64:128], [], [WK.r, Hh["Us"].r])
                        yield
                        for hh in range(2):
                            p = slice(hh * 64, hh * 64 + 64)
                            fs = slice(hh * 64, hh * 64 + 64)
                            Hh = hd[hh]
                            WK = Hh["WK"]
                            yo = WK.t[p, 128:256]
                            mm(yo, Sb.t[p, c, :], AR.t[p, o + 128:o + 256], True, False, [SbR[c], AR.r], [WK.r])
                            mm(yo, Hh["Us"].t[:], Hh["AMs"].t[:, 384:512], False, False, [Hh["Us"].r, Hh["AMs"].r], [WK.r])
                            mm(yo, TM.t[:, ch, 0, fs], Hh["AMs"].t[:, 128:256], False, True, [TM.r, Hh["AMs"].r], [WK.r])
                            so = WK.t[p, 256:320]
                            mm(so, TM.t[:, ch, 2, fs], Hh["Us"].t[:], True, False, [TM.r, Hh["Us"].r], [WK.r])
                            mm(so, TM.t[:, ch, 1, fs], TM.t[:, ch, 0, fs], False, True, [TM.r], [WK.r])
                        yield
                        for hh in range(2):
                            p = slice(hh * 64, hh * 64 + 64)
                            Hh = hd[hh]
                            WK = Hh["WK"]
                            yo = WK.t[p, 128:256]
                            so = WK.t[p, 256:320]
                            cp("act", B["d"].t[p, ch * 128:ch * 128 + 128], yo, [], [WK.r, B["d"].r])
                            tt("dve", Hh["tmpS"].t[p, :], so, S.t[p, c, :], ALU.add, [SR[c]], [WK.r, Hh["tmpS"].r])
                            wc = B["ecum"].t[p, ch * 128 + 127:ch * 128 + 128]
                            ts("dve", S.t[p, c, :], Hh["tmpS"].t[p, :], wc, None, ALU.mult, None, [Hh["tmpS"].r, B["ecum"].r], [SR[c]])
                            ts("pool", Sb.t[p, c, :], Hh["tmpS"].t[p, :], wc, None, ALU.mult, None, [Hh["tmpS"].r, B["ecum"].r], [SbR[c]])
                        yield
                    cp("pool", sqb.t[:], B["d"].t[:], [B["d"].r], [sqb.r])
                    mm(bB.t[:, 0:TT], bdb, sqb.t[:], True, True, [bdb_t.r, sqb.r], [bB.r])
                    stt("dve", B["d"].t[:], bB.t[:, 0:TT], -1.0 / 64, B["d"].t[:], ALU.mult, ALU.add, [], [bB.r, B["d"].r])
                    act(sqb.t[:], B["d"].t[:], AF.Square, [B["d"].r], [sqb.r])
                    mm(bB.t[:, TT:2 * TT], bdb, sqb.t[:], True, True, [bdb_t.r, sqb.r], [bB.r])
                    act(B["rn"].t[:], bB.t[:, TT:2 * TT], AF.Sqrt, [], [bB.r, B["rn"].r], bias=LNX_EPS, scale=1.0 / 64)
                    P.op("dve", lambda e: e.reciprocal(B["rn"].t[:], B["rn"].t[:]), [], [B["rn"].r])
                    yield
                    stt("dve", B["d"].t[:], B["d"].t[:], vD("lnx_w%d" % i, c), B["rn"].t[:], ALU.mult, ALU.mult, [B["rn"].r, vt.r], [B["d"].r])
                    stt("dve", B["d"].t[:], B["d"].t[:], vD("lnx_b%d" % i, c), B["bonus"].t[:], ALU.add, ALU.add, [B["bonus"].r, vt.r], [B["d"].r])
                    tt("pool", z.t[:, c, :], B["d"].t[:], B["g"].t[:], ALU.mult, [B["d"].r, B["g"].r], [z.r])
                    yield

                for it in range(T // TT):
                    t0 = it * TT
                    P.dma(xt.t[:], xtile(src, t0, TT), reads=rx(t0, TT), writes=[xt.r])
                    rms_stats(sq, xt.t[:], 8, TT, D, NORM_EPS, rstd, [xt.r], PS[3])
                    for c in range(8):
                        stt("dve", hb.t[:, c, 1:TT + 1], xt.t[:, c, :], vD("ng%d_0" % layer, c), rstd.t[:, :],
                            ALU.mult, ALU.mult, [xt.r, rstd.r, vt.r], [hb.r])
                    tt("pool", xx.t[:], hb.t[:, :, 0:TT], hb.t[:, :, 1:TT + 1], ALU.subtract, [hb.r], [xx.r])
                    for n in range(6):
                        for c in range(8):
                            stt("dve", xsn[n].t[:, c, :], xx.t[:, c, :], vD("mu%d_%d" % (i, n), c),
                                hb.t[:, c, 1:TT + 1], ALU.mult, ALU.add, [xx.r, hb.r, vt.r], [xsn[n].r])
                    for kc in range(8):
                        mm(PS[0].t[0:64, 0:TT], w1.t[:, kc, :], xsn[3].t[:, kc, :], kc == 0, kc == 7, [w1.r, xsn[3].r], [PS[0].r])
                    act(tw.t[:], PS[0].t[0:64, 0:TT], AF.Tanh, [], [PS[0].r, tw.r])
                    for kc in range(8):
                        mm(PS[1].t[0:64, 0:TT], a1.t[:, kc, :], xsn[4].t[:, kc, :], kc == 0, kc == 7, [a1.r, xsn[4].r], [PS[1].r])
                    cp("act", ta.t[:], PS[1].t[0:64, 0:TT], [], [PS[1].r, ta.r])
                    for kc in range(8):
                        mm(PS[2].t[:, 0:TT], g1.t[:, kc, 0:128], xsn[5].t[:, kc, :], kc == 0, kc == 7, [g1.r, xsn[5].r], [PS[2].r])
                    for kc in range(8):
                        mm(PS[2].t[0:32, TT:2 * TT], g1.t[:, kc, 128:160], xsn[5].t[:, kc, :], kc == 0, kc == 7, [g1.r, xsn[5].r], [PS[2].r])
                    act(tg.t[:, 0, :], PS[2].t[:, 0:TT], AF.Sigmoid, [], [PS[2].r, tg.r])
                    act(tg.t[0:32, 1, :], PS[2].t[0:32, TT:2 * TT], AF.Sigmoid, [], [PS[2].r, tg.r])

                    run_jobs((job(c, sets[c % 2]) for c in range(8)), 2, 20)

                    for m in range(8):
                        bk = PS[m % 4]
                        for kc in range(8):
                            mm(bk.t[:, 0:TT], wo.t[:, kc, m * 128:(m + 1) * 128], z.t[:, kc, :], kc == 0, kc == 7, [wo.r, z.r], [bk.r])
                        cp("act", msb.t[:, m, :], bk.t[:, 0:TT], [], [bk.r, msb.r])
                    rms_stats(sq, msb.t[:], 8, TT, D, NORM_EPS, rstd, [msb.r], PS[4])
                    for c in range(8):
                        stt("dve", msb.t[:, c, :], msb.t[:, c, :], vD("ng%d_1" % layer, c), rstd.t[:, :], ALU.mult, ALU.mult,
                            [rstd.r, vt.r], [msb.r])
                    tt("pool", msb.t[:], msb.t[:], xt.t[:], ALU.add, [xt.r], [msb.r])
                    P.dma(xtile(dst, t0, TT), msb.t[:], reads=[msb.r], writes=rx(t0, TT))
                    cp("pool", hb.t[:, :, 0:1], hb.t[:, :, TT:TT + 1], [], [hb.r])
                P.barrier()

        def ffn_layer(layer, src, dst):
            TT = 512
            with ExitStack() as st:
                win = sbt(st, "win", [128, 8, 2 * FF], BF16)
                stg.extend([sbt(st, "stgx%d" % k, [128, 1024], F32) for k in range(4)])
                load_wk(win, w_in_d[layer], 8, 2 * FF)
                del stg[2:]
                xts = [sbt(st, "xt%d" % k, [128, 8, TT], F32) for k in range(2)]
                xns = [sbt(st, "xn%d" % k, [128, 8, TT], BF16) for k in range(2)]
                sq = sbt(st, "sq", [128, 8, TT], BF16)
                rstd = sbt(st, "rstd", [128, TT], F32)
                carry = sbt(st, "carry", [128, NF, 2], F32)
                gbuf = [sbt(st, "gbuf%d" % k, [128, TT + 2], F32) for k in range(2)]
                acc = [sbt(st, "acc%d" % k, [128, TT], F32) for k in range(2)]
                gl = [sbt(st, "gl%d" % k, [128, TT], F32) for k in range(2)]
                hT = [sbt(st, "hT%d" % k, [128, TT], BF16) for k in range(2)]
                P.op("pool", lambda e: e.memset(carry.t[:], 0.0), [], [carry.r])

                def norm_in(it):
                    t0 = it * TT
                    xt, xn = xts[it % 2], xns[it % 2]
                    P.dma(xt.t[:], xtile(src, t0, TT), reads=rx(t0, TT), writes=[xt.r])
                    rms_stats(sq, xt.t[:], 8, TT, D, NORM_EPS, rstd, [xt.r], PS[7])
                    for c in range(8):
                        stt("dve", xn.t[:, c, :], xt.t[:, c, :], vD("ng%d_2" % layer, c), rstd.t[:, :], ALU.mult, ALU.mult,
                            [xt.r, rstd.r, vt.r], [xn.r])

                norm_in(0)
                for it in range(T // TT):
                    t0 = it * TT
                    xn = xns[it % 2]
                    for f in range(NF):
                        if f == 12 and it + 1 < T // TT:
                            norm_in(it + 1)
                        k2 = f % 2
                        gb, ub = PS[k2], PS[2 + k2]
                        for kc in range(8):
                            mm(gb.t[:, :], win.t[:, kc, f * 128:(f + 1) * 128], xn.t[:, kc, :], kc == 0, kc == 7,
                               [win.r, xn.r], [gb.r])
                        for kc in range(8):
                            mm(ub.t[:, :], win.t[:, kc, FF + f * 128:FF + (f + 1) * 128], xn.t[:, kc, :], kc == 0, kc == 7,
                               [win.r, xn.r], [ub.r])
                        G = gbuf[k2]
                        cp("pool", G.t[:, 0:2], carry.t[:, f, :], [carry.r], [G.r])
                        cp("act", G.t[:, 2:TT + 2], gb.t[:, :], [], [gb.r, G.r])
                        cp("pool", carry.t[:, f, :], G.t[:, TT:TT + 2], [G.r], [carry.r])
                        A = acc[k2]
                        ts("dve", A.t[:], G.t[:, 2:TT + 2], vF(layer, 2, f), vF(layer, 3, f), ALU.mult, ALU.add,
                           [G.r, vt.r], [A.r])
                        stt("dve", A.t[:], G.t[:, 1:TT + 1], vF(layer, 1, f), A.t[:], ALU.mult, ALU.add, [G.r, vt.r], [A.r])
                        stt("dve", A.t[:], G.t[:, 0:TT], vF(layer, 0, f), A.t[:], ALU.mult, ALU.add, [G.r, vt.r], [A.r])
                        act(gl[k2].t[:], A.t[:], AF.Gelu_apprx_tanh, [A.r], [gl[k2].r])
                        tt("dve", hT[k2].t[:], ub.t[:, :], gl[k2].t[:], ALU.mult, [gl[k2].r], [ub.r, hT[k2].r])
                        P.dma(hscr[f * 128:(f + 1) * 128, t0:t0 + TT], hT[k2].t[:], reads=[hT[k2].r], writes=[Rh[it]])
                P.barrier()
            with ExitStack() as st:
                wout = sbt(st, "wout", [128, NF, D], BF16)
                stg.extend([sbt(st, "stgy%d" % k, [128, 1024], F32) for k in range(4)])
                load_wk(wout, w_out_d[layer], NF, D)
                del stg[2:]
                xt = sbt(st, "xt", [128, 8, TT], F32)
                ht = [sbt(st, "ht%d" % k, [128, NF, TT], BF16) for k in range(2)]
                msb = sbt(st, "msb", [128, 8, TT], F32)
                sq = sbt(st, "sq", [128, 8, TT], BF16)
                rstd = sbt(st, "rstd", [128, TT], F32)
                for it in range(T // TT):
                    t0 = it * TT
                    hh = ht[it % 2]
                    P.dma(hh.t[:], hscr.rearrange("(f p) t -> p f t", p=128)[:, :, t0:t0 + TT], reads=[Rh[it]], writes=[hh.r])
                    P.dma(xt.t[:], xtile(src, t0, TT), reads=rx(t0, TT), writes=[xt.r])
                    for m in range(8):
                        bk = PS[m % 4]
                        for f in range(NF):
                            mm(bk.t[:, :], wout.t[:, f, m * 128:(m + 1) * 128], hh.t[:, f, :], f == 0, f == NF - 1,
                               [wout.r, hh.r], [bk.r])
                        cp("act", msb.t[:, m, :], bk.t[:, :], [], [bk.r, msb.r])
                    rms_stats(sq, msb.t[:], 8, TT, D, NORM_EPS, rstd, [msb.r], PS[7])
                    for c in range(8):
                        stt("dve", msb.t[:, c, :], msb.t[:, c, :], vD("ng%d_3" % layer, c), rstd.t[:, :], ALU.mult, ALU.mult,
                            [rstd.r, vt.r], [msb.r])
                    tt("pool", msb.t[:], msb.t[:], xt.t[:], ALU.add, [xt.r], [msb.r])
                    P.dma(xtile(dst, t0, TT), msb.t[:], reads=[msb.r], writes=rx(t0, TT))
                P.barrier()

        def kv_prep(src):
            TT = 512
            with ExitStack() as st:
                kd = sbt(st, "kd", [128, 8, 288], BF16)
                kds = sbt(st, "kds", [128, 8, 32], BF16)
                kuk = sbt(st, "kuk", [128, 2, H * 64], BF16)
                kuv = sbt(st, "kuv", [128, 2, H * 64], BF16)
                load_wk(kd, kd_d, 8, 288)
                load_wk(kds, kds_d, 8, 32)
                load_wk(kuk, kuk_d, 2, H * 64)
                load_wk(kuv, kuv_d, 2, H * 64)
                xt = sbt(st, "xt", [128, 8, TT], F32)
                xn = sbt(st, "xn", [128, 8, TT], BF16)
                sq = sbt(st, "sq", [128, 8, TT], BF16)
                rstd = sbt(st, "rstd", [128, TT], F32)
                ckv = sbt(st, "ckv", [128, 2, TT], F32)
                ckvn = sbt(st, "ckvn", [128, 2, TT], BF16)
                rp = sbt(st, "rp", [128, 2, TT], F32)
                t1 = sbt(st, "t1", [128, TT], F32)
                t2 = sbt(st, "t2", [128, TT], F32)
                kr = sbt(st, "kr", [128, TT], BF16)
                KT = [sbt(st, "KT%d" % k, [128, TT], BF16) for k in range(2)]
                Vt = [sbt(st, "Vt%d" % k, [128, H, 128], BF16) for k in range(2)]
                for k in range(2):
                    P.op("pool", lambda e, k=k: e.memset(Vt[k].t[:, :, 64:128], 1.0), [], [Vt[k].r])
                R = slice(64, 96)
                for it in range(T // TT):
                    t0 = it * TT
                    P.dma(xt.t[:], xtile(src, t0, TT), reads=rx(t0, TT), writes=[xt.r])
                    P.dma(rp.t[R, :, :], rope_d[R, :, t0:t0 + TT], reads=[Rin], writes=[rp.r])
                    rms_stats(sq, xt.t[:], 8, TT, D, NORM_EPS, rstd, [xt.r], PS[7])
                    for c in range(8):
                        stt(ew(), xn.t[:, c, :], xt.t[:, c, :], vD("kvng", c), rstd.t[:, :], ALU.mult, ALU.mult,
                            [xt.r, rstd.r, vt.r], [xn.r])
                    for j in range(2):
                        for kc in range(8):
                            mm(PS[j].t[:, :], kd.t[:, kc, j * 128:(j + 1) * 128], xn.t[:, kc, :], kc == 0, kc == 7,
                               [kd.r, xn.r], [PS[j].r])
                        cp("act", ckv.t[:, j, :], PS[j].t[:, :], [], [PS[j].r, ckv.r])
                    for kc in range(8):
                        mm(PS[2].t[R, :], kd.t[:, kc, 256:288], xn.t[:, kc, :], kc == 0, kc == 7, [kd.r, xn.r], [PS[2].r])
                    for kc in range(8):
                        mm(PS[3].t[R, :], kds.t[:, kc, :], xn.t[:, kc, :], kc == 0, kc == 7, [kds.r, xn.r], [PS[3].r])
                    tt("dve", t1.t[R, :], PS[2].t[R, :], rp.t[R, 0, :], ALU.mult, [rp.r], [PS[2].r, t1.r])
                    tt("dve", t2.t[R, :], PS[3].t[R, :], rp.t[R, 1, :], ALU.mult, [rp.r], [PS[3].r, t2.r])
                    tt("dve", kr.t[R, :], t1.t[R, :], t2.t[R, :], ALU.add, [t1.r, t2.r], [kr.r])
                    rms_stats(sq, ckv.t[:], 2, TT, KVL, NORM_EPS, rstd, [ckv.r], PS[7])
                    for c in range(2):
                        stt("dve", ckvn.t[:, c, :], ckv.t[:, c, :], vK(c), rstd.t[:, :], ALU.mult, ALU.mult,
                            [ckv.r, rstd.r, vt.r], [ckvn.r])
                    for h in range(H):
                        K = KT[h % 2]
                        bk = PS[h % 2]
                        for kc in range(2):
                            mm(bk.t[0:64, :], kuk.t[:, kc, h * 64:(h + 1) * 64], ckvn.t[:, kc, :], kc == 0, kc == 1,
                               [kuk.r, ckvn.r], [bk.r])
                        cp("act", K.t[0:64, :], bk.t[0:64, :], [], [bk.r, K.r])
                        cp("pool", K.t[R, :], kr.t[R, :], [kr.r], [K.r])
                        P.dma(kscr[h, :, t0:t0 + TT], K.t[0:96, :], reads=[K.r], writes=[Rkv[it]])
                    for tb in range(TT // 128):
                        V = Vt[tb % 2]
                        for hf in range(2):
                            bk = PS[4 + hf]
                            for kc in range(2):
                                mm(bk.t[:, :], ckvn.t[:, kc, tb * 128:(tb + 1) * 128], kuv.t[:, kc, hf * 512:(hf + 1) * 512],
                                   kc == 0, kc == 1, [kuv.r, ckvn.r], [bk.r])
                            cp("act" if hf == 0 else "dve", V.t[:, hf * 8:(hf + 1) * 8, 0:64],
                               bk.t[:, :].rearrange("p (h d) -> p h d", d=64), [], [bk.r, V.r])
                        P.dma(vscr[:, t0 + tb * 128:t0 + (tb + 1) * 128, :].rearrange("h t d -> t h d"), V.t[:],
                              reads=[V.r], writes=[Rkv[it]])
                P.barrier()

        def mla_layer(j, layer, src, dst):
            TT = 512
            with ExitStack() as st:
                qd = sbt(st, "qd", [128, 8, QL], BF16)
                qu = sbt(st, "qu", [128, 3, H * 96], BF16)
                qus = sbt(st, "qus", [128, 3, H * 32], BF16)
                ow = sbt(st, "ow", [64, H, D], BF16)
                load_wk(qd, qd_d[j], 8, QL)
                load_wk(qu, qu_d[j], 3, H * 96)
                load_wk(qus, qus_d[j], 3, H * 32)
                for h in range(H):
                    load_w(lambda r, c0, cw, h=h: ow.t[0:r, h, c0:c0 + cw], ow.r, ow_d[j, h * 64:(h + 1) * 64, :], 64, D)
                xt = sbt(st, "xt", [128, 8, TT], F32)
                xn = sbt(st, "xn", [128, 8, TT], BF16)
                sq = sbt(st, "sq", [128, 8, TT], BF16)
                rstd = sbt(st, "rstd", [128, TT], F32)
                cq = sbt(st, "cq", [128, 3, TT], F32)
                cqn = sbt(st, "cqn", [128, 3, TT], BF16)
                rp = sbt(st, "rp", [128, 2, TT], F32)
                t1 = sbt(st, "t1", [128, TT], F32)
                t2 = sbt(st, "t2", [128, TT], F32)
                QT = sbt(st, "QT", [128, H, TT], BF16)
                OT = sbt(st, "OT", [64, H, TT], BF16)
                KT = [sbt(st, "KT%d" % k, [128, T], BF16) for k in range(2)]
                VT = [sbt(st, "VT%d" % k, [128, T // 128, 128], BF16) for k in range(2)]
                PT = [sbt(st, "PT%d" % k, [128, TT], BF16) for k in range(3)]
                rec = sbt(st, "rec", [64, TT], F32)
                msb = sbt(st, "msb", [128, 8, TT], F32)
                R = slice(64, 96)
                pi = 0
                for it in range(T // TT):
                    t0 = it * TT
                    P.dma(xt.t[:], xtile(src, t0, TT), reads=rx(t0, TT), writes=[xt.r])
                    P.dma(rp.t[R, :, :], rope_d[R, :, t0:t0 + TT], reads=[Rin], writes=[rp.r])
                    rms_stats(sq, xt.t[:], 8, TT, D, NORM_EPS, rstd, [xt.r], PS[2])
                    for c in range(8):
                        stt(ew(), xn.t[:, c, :], xt.t[:, c, :], vD("ng%d_0" % layer, c), rstd.t[:, :], ALU.mult, ALU.mult,
                            [xt.r, rstd.r, vt.r], [xn.r])
                    for m in range(3):
                        bk = PS[m % 2]
                        for kc in range(8):
                            mm(bk.t[:, :], qd.t[:, kc, m * 128:(m + 1) * 128], xn.t[:, kc, :], kc == 0, kc == 7,
                               [qd.r, xn.r], [bk.r])
                        cp("act", cq.t[:, m, :], bk.t[:, :], [], [bk.r, cq.r])
                    rms_stats(sq, cq.t[:], 3, TT, QL, NORM_EPS, rstd, [cq.r], PS[2])
                    for c in range(3):
                        stt("dve", cqn.t[:, c, :], cq.t[:, c, :], vQ(j, c), rstd.t[:, :], ALU.mult, ALU.mult,
                            [cq.r, rstd.r, vt.r], [cqn.r])
                    for h in range(H):
                        A = PS[h % 2]
                        Bk = PS[2]
                        for kc in range(3):
                            mm(A.t[0:96, :], qu.t[:, kc, h * 96:(h + 1) * 96], cqn.t[:, kc, :], kc == 0, kc == 2,
                               [qu.r, cqn.r], [A.r])
                        for kc in range(3):
                            mm(Bk.t[R, :], qus.t[:, kc, h * 32:(h + 1) * 32], cqn.t[:, kc, :], kc == 0, kc == 2,
                               [qus.r, cqn.r], [Bk.r])
                        act(QT.t[0:64, h, :], A.t[0:64, :], AF.Copy, [], [A.r, QT.r], scale=SCALE)
                        stt("dve", t1.t[R, :], A.t[R, :], SCALE, rp.t[R, 0, :], ALU.mult, ALU.mult, [rp.r], [A.r, t1.r])
                        stt("dve", t2.t[R, :], Bk.t[R, :], SCALE, rp.t[R, 1, :], ALU.mult, ALU.mult, [rp.r], [Bk.r, t2.r])
                        tt("dve", QT.t[R, h, :], t1.t[R, :], t2.t[R, :], ALU.add, [t1.r, t2.r], [QT.r])
                    nkb = (it + 1) * 4
                    nk = nkb * 128
                    for h in range(H):
                        K = KT[h % 2]
                        V = VT[h % 2]
                        P.dma(K.t[0:96, 0:nk], kscr[h, :, 0:nk], reads=Rkv[0:it + 1], writes=[K.r])
                        P.dma(V.t[:, 0:nkb, :], vscr[h, 0:nk, :].rearrange("(kb p) d -> p kb d", p=128),
                              reads=Rkv[0:it + 1], writes=[V.r])
                        Ob = PS[6 + h % 2]
                        pts = {}

                        def qk(kb, h=h, K=K):
                            nonlocal pi
                            jd = kb - it * 4
                            c0 = max(jd, 0) * 128
                            Sb_ = PS[3 + pi % 3]
                            Pt = PT[pi % 3]
                            pi += 1
                            mm(Sb_.t[:, c0:TT], K.t[0:96, kb * 128:(kb + 1) * 128], QT.t[0:96, h, c0:TT], True, True,
                               [K.r, QT.r], [Sb_.r])
                            act(Pt.t[:, c0:TT], Sb_.t[:, c0:TT], AF.Exp, [], [Sb_.r, Pt.r])
                            if jd >= 0:
                                P.op("pool", lambda e, Pt=Pt, c0=c0: e.memset(Pt.t[64:128, c0:c0 + 64], 0.0), [], [Pt.r])
                            pts[kb] = (Pt, c0)

                        def pv(kb, V=V, Ob=Ob):
                            Pt, c0 = pts.pop(kb)
                            mm(Ob.t[:, c0:TT], V.t[:, kb, :], Pt.t[:, c0:TT], kb == 0, kb == nkb - 1, [V.r, Pt.r], [Ob.r])

                        LA = 2
                        for kb in range(min(LA, nkb)):
                            qk(kb)
                        for kb in range(nkb):
                            if kb + LA < nkb:
                                qk(kb + LA)
                            pv(kb)
                        P.op("dve", lambda e, Ob=Ob: e.reciprocal(rec.t[:, :], Ob.t[64:128, :]), [], [Ob.r, rec.r])
                        tt("dve", OT.t[:, h, :], Ob.t[0:64, :], rec.t[:, :], ALU.mult, [rec.r], [Ob.r, OT.r])
                    for m in range(8):
                        bk = PS[m % 2]
                        for h in range(H):
                            mm(bk.t[:, :], ow.t[0:64, h, m * 128:(m + 1) * 128], OT.t[0:64, h, :], h == 0, h == H - 1,
                               [ow.r, OT.r], [bk.r])
                        cp("act", msb.t[:, m, :], bk.t[:, :], [], [bk.r, msb.r])
                    rms_stats(sq, msb.t[:], 8, TT, D, NORM_EPS, rstd, [msb.r], PS[2])
                    for c in range(8):
                        stt("dve", msb.t[:, c, :], msb.t[:, c, :], vD("ng%d_1" % layer, c), rstd.t[:, :], ALU.mult, ALU.mult,
                            [rstd.r, vt.r], [msb.r])
                    tt("pool", msb.t[:], msb.t[:], xt.t[:], ALU.add, [xt.r], [msb.r])
                    P.dma(xtile(dst, t0, TT), msb.t[:], reads=[msb.r], writes=rx(t0, TT))
                P.barrier()

        P.barrier()
        cur = xT
        for layer in range(depth):
            if layer < NA:
                rwkv_layer(layer, layer, cur, xs)
            else:
                if layer == NA:
                    kv_prep(xs)
                mla_layer(layer - NA, layer, xs, xs)
            cur = xs
            ffn_layer(layer, xs, y if layer == depth - 1 else xs)
        P.barrier()
        P.emit()
        nc._n_ops = P.n
    return nc


NVT = [0, 0, 0]
_CACHE = {}


def prep_inputs(inputs):
    inp = {k: np.asarray(v) for k, v in inputs.items()}
    B, T, _ = inp["x"].shape
    vt, nd, nf = _pack_tables(inp)
    NVT[0], NVT[1], NVT[2] = vt.shape[1], nd, nf
    f32 = lambda a: np.ascontiguousarray(a, dtype=np.float32)
    kd = inp["kv_w_down"]
    kds = np.concatenate([kd[:, 272:288], kd[:, 256:272]], 1)
    ku = inp["kv_w_up"].reshape(KVL, H, 128)
    qu = inp["q_w_up"].reshape(2, QL, H, 96)
    qus = np.concatenate([qu[..., 80:96], qu[..., 64:80]], -1).reshape(2, QL, H * 32)
    shared = {
        "vt": vt, "cst": _consts(), "rope": _rope(T),
        "ffn_w_in": f32(inp["ffn_w_in"]), "ffn_w_out": f32(inp["ffn_w_out"]),
        "a_w_rkv": f32(inp["a_w_rkv"]), "a_w1": f32(inp["a_w1"]), "a_w2": f32(inp["a_w2"]),
        "a_a1": f32(inp["a_a1"]), "a_a2": f32(inp["a_a2"]), "a_g1": f32(inp["a_g1"]), "a_g2": f32(inp["a_g2"]),
        "a_w_o": f32(inp["a_w_o"]), "kv_w_down": f32(kd), "kv_w_down_sw": f32(kds),
        "kv_w_up_k": f32(ku[:, :, 0:64].reshape(KVL, H * 64)), "kv_w_up_v": f32(ku[:, :, 64:128].reshape(KVL, H * 64)),
        "q_w_down": f32(inp["q_w_down"]), "q_w_up": f32(inp["q_w_up"]), "q_w_up_sw": f32(qus),
        "o_w": f32(inp["o_w"]),
    }
    maps = []
    for b in range(B):
        m = dict(shared)
        m["xT"] = f32(inp["x"][b].T)
        maps.append(m)
    return maps, B, T


def kernel(**inputs):
    maps, B, T = prep_inputs(inputs)
    key = (T, DEPTH)
    if key not in _CACHE:
        _CACHE[key] = build(T)
    nc = _CACHE[key]
    res = run_bass_kernel_spmd(nc, maps, core_ids=list(range(B)))
    out = np.stack([np.asarray(r["y"]).T for r in res.results], 0)
    return np.ascontiguousarray(out.astype(np.float32))
```

```python
import math
from contextlib import ExitStack
import numpy as np
import concourse.bass as bass
import concourse.mybir as mybir
from concourse.bass_utils import run_bass_kernel_spmd

F32 = mybir.dt.float32
BF16 = mybir.dt.bfloat16
AF = mybir.ActivationFunctionType
ALU = mybir.AluOpType

D = 1024
DEPTH = 4
NA = 2
H = 16
FF = 2816
NF = FF // 128
QL = 384
KVL = 256
LNX_EPS = 64e-5
NORM_EPS = 1e-6
SCALE = 1.0 / math.sqrt(96.0)

COMPUTE = ("pe", "act", "dve", "pool")
SEM_LIMIT = 30000
NDMA_SEM = 24


class Res:
    __slots__ = ("name", "w", "r")

    def __init__(self, name=""):
        self.name = name
        self.w = None
        self.r = {}


class Tl:
    __slots__ = ("t", "r")

    def __init__(self, t, r):
        self.t = t
        self.r = r


class Prog:
    def __init__(self, nc, stack):
        self.nc = nc
        self.stack = stack
        self.streams = {e: [] for e in COMPUTE + ("sp",)}
        self.sems = {}
        self.cnt = {}
        for e in COMPUTE:
            self.sems[e] = [self._newsem(e + "0")]
            self.cnt[e] = (0, 0)
        self.dma_sems = [self._newsem("dma%d" % i) for i in range(NDMA_SEM)]
        self.dma_cnt = [0] * NDMA_SEM
        self.dma_i = 0
        self.seen = {e: {} for e in self.streams}
        self.n = 0

    def _newsem(self, name):
        return self.stack.enter_context(self.nc.semaphore(name))

    def _semof(self, key, idx):
        if isinstance(key, tuple):
            return self.dma_sems[key[1]]
        return self.sems[key][idx]

    @staticmethod
    def _need(waits, tok):
        if tok is None:
            return
        key, idx, val = tok
        cur = waits.get(key)
        if cur is None or (idx, val) > cur:
            waits[key] = (idx, val)

    def _collect(self, q, reads, writes, eng):
        waits = {}
        for r in reads:
            self._need(waits, r.w)
        for w in writes:
            self._need(waits, w.w)
            for k, (i, v) in w.r.items():
                if k == eng:
                    continue
                self._need(waits, (k, i, v))
        if eng == "pe":
            waits.pop("pe", None)
        out = []
        for key, (idx, val) in waits.items():
            s = self.seen[q].get(key)
            if s is not None and s >= (idx, val):
                continue
            self.seen[q][key] = (idx, val)
            out.append((self._semof(key, idx), val))
        return out

    def op(self, eng, fn, reads=(), writes=()):
        waits = self._collect(eng, reads, writes, eng)
        idx, val = self.cnt[eng]
        if val >= SEM_LIMIT:
            idx += 1
            val = 0
            self.sems[eng].append(self._newsem("%s%d" % (eng, idx)))
        val += 1
        self.cnt[eng] = (idx, val)
        self.streams[eng].append((waits, fn, (self.sems[eng][idx], 1)))
        tok = (eng, idx, val)
        for r in reads:
            r.r[eng] = (idx, val)
        for w in writes:
            w.w = tok
            w.r = {}
        self.n += 1
        return tok

    def dma(self, out, in_, reads=(), writes=(), q="sp"):
        waits = self._collect(q, reads, writes, None)
        j = self.dma_i % NDMA_SEM
        self.dma_i += 1
        self.dma_cnt[j] += 16
        val = self.dma_cnt[j]
        key = ("dma", j)
        self.streams[q].append(
            (waits, lambda e: e.dma_start(out=out, in_=in_), (self.dma_sems[j], 16)))
        tok = (key, 0, val)
        for r in reads:
            r.r[key] = (0, val)
        for w in writes:
            w.w = tok
            w.r = {}
        self.n += 1
        return tok

    def barrier(self):
        for q in self.streams:
            waits = []
            for e in COMPUTE:
                if e == q:
                    continue
                idx, val = self.cnt[e]
                if val == 0:
                    continue
                sn = self.seen[q].get(e)
                if sn is not None and sn >= (idx, val):
                    continue
                self.seen[q][e] = (idx, val)
                waits.append((self.sems[e][idx], val))
            for j in range(NDMA_SEM):
                val = self.dma_cnt[j]
                if val == 0:
                    continue
                key = ("dma", j)
                sn = self.seen[q].get(key)
                if sn is not None and sn >= (0, val):
                    continue
                self.seen[q][key] = (0, val)
                waits.append((self.dma_sems[j], val))
            self.streams[q].append((waits, None, None))

    def emit(self):
        nc = self.nc
        with nc.Block() as block:
            def run(stream):
                def body(e):
                    for waits, fn, inc in stream:
                        for s, v in waits:
                            e.wait_ge(s, v)
                        if fn is not None:
                            ins = fn(e)
                            if inc is not None:
                                ins.then_inc(inc[0], inc[1])
                return body
            block.tensor(run(self.streams["pe"]))
            block.scalar(run(self.streams["act"]))
            block.vector(run(self.streams["dve"]))
            block.gpsimd(run(self.streams["pool"]))
            block.sync(run(self.streams["sp"]))


VD = {}


def _pack_tables(inp):
    vecs = []

    def add(name, v):
        VD[name] = len(vecs)
        vecs.append(np.asarray(v, np.float32).reshape(D))

    for l in range(DEPTH):
        for j in range(4):
            add("ng%d_%d" % (l, j), inp["norm_g"][l, j])
    for i in range(NA):
        for n in range(6):
            add("mu%d_%d" % (i, n), inp["a_mu"][i, n])
        for nm in ("w0", "a0", "k_k", "k_a", "r_k", "lnx_w", "lnx_b"):
            add("%s%d" % (nm, i), inp["a_" + nm][i])
    add("kvng", inp["kv_norm_g"])
    tabD = np.stack(vecs, 0).reshape(len(vecs), 8, 128).transpose(2, 0, 1).reshape(128, -1)
    fv = []
    for l in range(DEPTH):
        for j in range(3):
            fv.append(inp["ffn_conv_w"][l, j])
        fv.append(inp["ffn_conv_b"][l])
    tabF = np.stack(fv, 0).reshape(len(fv), NF, 128).transpose(2, 0, 1).reshape(128, -1)
    qg = np.asarray(inp["q_norm_g"]).reshape(2, 3, 128).transpose(2, 0, 1).reshape(128, 6)
    kg = np.asarray(inp["kv_a_norm_g"]).reshape(2, 128).T
    vt = np.ascontiguousarray(np.concatenate([tabD, tabF, qg, kg], 1).astype(np.float32))
    return vt, tabD.shape[1], tabF.shape[1]


def _consts():
    p = np.arange(128)
    ident = np.eye(128, dtype=np.float32)
    bd = (p[:, None] // 64 == p[None, :] // 64).astype(np.float32)
    su = (p[:, None] < p[None, :]).astype(np.float32)
    ui = (p[:, None] <= p[None, :]).astype(np.float32)
    low = (p[None, :] < p[:, None]).astype(np.float32)
    ones = np.ones((128, 128), np.float32)
    return np.ascontiguousarray(np.concatenate([ident, bd, su, ui, su, ui, low, ones], 1))


C_ID, C_BD, C_M4, C_LOW, C_ONE = 0, 128, 256, 768, 896
NCST = 1024


def _rope(T):
    inv = 1.0 / (10000.0 ** (np.arange(0, 32, 2, dtype=np.float32) / 32.0))
    ang = np.arange(T, dtype=np.float32)[:, None] * inv[None, :].astype(np.float32)
    cos = np.cos(ang).astype(np.float32).T
    sin = np.sin(ang).astype(np.float32).T
    tab = np.zeros((128, 2, T), np.float32)
    tab[64:80, 0] = cos
    tab[80:96, 0] = cos
    tab[64:80, 1] = -sin
    tab[80:96, 1] = sin
    return tab


def build(T, depth=DEPTH, dbg=False):
    nc = bass.Bass("TRN2", target_bir_lowering=False)
    NT5 = T // 512
    NT2 = T // 256

    def din(name, shape):
        return nc.dram_tensor(name, list(shape), F32, kind="ExternalInput").ap()

    xT = din("xT", [D, T])
    vt_d = din("vt", [128, NVT[0]])
    cst_d = din("cst", [128, NCST])
    rope_d = din("rope", [128, 2, T])
    w_in_d = din("ffn_w_in", [DEPTH, D, 2 * FF])
    w_out_d = din("ffn_w_out", [DEPTH, FF, D])
    wrkv_d = din("a_w_rkv", [NA, 3, D, D])
    w1_d = din("a_w1", [NA, D, 64])
    w2_d = din("a_w2", [NA, 64, D])
    a1_d = din("a_a1", [NA, D, 64])
    a2_d = din("a_a2", [NA, 64, D])
    g1_d = din("a_g1", [NA, D, 160])
    g2_d = din("a_g2", [NA, 160, D])
    wo_d = din("a_w_o", [NA, D, D])
    kd_d = din("kv_w_down", [D, 288])
    kds_d = din("kv_w_down_sw", [D, 32])
    kuk_d = din("kv_w_up_k", [KVL, H * 64])
    kuv_d = din("kv_w_up_v", [KVL, H * 64])
    qd_d = din("q_w_down", [2, D, QL])
    qu_d = din("q_w_up", [2, QL, H * 96])
    qus_d = din("q_w_up_sw", [2, QL, H * 32])
    ow_d = din("o_w", [2, D, D])
    y = nc.dram_tensor("y", [D, T], F32, kind="ExternalOutput").ap()
    xs = nc.dram_tensor("xs", [D, T], F32).ap()
    hscr = nc.dram_tensor("hscr", [FF, T], BF16).ap()
    kscr = nc.dram_tensor("kscr", [H, 96, T], BF16).ap()
    vscr = nc.dram_tensor("vscr", [H, T, 128], BF16).ap()

    Rx = [Res("x%d" % i) for i in range(NT2)]
    Rh = [Res("h%d" % i) for i in range(NT5)]
    Rkv = [Res("kv%d" % i) for i in range(NT5)]
    Rin = Res("in")

    with ExitStack() as top:
        P = Prog(nc, top)
        top.enter_context(nc.allow_low_precision("bf16 matmul operands, fp32 accumulate"))

        uid = [0]

        def sbt(st, name, shape, dt):
            uid[0] += 1
            nm = "sb%d_%s" % (uid[0], name)
            return Tl(st.enter_context(nc.sbuf_tensor(nm, list(shape), dt)), Res(nm))

        PS = [Tl(top.enter_context(nc.psum_tensor("ps%d" % i, [128, 512], F32)), Res("ps%d" % i))
              for i in range(8)]
        vt = sbt(top, "vt", [128, NVT[0]], F32)
        cst = sbt(top, "cst", [128, NCST], F32)
        onesb = sbt(top, "onesb", [128, 128], BF16)
        stg = [sbt(top, "stg%d" % i, [128, 1024], F32) for i in range(2)]
        stg_i = [0]
        P.dma(vt.t[:], vt_d, writes=[vt.r])
        P.dma(cst.t[:], cst_d, writes=[cst.r])
        P.op("dve", lambda e: e.tensor_copy(onesb.t[:], cst.t[:, C_ONE:C_ONE + 128]), [cst.r], [onesb.r])
        identb_t = sbt(top, "identb", [128, 128], BF16)
        P.op("dve", lambda e: e.tensor_copy(identb_t.t[:], cst.t[:, C_ID:C_ID + 128]), [cst.r], [identb_t.r])
        identb = identb_t.t[:, :]
        bdb_t = sbt(top, "bdb", [128, 128], BF16)
        P.op("dve", lambda e: e.tensor_copy(bdb_t.t[:], cst.t[:, C_BD:C_BD + 128]), [cst.r], [bdb_t.r])
        bdb = bdb_t.t[:, :]

        ident = cst.t[:, C_ID:C_ID + 128]
        bdones = cst.t[:, C_BD:C_BD + 128]
        mask4 = cst.t[:, C_M4:C_M4 + 512]
        lowm = cst.t[:, C_LOW:C_LOW + 128]
        ones = cst.t[:, C_ONE:C_ONE + 128]

        def vD(name, c):
            i = VD[name] * 8 + c
            return vt.t[:, i:i + 1]

        def vF(l, j, f):
            i = NVT[1] + (l * 4 + j) * NF + f
            return vt.t[:, i:i + 1]

        def vQ(j, c):
            i = NVT[1] + NVT[2] + j * 3 + c
            return vt.t[:, i:i + 1]

        def vK(c):
            i = NVT[1] + NVT[2] + 6 + c
            return vt.t[:, i:i + 1]

        def mm(out, lhsT, rhs, start, stop, reads, writes):
            P.op("pe", lambda e: e.matmul(out, lhsT, rhs, start=start, stop=stop), reads, writes)

        def act(out, in_, func, reads, writes, bias=None, scale=None):
            kw = {}
            if bias is not None:
                kw["bias"] = bias
            if scale is not None:
                kw["scale"] = scale
            P.op("act", lambda e: e.activation(out, in_, func, **kw), reads, writes)

        def tt(eng, out, in0, in1, op, reads, writes):
            P.op(eng, lambda e: e.tensor_tensor(out, in0, in1, op), reads, writes)

        def ts(eng, out, in0, s1, s2, op0, op1, reads, writes):
            if s2 is None:
                P.op(eng, lambda e: e.tensor_scalar(out, in0, s1, None, op0), reads, writes)
            else:
                P.op(eng, lambda e: e.tensor_scalar(out, in0, s1, s2, op0, op1), reads, writes)

        def stt(eng, out, in0, sc, in1, op0, op1, reads, writes):
            P.op("dve", lambda e: e.scalar_tensor_tensor(out, in0, sc, in1, op0, op1), reads, writes)

        def cp(eng, out, in_, reads, writes):
            if eng == "act":
                act(out, in_, AF.Copy, reads, writes)
            else:
                P.op(eng, lambda e: e.tensor_copy(out, in_), reads, writes)

        rr = [0]

        def ew():
            rr[0] += 1
            return "pool" if rr[0] % 3 == 0 else "dve"

        def load_w(view, res, src, rows, cols):
            c0 = 0
            while c0 < cols:
                cw = min(1024, cols - c0)
                s = stg[stg_i[0] % len(stg)]
                stg_i[0] += 1
                P.dma(s.t[0:rows, 0:cw], src[:, c0:c0 + cw], reads=[Rin], writes=[s.r])
                eng = "pool" if stg_i[0] % 2 == 0 else "dve"
                cp(eng, view(rows, c0, cw), s.t[0:rows, 0:cw], [s.r], [res])
                c0 += cw

        def load_wk(tile, src, nk, cols, rows_last=128):
            for k in range(nk):
                rows = rows_last if k == nk - 1 else 128
                load_w(lambda r, c0, cw, k=k: tile.t[0:r, k, c0:c0 + cw], tile.r,
                       src[k * 128:k * 128 + rows, :], rows, cols)

        def rms_stats(st_sq, src_ap, nch, TT, Dn, eps, rstd, src_reads, bank):
            act(st_sq.t[:, 0:nch, 0:TT], src_ap, AF.Square, src_reads, [st_sq.r])
            for c in range(nch):
                mm(bank.t[:, 0:TT], onesb.t[:, :], st_sq.t[:, c, 0:TT], c == 0, c == nch - 1,
                   [st_sq.r, onesb.r], [bank.r])
            act(rstd.t[:, 0:TT], bank.t[:, 0:TT], AF.Sqrt, [], [bank.r, rstd.r], bias=eps, scale=1.0 / Dn)
            P.op("dve", lambda e: e.reciprocal(rstd.t[:, 0:TT], rstd.t[:, 0:TT]), [], [rstd.r])

        def xtile(ap, t0, TT):
            return ap.rearrange("(c p) t -> p c t", p=128)[:, :, t0:t0 + TT]

        def rx(t0, TT):
            return Rx[t0 // 256:(t0 + TT) // 256]

        def run_jobs(gens, width, stagger):
            active = []
            it = iter(gens)
            steps0 = 0
            done = False
            while True:
                while not done and len(active) < width and (len(active) == 0 or steps0 >= stagger):
                    g = next(it, None)
                    if g is None:
                        done = True
                        break
                    active.append(g)
                    if len(active) == 1:
                        steps0 = 0
                if not active:
                    break
                for g in list(active):
                    try:
                        next(g)
                    except StopIteration:
                        active.remove(g)
                        steps0 = stagger
                steps0 += 1

        def rwkv_layer(i, layer, src, dst):
            TT = 256
            with ExitStack() as st:
                wr = sbt(st, "wr", [128, 8, D], BF16)
                wk = sbt(st, "wk", [128, 8, D], BF16)
                wv = sbt(st, "wv", [128, 8, D], BF16)
                wo = sbt(st, "wo", [128, 8, D], BF16)
                w1 = sbt(st, "w1", [128, 8, 64], BF16)
                a1 = sbt(st, "a1", [128, 8, 64], BF16)
                g1 = sbt(st, "g1", [128, 8, 160], BF16)
                w2 = sbt(st, "w2", [128, 1, D], BF16)
                a2 = sbt(st, "a2", [128, 1, D], BF16)
                g2 = sbt(st, "g2", [128, 2, D], BF16)
                for tl, srcw in ((wr, wrkv_d[i, 0]), (wk, wrkv_d[i, 1]), (wv, wrkv_d[i, 2]), (wo, wo_d[i])):
                    load_wk(tl, srcw, 8, D)
                load_wk(w1, w1_d[i], 8, 64)
                load_wk(a1, a1_d[i], 8, 64)
                load_wk(g1, g1_d[i], 8, 160)
                load_wk(w2, w2_d[i], 1, D, rows_last=64)
                load_wk(a2, a2_d[i], 1, D, rows_last=64)
                load_wk(g2, g2_d[i], 2, D, rows_last=32)

                xt = sbt(st, "xt", [128, 8, TT], F32)
                hb = sbt(st, "hb", [128, 8, TT + 1], F32)
                xsn = [sbt(st, "xs%d" % n, [128, 8, TT], BF16) for n in range(6)]
                rstd = sbt(st, "rstd", [128, TT], F32)
                tw = sbt(st, "tw", [64, TT], BF16)
                ta = sbt(st, "ta", [64, TT], BF16)
                tg = sbt(st, "tg", [128, 2, TT], BF16)
                z = sbt(st, "z", [128, 8, TT], BF16)
                sq = z
                msb = sbt(st, "msb", [128, 8, TT], F32)
                xx = msb
                S = sbt(st, "S", [128, 8, 64], F32)
                Sb = sbt(st, "Sb", [128, 8, 64], BF16)
                names = "r k v kk a lw g rn kmod b cum cex ecum bonus d".split()
                sets = []
                for k in range(2):
                    J = dict(
                        cb={nm: sbt(st, "c%d_%s" % (k, nm), [128, TT], F32) for nm in names},
                        vb=sbt(st, "vb%d" % k, [128, TT], BF16),
                        sqb=sbt(st, "sqb%d" % k, [128, TT], BF16),
                        AR=sbt(st, "AR%d" % k, [128, 2 * TT], BF16),
                        KB=sbt(st, "KB%d" % k, [128, 2 * TT], BF16),
                        TM=sbt(st, "TM%d" % k, [128, 2, 3, 128], BF16),
                        bA=PS[4 * k], bB=PS[4 * k + 1], hd=[])
                    for hh in range(2):
                        J["hd"].append(dict(
                            AMs=sbt(st, "AMs%d_%d" % (k, hh), [128, 512], BF16),
                            Ls=sbt(st, "Ls%d_%d" % (k, hh), [128, 128], BF16),
                            LM=[sbt(st, "LM%d_%d_%d" % (k, hh, q), [128, 256], BF16) for q in range(2)],
                            Tt=[sbt(st, "Tt%d_%d_%d" % (k, hh, q), [128, 128], BF16) for q in range(2)],
                            Xs=sbt(st, "Xs%d_%d" % (k, hh), [128, 64], BF16),
                            Us=sbt(st, "Us%d_%d" % (k, hh), [128, 64], BF16),
                            tmpS=sbt(st, "tmpS%d_%d" % (k, hh), [128, 64], F32),
                            WK=PS[4 * k + 2 + hh]))
                    sets.append(J)
                SR = [Res("S%d" % c) for c in range(8)]
                SbR = [Res("Sb%d" % c) for c in range(8)]
                P.op("pool", lambda e: e.memset(S.t[:], 0.0), [], SR)
                P.op("pool", lambda e: e.memset(Sb.t[:], 0.0), [], SbR)
                P.op("pool", lambda e: e.memset(hb.t[:, :, 0:1], 0.0), [], [hb.r])

                def job(c, J):
                    B = J["cb"]
                    vb, AR, KB, TM, bA, bB, hd = J["vb"], J["AR"], J["KB"], J["TM"], J["bA"], J["bB"], J["hd"]
                    sqb = J["sqb"]
                    cs = slice(c * 128, (c + 1) * 128)
                    for kc in range(8):
                        mm(bA.t[:, 0:TT], wr.t[:, kc, cs], xsn[0].t[:, kc, :], kc == 0, kc == 7, [wr.r, xsn[0].r], [bA.r])
                    for kc in range(8):
                        mm(bA.t[:, TT:2 * TT], wk.t[:, kc, cs], xsn[1].t[:, kc, :], kc == 0, kc == 7, [wk.r, xsn[1].r], [bA.r])
                    for kc in range(8):
                        mm(bB.t[:, 0:TT], wv.t[:, kc, cs], xsn[2].t[:, kc, :], kc == 0, kc == 7, [wv.r, xsn[2].r], [bB.r])
                    mm(bB.t[:, TT:2 * TT], w2.t[0:64, 0, cs], tw.t[:], True, True, [w2.r, tw.r], [bB.r])
                    yield
                    cp("act", B["r"].t[:], bA.t[:, 0:TT], [], [bA.r, B["r"].r])
                    ts("dve", B["kk"].t[:], bA.t[:, TT:2 * TT], vD("k_k%d" % i, c), None, ALU.mult, None, [vt.r], [bA.r, B["kk"].r])
                    cp("act", B["k"].t[:], bA.t[:, TT:2 * TT], [], [bA.r, B["k"].r])
                    yield
                    cp("dve", B["v"].t[:], bB.t[:, 0:TT], [], [bB.r, B["v"].r])
                    cp("act", vb.t[:], bB.t[:, 0:TT], [], [bB.r, vb.r])
                    act(B["lw"].t[:], bB.t[:, TT:2 * TT], AF.Sigmoid, [vt.r], [bB.r, B["lw"].r], bias=vD("w0%d" % i, c))
                    ts("pool", B["lw"].t[:], B["lw"].t[:], -math.exp(-0.5), None, ALU.mult, None, [], [B["lw"].r])
                    yield
                    mm(bA.t[:, 0:TT], a2.t[0:64, 0, cs], ta.t[:], True, True, [a2.r, ta.r], [bA.r])
                    mm(bA.t[:, TT:2 * TT], g2.t[:, 0, cs], tg.t[:, 0, :], True, False, [g2.r, tg.r], [bA.r])
                    mm(bA.t[:, TT:2 * TT], g2.t[0:32, 1, cs], tg.t[0:32, 1, :], False, True, [g2.r, tg.r], [bA.r])
                    act(B["a"].t[:], bA.t[:, 0:TT], AF.Sigmoid, [vt.r], [bA.r, B["a"].r], bias=vD("a0%d" % i, c))
                    cp("act", B["g"].t[:], bA.t[:, TT:2 * TT], [], [bA.r, B["g"].r])
                    yield
                    act(sqb.t[:], B["kk"].t[:], AF.Square, [B["kk"].r], [sqb.r])
                    mm(bB.t[:, 0:TT], bdb, sqb.t[:], True, True, [bdb_t.r, sqb.r], [bB.r])
                    act(B["rn"].t[:], bB.t[:, 0:TT], AF.Sqrt, [], [bB.r, B["rn"].r])
                    ts("dve", B["rn"].t[:], B["rn"].t[:], 1e-12, None, ALU.max, None, [], [B["rn"].r])
                    P.op("dve", lambda e: e.reciprocal(B["rn"].t[:], B["rn"].t[:]), [], [B["rn"].r])
                    tt("dve", B["kk"].t[:], B["kk"].t[:], B["rn"].t[:], ALU.mult, [B["rn"].r], [B["kk"].r])
                    yield
                    ts("pool", B["kmod"].t[:], B["a"].t[:], -1.0, vD("k_a%d" % i, c), ALU.add, ALU.mult, [B["a"].r, vt.r], [B["kmod"].r])
                    stt("dve", B["kmod"].t[:], B["kmod"].t[:], 1.0, B["k"].t[:], ALU.add, ALU.mult, [B["k"].r], [B["kmod"].r])
                    tt("pool", B["b"].t[:], B["kk"].t[:], B["a"].t[:], ALU.mult, [B["kk"].r, B["a"].r], [B["b"].r])
                    for ch in range(2):
                        sl = slice(ch * 128, (ch + 1) * 128)
                        P.op("dve", lambda e, sl=sl: e.tensor_tensor_scan(
                            B["cum"].t[:, sl], ones, B["lw"].t[:, sl], 0.0, ALU.mult, ALU.add), [cst.r, B["lw"].r], [B["cum"].r])
                    tt("pool", B["cex"].t[:], B["cum"].t[:], B["lw"].t[:], ALU.subtract, [B["cum"].r, B["lw"].r], [B["cex"].r])
                    act(B["ecum"].t[:], B["cum"].t[:], AF.Exp, [B["cum"].r], [B["ecum"].r])
                    act(B["cum"].t[:], B["cum"].t[:], AF.Exp, [], [B["cum"].r], scale=-1.0)
                    act(B["cex"].t[:], B["cex"].t[:], AF.Exp, [], [B["cex"].r])
                    yield
                    for ch in range(2):
                        sl = slice(ch * 128, (ch + 1) * 128)
                        o = ch * 256
                        stt("dve", AR.t[:, o:o + 128], B["kk"].t[:, sl], -1.0, B["cex"].t[:, sl], ALU.mult, ALU.mult,
                            [B["kk"].r, B["cex"].r], [AR.r])
                        tt("pool", AR.t[:, o + 128:o + 256], B["r"].t[:, sl], B["ecum"].t[:, sl], ALU.mult, [B["r"].r, B["ecum"].r], [AR.r])
                        tt("dve", KB.t[:, o:o + 128], B["kmod"].t[:, sl], B["cum"].t[:, sl], ALU.mult, [B["kmod"].r, B["cum"].r], [KB.r])
                        tt("pool", KB.t[:, o + 128:o + 256], B["b"].t[:, sl], B["cum"].t[:, sl], ALU.mult, [B["b"].r, B["cum"].r], [KB.r])
                    yield
                    stt("dve", sqb.t[:], B["r"].t[:], vD("r_k%d" % i, c), B["kmod"].t[:], ALU.mult, ALU.mult,
                        [B["r"].r, B["kmod"].r, vt.r], [sqb.r])
                    mm(bB.t[:, TT:2 * TT], bdb, sqb.t[:], True, True, [bdb_t.r, sqb.r], [bB.r])
                    tt("dve", B["bonus"].t[:], bB.t[:, TT:2 * TT], B["v"].t[:], ALU.mult, [B["v"].r], [bB.r, B["bonus"].r])
                    for ch in range(2):
                        sl = slice(ch * 128, (ch + 1) * 128)
                        o = ch * 256
                        mm(bA.t[:, ch * 128:ch * 128 + 128], vb.t[:, sl], identb, True, True, [vb.r, identb_t.r], [bA.r])
                        mm(bA.t[:, 256 + ch * 128:256 + ch * 128 + 128], KB.t[:, o:o + 128], identb, True, True, [KB.r, identb_t.r], [bA.r])
                        mm(bB.t[:, ch * 128:ch * 128 + 128], KB.t[:, o + 128:o + 256], identb, True, True, [KB.r, identb_t.r], [bB.r])
                    yield
                    for ch in range(2):
                        cp("act", TM.t[:, ch, 0, :], bA.t[:, ch * 128:ch * 128 + 128], [], [bA.r, TM.r])
                        cp("dve", TM.t[:, ch, 1, :], bA.t[:, 256 + ch * 128:256 + ch * 128 + 128], [], [bA.r, TM.r])
                        cp("act", TM.t[:, ch, 2, :], bB.t[:, ch * 128:ch * 128 + 128], [], [bB.r, TM.r])
                    yield
                    for ch in range(2):
                        o = ch * 256
                        for hh in range(2):
                            p = slice(hh * 64, hh * 64 + 64)
                            Hh = hd[hh]
                            WK = Hh["WK"]
                            mm(WK.t[:, 0:256], KB.t[p, o:o + 128], AR.t[p, o:o + 256], True, True, [KB.r, AR.r], [WK.r])
                            mm(WK.t[:, 256:512], KB.t[p, o + 128:o + 256], AR.t[p, o:o + 256], True, True, [KB.r, AR.r], [WK.r])
                            TB = bA if hh == 0 else bB
                            mm(TB.t[:, 128:256], AR.t[p, o:o + 128], KB.t[p, o + 128:o + 256], True, True, [KB.r, AR.r], [TB.r])
                            yield
                        for hh in range(2):
                            Hh = hd[hh]
                            WK = Hh["WK"]
                            TB = bA if hh == 0 else bB
                            tt("dve", Hh["AMs"].t[:], WK.t[:], mask4, ALU.mult, [cst.r], [WK.r, Hh["AMs"].r])
                            tt("dve", Hh["Ls"].t[:], TB.t[:, 128:256], lowm, ALU.mult, [cst.r], [TB.r, Hh["Ls"].r])
                            tt("pool", Hh["Tt"][0].t[:], Hh["AMs"].t[:, 256:384], identb, ALU.add, [Hh["AMs"].r, identb_t.r], [Hh["Tt"][0].r])
                            yield
                        for lv in range(1, 7):
                            for hh in range(2):
                                Hh = hd[hh]
                                WK = Hh["WK"]
                                if lv == 1:
                                    Lp, Mp, rd = Hh["Ls"].t[:], Hh["AMs"].t[:, 256:384], [Hh["Ls"].r, Hh["AMs"].r]
                                else:
                                    pl = Hh["LM"][(lv - 1) % 2]
                                    Lp, Mp, rd = pl.t[:, 0:128], pl.t[:, 128:256], [pl.r]
                                nl = Hh["LM"][lv % 2]
                                mm(WK.t[:, 128:256], Mp, Lp, True, True, rd, [WK.r])
                                if lv < 6:
                                    mm(WK.t[:, 256:384], Lp, Mp, True, True, rd, [WK.r])
                                    cp("act", nl.t[:, 0:256], WK.t[:, 128:384], [], [WK.r, nl.r])
                                else:
                                    cp("act", nl.t[:, 0:128], WK.t[:, 128:256], [], [WK.r, nl.r])
                                yield
                            for hh in range(2):
                                Hh = hd[hh]
                                WK = Hh["WK"]
                                nl = Hh["LM"][lv % 2]
                                Tc = Hh["Tt"][(lv - 1) % 2]
                                Tn = Hh["Tt"][lv % 2]
                                TB = bA if hh == 0 else bB
                                mm(TB.t[:, 0:128], nl.t[:, 0:128], Tc.t[:], True, True, [nl.r, Tc.r], [TB.r])
                                tt("dve", Tn.t[:], TB.t[:, 0:128], Tc.t[:], ALU.add, [Tc.r], [TB.r, Tn.r])
                                yield
                        for hh in range(2):
                            p = slice(hh * 64, hh * 64 + 64)
                            fs = slice(hh * 64, hh * 64 + 64)
                            Hh = hd[hh]
                            WK = Hh["WK"]
                            mm(WK.t[:, 0:64], AR.t[p, o:o + 128], Sb.t[p, c, :], True, False, [AR.r, SbR[c]], [WK.r])
                            mm(WK.t[:, 0:64], Hh["AMs"].t[:, 0:128], TM.t[:, ch, 0, fs], False, True, [Hh["AMs"].r, TM.r], [WK.r])
                            cp("act", Hh["Xs"].t[:], WK.t[:, 0:64], [], [WK.r, Hh["Xs"].r])
                            yield
                        for hh in range(2):
                            Hh = hd[hh]
                            WK = Hh["WK"]
                            Tf = Hh["Tt"][0]
                            mm(WK.t[:, 64:128], Tf.t[:], Hh["Xs"].t[:], True, True, [Tf.r, Hh["Xs"].r], [WK.r])
                            cp("act", Hh["Us"].t[:], WK.t[:, 64:128], [], [WK.r, Hh["Us"].r])
                            yield
                        for hh in range(2):
                            p = slice(hh * 64, hh * 64 + 64)
                            fs = slice(hh * 64, hh * 64 + 64)
                            Hh = hd[hh]
                            WK = Hh["WK"]
                            yo = WK.t[p, 128:256]
                            mm(yo, Sb.t[p, c, :], AR.t[p, o + 128:o + 256], True, False, [SbR[c], AR.r], [WK.r])
                            mm(yo, Hh["Us"].t[:], Hh["AMs"].t[:, 384:512], False, False, [Hh["Us"].r, Hh["AMs"].r], [WK.r])
                            mm(yo, TM.t[:, ch, 0, fs], Hh["AMs"].t[:, 128:256], False, True, [TM.r, Hh["AMs"].r], [WK.r])
                            so = WK.t[p, 256:320]
                            mm(so, TM.t[:, ch, 2, fs], Hh["Us"].t[:], True, False, [TM.r, Hh["Us"].r], [WK.r])
                            mm(so, TM.t[:, ch, 1, fs], TM.t[:, ch, 0, fs], False, True, [TM.r], [WK.r])
                            yield
                        for hh in range(2):
                            p = slice(hh * 64, hh * 64 + 64)
                            Hh = hd[hh]
                            WK = Hh["WK"]
                            yo = WK.t[p, 128:256]
                            so = WK.t[p, 256:320]
                            cp("act", B["d"].t[p, ch * 128:ch * 128 + 128], yo, [], [WK.r, B["d"].r])
                            tt("dve", Hh["tmpS"].t[p, :], so, S.t[p, c, :], ALU.add, [SR[c]], [WK.r, Hh["tmpS"].r])
                            wc = B["ecum"].t[p, ch * 128 + 127:ch * 128 + 128]
                            ts("dve", S.t[p, c, :], Hh["tmpS"].t[p, :], wc, None, ALU.mult, None, [Hh["tmpS"].r, B["ecum"].r], [SR[c]])
                            ts("pool", Sb.t[p, c, :], Hh["tmpS"].t[p, :], wc, None, ALU.mult, None, [Hh["tmpS"].r, B["ecum"].r], [SbR[c]])
                            yield
                    cp("pool", sqb.t[:], B["d"].t[:], [B["d"].r], [sqb.r])
                    mm(bB.t[:, 0:TT], bdb, sqb.t[:], True, True, [bdb_t.r, sqb.r], [bB.r])
                    stt("dve", B["d"].t[:], bB.t[:, 0:TT], -1.0 / 64, B["d"].t[:], ALU.mult, ALU.add, [], [bB.r, B["d"].r])
                    act(sqb.t[:], B["d"].t[:], AF.Square, [B["d"].r], [sqb.r])
                    mm(bB.t[:, TT:2 * TT], bdb, sqb.t[:], True, True, [bdb_t.r, sqb.r], [bB.r])
                    act(B["rn"].t[:], bB.t[:, TT:2 * TT], AF.Sqrt, [], [bB.r, B["rn"].r], bias=LNX_EPS, scale=1.0 / 64)
                    P.op("dve", lambda e: e.reciprocal(B["rn"].t[:], B["rn"].t[:]), [], [B["rn"].r])
                    yield
                    stt("dve", B["d"].t[:], B["d"].t[:], vD("lnx_w%d" % i, c), B["rn"].t[:], ALU.mult, ALU.mult, [B["rn"].r, vt.r], [B["d"].r])
                    stt("dve", B["d"].t[:], B["d"].t[:], vD("lnx_b%d" % i, c), B["bonus"].t[:], ALU.add, ALU.add, [B["bonus"].r, vt.r], [B["d"].r])
                    tt("pool", z.t[:, c, :], B["d"].t[:], B["g"].t[:], ALU.mult, [B["d"].r, B["g"].r], [z.r])
                    yield

                for it in range(T // TT):
                    t0 = it * TT
                    P.dma(xt.t[:], xtile(src, t0, TT), reads=rx(t0, TT), writes=[xt.r])
                    rms_stats(sq, xt.t[:], 8, TT, D, NORM_EPS, rstd, [xt.r], PS[3])
                    for c in range(8):
                        stt("dve", hb.t[:, c, 1:TT + 1], xt.t[:, c, :], vD("ng%d_0" % layer, c), rstd.t[:, :],
                            ALU.mult, ALU.mult, [xt.r, rstd.r, vt.r], [hb.r])
                    tt("pool", xx.t[:], hb.t[:, :, 0:TT], hb.t[:, :, 1:TT + 1], ALU.subtract, [hb.r], [xx.r])
                    for n in range(6):
                        for c in range(8):
                            stt("dve", xsn[n].t[:, c, :], xx.t[:, c, :], vD("mu%d_%d" % (i, n), c),
                                hb.t[:, c, 1:TT + 1], ALU.mult, ALU.add, [xx.r, hb.r, vt.r], [xsn[n].r])
                    for kc in range(8):
                        mm(PS[0].t[0:64, 0:TT], w1.t[:, kc, :], xsn[3].t[:, kc, :], kc == 0, kc == 7, [w1.r, xsn[3].r], [PS[0].r])
                    act(tw.t[:], PS[0].t[0:64, 0:TT], AF.Tanh, [], [PS[0].r, tw.r])
                    for kc in range(8):
                        mm(PS[1].t[0:64, 0:TT], a1.t[:, kc, :], xsn[4].t[:, kc, :], kc == 0, kc == 7, [a1.r, xsn[4].r], [PS[1].r])
                    cp("act", ta.t[:], PS[1].t[0:64, 0:TT], [], [PS[1].r, ta.r])
                    for kc in range(8):
                        mm(PS[2].t[:, 0:TT], g1.t[:, kc, 0:128], xsn[5].t[:, kc, :], kc == 0, kc == 7, [g1.r, xsn[5].r], [PS[2].r])
                    for kc in range(8):
                        mm(PS[2].t[0:32, TT:2 * TT], g1.t[:, kc, 128:160], xsn[5].t[:, kc, :], kc == 0, kc == 7, [g1.r, xsn[5].r], [PS[2].r])
                    act(tg.t[:, 0, :], PS[2].t[:, 0:TT], AF.Sigmoid, [], [PS[2].r, tg.r])
                    act(tg.t[0:32, 1, :], PS[2].t[0:32, TT:2 * TT], AF.Sigmoid, [], [PS[2].r, tg.r])

                    run_jobs((job(c, sets[c % 2]) for c in range(8)), 2, 38)

                    for m in range(8):
                        bk = PS[m % 4]
                        for kc in range(8):
                            mm(bk.t[:, 0:TT], wo.t[:, kc, m * 128:(m + 1) * 128], z.t[:, kc, :], kc == 0, kc == 7, [wo.r, z.r], [bk.r])
                        cp("act", msb.t[:, m, :], bk.t[:, 0:TT], [], [bk.r, msb.r])
                    rms_stats(sq, msb.t[:], 8, TT, D, NORM_EPS, rstd, [msb.r], PS[4])
                    for c in range(8):
                        stt("dve", msb.t[:, c, :], msb.t[:, c, :], vD("ng%d_1" % layer, c), rstd.t[:, :], ALU.mult, ALU.mult,
                            [rstd.r, vt.r], [msb.r])
                    tt("pool", msb.t[:], msb.t[:], xt.t[:], ALU.add, [xt.r], [msb.r])
                    P.dma(xtile(dst, t0, TT), msb.t[:], reads=[msb.r], writes=rx(t0, TT))
                    cp("pool", hb.t[:, :, 0:1], hb.t[:, :, TT:TT + 1], [], [hb.r])
                P.barrier()

        def ffn_layer(layer, src, dst):
            TT = 512
            with ExitStack() as st:
                win = sbt(st, "win", [128, 8, 2 * FF], BF16)
                stg.extend([sbt(st, "stgx%d" % k, [128, 1024], F32) for k in range(4)])
                load_wk(win, w_in_d[layer], 8, 2 * FF)
                del stg[2:]
                xts = [sbt(st, "xt%d" % k, [128, 8, TT], F32) for k in range(2)]
                xns = [sbt(st, "xn%d" % k, [128, 8, TT], BF16) for k in range(2)]
                sq = sbt(st, "sq", [128, 8, TT], BF16)
                rstd = sbt(st, "rstd", [128, TT], F32)
                carry = sbt(st, "carry", [128, NF, 2], F32)
                gbuf = [sbt(st, "gbuf%d" % k, [128, TT + 2], F32) for k in range(2)]
                acc = [sbt(st, "acc%d" % k, [128, TT], F32) for k in range(2)]
                gl = [sbt(st, "gl%d" % k, [128, TT], F32) for k in range(2)]
                hT = [sbt(st, "hT%d" % k, [128, TT], BF16) for k in range(2)]
                P.op("pool", lambda e: e.memset(carry.t[:], 0.0), [], [carry.r])

                def norm_in(it):
                    t0 = it * TT
                    xt, xn = xts[it % 2], xns[it % 2]
                    P.dma(xt.t[:], xtile(src, t0, TT), reads=rx(t0, TT), writes=[xt.r])
                    rms_stats(sq, xt.t[:], 8, TT, D, NORM_EPS, rstd, [xt.r], PS[7])
                    for c in range(8):
                        stt("dve", xn.t[:, c, :], xt.t[:, c, :], vD("ng%d_2" % layer, c), rstd.t[:, :], ALU.mult, ALU.mult,
                            [xt.r, rstd.r, vt.r], [xn.r])

                norm_in(0)
                for it in range(T // TT):
                    t0 = it * TT
                    xn = xns[it % 2]
                    for f in range(NF):
                        if f == 12 and it + 1 < T // TT:
                            norm_in(it + 1)
                        k2 = f % 2
                        gb, ub = PS[k2], PS[2 + k2]
                        for kc in range(8):
                            mm(gb.t[:, :], win.t[:, kc, f * 128:(f + 1) * 128], xn.t[:, kc, :], kc == 0, kc == 7,
                               [win.r, xn.r], [gb.r])
                        for kc in range(8):
                            mm(ub.t[:, :], win.t[:, kc, FF + f * 128:FF + (f + 1) * 128], xn.t[:, kc, :], kc == 0, kc == 7,
                               [win.r, xn.r], [ub.r])
                        G = gbuf[k2]
                        cp("pool", G.t[:, 0:2], carry.t[:, f, :], [carry.r], [G.r])
                        cp("act", G.t[:, 2:TT + 2], gb.t[:, :], [], [gb.r, G.r])
                        cp("pool", carry.t[:, f, :], G.t[:, TT:TT + 2], [G.r], [carry.r])
                        A = acc[k2]
                        ts("dve", A.t[:], G.t[:, 2:TT + 2], vF(layer, 2, f), vF(layer, 3, f), ALU.mult, ALU.add,
                           [G.r, vt.r], [A.r])
                        stt("dve", A.t[:], G.t[:, 1:TT + 1], vF(layer, 1, f), A.t[:], ALU.mult, ALU.add, [G.r, vt.r], [A.r])
                        stt("dve", A.t[:], G.t[:, 0:TT], vF(layer, 0, f), A.t[:], ALU.mult, ALU.add, [G.r, vt.r], [A.r])
                        act(gl[k2].t[:], A.t[:], AF.Gelu_apprx_tanh, [A.r], [gl[k2].r])
                        tt("dve", hT[k2].t[:], ub.t[:, :], gl[k2].t[:], ALU.mult, [gl[k2].r], [ub.r, hT[k2].r])
                        P.dma(hscr[f * 128:(f + 1) * 128, t0:t0 + TT], hT[k2].t[:], reads=[hT[k2].r], writes=[Rh[it]])
                P.barrier()
            with ExitStack() as st:
                wout = sbt(st, "wout", [128, NF, D], BF16)
                stg.extend([sbt(st, "stgy%d" % k, [128, 1024], F32) for k in range(4)])
                load_wk(wout, w_out_d[layer], NF, D)
                del stg[2:]
                xt = sbt(st, "xt", [128, 8, TT], F32)
                ht = [sbt(st, "ht%d" % k, [128, NF, TT], BF16) for k in range(2)]
                msb = sbt(st, "msb", [128, 8, TT], F32)
                sq = sbt(st, "sq", [128, 8, TT], BF16)
                rstd = sbt(st, "rstd", [128, TT], F32)
                for it in range(T // TT):
                    t0 = it * TT
                    hh = ht[it % 2]
                    P.dma(hh.t[:], hscr.rearrange("(f p) t -> p f t", p=128)[:, :, t0:t0 + TT], reads=[Rh[it]], writes=[hh.r])
                    P.dma(xt.t[:], xtile(src, t0, TT), reads=rx(t0, TT), writes=[xt.r])
                    for m in range(8):
                        bk = PS[m % 4]
                        for f in range(NF):
                            mm(bk.t[:, :], wout.t[:, f, m * 128:(m + 1) * 128], hh.t[:, f, :], f == 0, f == NF - 1,
                               [wout.r, hh.r], [bk.r])
                        cp("act", msb.t[:, m, :], bk.t[:, :], [], [bk.r, msb.r])
                    rms_stats(sq, msb.t[:], 8, TT, D, NORM_EPS, rstd, [msb.r], PS[7])
                    for c in range(8):
                        stt("dve", msb.t[:, c, :], msb.t[:, c, :], vD("ng%d_3" % layer, c), rstd.t[:, :], ALU.mult, ALU.mult,
                            [rstd.r, vt.r], [msb.r])
                    tt("pool", msb.t[:], msb.t[:], xt.t[:], ALU.add, [xt.r], [msb.r])
                    P.dma(xtile(dst, t0, TT), msb.t[:], reads=[msb.r], writes=rx(t0, TT))
                P.barrier()

        def kv_prep(src):
            TT = 512
            with ExitStack() as st:
                kd = sbt(st, "kd", [128, 8, 288], BF16)
                kds = sbt(st, "kds", [128, 8, 32], BF16)
                kuk = sbt(st, "kuk", [128, 2, H * 64], BF16)
                kuv = sbt(st, "kuv", [128, 2, H * 64], BF16)
                load_wk(kd, kd_d, 8, 288)
                load_wk(kds, kds_d, 8, 32)
                load_wk(kuk, kuk_d, 2, H * 64)
                load_wk(kuv, kuv_d, 2, H * 64)
                xt = sbt(st, "xt", [128, 8, TT], F32)
                xn = sbt(st, "xn", [128, 8, TT], BF16)
                sq = sbt(st, "sq", [128, 8, TT], BF16)
                rstd = sbt(st, "rstd", [128, TT], F32)
                ckv = sbt(st, "ckv", [128, 2, TT], F32)
                ckvn = sbt(st, "ckvn", [128, 2, TT], BF16)
                rp = sbt(st, "rp", [128, 2, TT], F32)
                t1 = sbt(st, "t1", [128, TT], F32)
                t2 = sbt(st, "t2", [128, TT], F32)
                kr = sbt(st, "kr", [128, TT], BF16)
                KT = [sbt(st, "KT%d" % k, [128, TT], BF16) for k in range(2)]
                Vt = [sbt(st, "Vt%d" % k, [128, H, 128], BF16) for k in range(2)]
                for k in range(2):
                    P.op("pool", lambda e, k=k: e.memset(Vt[k].t[:, :, 64:128], 1.0), [], [Vt[k].r])
                R = slice(64, 96)
                for it in range(T // TT):
                    t0 = it * TT
                    P.dma(xt.t[:], xtile(src, t0, TT), reads=rx(t0, TT), writes=[xt.r])
                    P.dma(rp.t[R, :, :], rope_d[R, :, t0:t0 + TT], reads=[Rin], writes=[rp.r])
                    rms_stats(sq, xt.t[:], 8, TT, D, NORM_EPS, rstd, [xt.r], PS[7])
                    for c in range(8):
                        stt(ew(), xn.t[:, c, :], xt.t[:, c, :], vD("kvng", c), rstd.t[:, :], ALU.mult, ALU.mult,
                            [xt.r, rstd.r, vt.r], [xn.r])
                    for j in range(2):
                        for kc in range(8):
                            mm(PS[j].t[:, :], kd.t[:, kc, j * 128:(j + 1) * 128], xn.t[:, kc, :], kc == 0, kc == 7,
                               [kd.r, xn.r], [PS[j].r])
                        cp("act", ckv.t[:, j, :], PS[j].t[:, :], [], [PS[j].r, ckv.r])
                    for kc in range(8):
                        mm(PS[2].t[R, :], kd.t[:, kc, 256:288], xn.t[:, kc, :], kc == 0, kc == 7, [kd.r, xn.r], [PS[2].r])
                    for kc in range(8):
                        mm(PS[3].t[R, :], kds.t[:, kc, :], xn.t[:, kc, :], kc == 0, kc == 7, [kds.r, xn.r], [PS[3].r])
                    tt("dve", t1.t[R, :], PS[2].t[R, :], rp.t[R, 0, :], ALU.mult, [rp.r], [PS[2].r, t1.r])
                    tt("dve", t2.t[R, :], PS[3].t[R, :], rp.t[R, 1, :], ALU.mult, [rp.r], [PS[3].r, t2.r])
                    tt("dve", kr.t[R, :], t1.t[R, :], t2.t[R, :], ALU.add, [t1.r, t2.r], [kr.r])
                    rms_stats(sq, ckv.t[:], 2, TT, KVL, NORM_EPS, rstd, [ckv.r], PS[7])
                    for c in range(2):
                        stt("dve", ckvn.t[:, c, :], ckv.t[:, c, :], vK(c), rstd.t[:, :], ALU.mult, ALU.mult,
                            [ckv.r, rstd.r, vt.r], [ckvn.r])
                    for h in range(H):
                        K = KT[h % 2]
                        bk = PS[h % 2]
                        for kc in range(2):
                            mm(bk.t[0:64, :], kuk.t[:, kc, h * 64:(h + 1) * 64], ckvn.t[:, kc, :], kc == 0, kc == 1,
                               [kuk.r, ckvn.r], [bk.r])
                        cp("act", K.t[0:64, :], bk.t[0:64, :], [], [bk.r, K.r])
                        cp("pool", K.t[R, :], kr.t[R, :], [kr.r], [K.r])
                        P.dma(kscr[h, :, t0:t0 + TT], K.t[0:96, :], reads=[K.r], writes=[Rkv[it]])
                    for tb in range(TT // 128):
                        V = Vt[tb % 2]
                        for hf in range(2):
                            bk = PS[4 + hf]
                            for kc in range(2):
                                mm(bk.t[:, :], ckvn.t[:, kc, tb * 128:(tb + 1) * 128], kuv.t[:, kc, hf * 512:(hf + 1) * 512],
                                   kc == 0, kc == 1, [kuv.r, ckvn.r], [bk.r])
                            cp("act" if hf == 0 else "dve", V.t[:, hf * 8:(hf + 1) * 8, 0:64],
                               bk.t[:, :].rearrange("p (h d) -> p h d", d=64), [], [bk.r, V.r])
                        P.dma(vscr[:, t0 + tb * 128:t0 + (tb + 1) * 128, :].rearrange("h t d -> t h d"), V.t[:],
                              reads=[V.r], writes=[Rkv[it]])
                P.barrier()

        def mla_layer(j, layer, src, dst):
            TT = 512
            with ExitStack() as st:
                qd = sbt(st, "qd", [128, 8, QL], BF16)
                qu = sbt(st, "qu", [128, 3, H * 96], BF16)
                qus = sbt(st, "qus", [128, 3, H * 32], BF16)
                ow = sbt(st, "ow", [64, H, D], BF16)
                load_wk(qd, qd_d[j], 8, QL)
                load_wk(qu, qu_d[j], 3, H * 96)
                load_wk(qus, qus_d[j], 3, H * 32)
                for h in range(H):
                    load_w(lambda r, c0, cw, h=h: ow.t[0:r, h, c0:c0 + cw], ow.r, ow_d[j, h * 64:(h + 1) * 64, :], 64, D)
                xt = sbt(st, "xt", [128, 8, TT], F32)
                xn = sbt(st, "xn", [128, 8, TT], BF16)
                sq = sbt(st, "sq", [128, 8, TT], BF16)
                rstd = sbt(st, "rstd", [128, TT], F32)
                cq = sbt(st, "cq", [128, 3, TT], F32)
                cqn = sbt(st, "cqn", [128, 3, TT], BF16)
                rp = sbt(st, "rp", [128, 2, TT], F32)
                t1 = sbt(st, "t1", [128, TT], F32)
                t2 = sbt(st, "t2", [128, TT], F32)
                QT = sbt(st, "QT", [128, H, TT], BF16)
                OT = sbt(st, "OT", [64, H, TT], BF16)
                KT = [sbt(st, "KT%d" % k, [128, T], BF16) for k in range(2)]
                VT = [sbt(st, "VT%d" % k, [128, T // 128, 128], BF16) for k in range(2)]
                PT = [sbt(st, "PT%d" % k, [128, TT], BF16) for k in range(3)]
                rec = sbt(st, "rec", [64, TT], F32)
                msb = sbt(st, "msb", [128, 8, TT], F32)
                R = slice(64, 96)
                pi = 0
                for it in range(T // TT):
                    t0 = it * TT
                    P.dma(xt.t[:], xtile(src, t0, TT), reads=rx(t0, TT), writes=[xt.r])
                    P.dma(rp.t[R, :, :], rope_d[R, :, t0:t0 + TT], reads=[Rin], writes=[rp.r])
                    rms_stats(sq, xt.t[:], 8, TT, D, NORM_EPS, rstd, [xt.r], PS[2])
                    for c in range(8):
                        stt(ew(), xn.t[:, c, :], xt.t[:, c, :], vD("ng%d_0" % layer, c), rstd.t[:, :], ALU.mult, ALU.mult,
                            [xt.r, rstd.r, vt.r], [xn.r])
                    for m in range(3):
                        bk = PS[m % 2]
                        for kc in range(8):
                            mm(bk.t[:, :], qd.t[:, kc, m * 128:(m + 1) * 128], xn.t[:, kc, :], kc == 0, kc == 7,
                               [qd.r, xn.r], [bk.r])
                        cp("act", cq.t[:, m, :], bk.t[:, :], [], [bk.r, cq.r])
                    rms_stats(sq, cq.t[:], 3, TT, QL, NORM_EPS, rstd, [cq.r], PS[2])
                    for c in range(3):
                        stt("dve", cqn.t[:, c, :], cq.t[:, c, :], vQ(j, c), rstd.t[:, :], ALU.mult, ALU.mult,
                            [cq.r, rstd.r, vt.r], [cqn.r])
                    for h in range(H):
                        A = PS[h % 2]
                        Bk = PS[2]
                        for kc in range(3):
                            mm(A.t[0:96, :], qu.t[:, kc, h * 96:(h + 1) * 96], cqn.t[:, kc, :], kc == 0, kc == 2,
                               [qu.r, cqn.r], [A.r])
                        for kc in range(3):
                            mm(Bk.t[R, :], qus.t[:, kc, h * 32:(h + 1) * 32], cqn.t[:, kc, :], kc == 0, kc == 2,
                               [qus.r, cqn.r], [Bk.r])
                        act(QT.t[0:64, h, :], A.t[0:64, :], AF.Copy, [], [A.r, QT.r], scale=SCALE)
                        stt("dve", t1.t[R, :], A.t[R, :], SCALE, rp.t[R, 0, :], ALU.mult, ALU.mult, [rp.r], [A.r, t1.r])
                        stt("dve", t2.t[R, :], Bk.t[R, :], SCALE, rp.t[R, 1, :], ALU.mult, ALU.mult, [rp.r], [Bk.r, t2.r])
                        tt("dve", QT.t[R, h, :], t1.t[R, :], t2.t[R, :], ALU.add, [t1.r, t2.r], [QT.r])
                    nkb = (it + 1) * 4
                    nk = nkb * 128
                    for h in range(H):
                        K = KT[h % 2]
                        V = VT[h % 2]
                        P.dma(K.t[0:96, 0:nk], kscr[h, :, 0:nk], reads=Rkv[0:it + 1], writes=[K.r])
                        P.dma(V.t[:, 0:nkb, :], vscr[h, 0:nk, :].rearrange("(kb p) d -> p kb d", p=128),
                              reads=Rkv[0:it + 1], writes=[V.r])
                        Ob = PS[6 + h % 2]
                        pts = {}

                        def qk(kb, h=h, K=K):
                            nonlocal pi
                            jd = kb - it * 4
                            c0 = max(jd, 0) * 128
                            Sb_ = PS[3 + pi % 3]
                            Pt = PT[pi % 3]
                            pi += 1
                            mm(Sb_.t[:, c0:TT], K.t[0:96, kb * 128:(kb + 1) * 128], QT.t[0:96, h, c0:TT], True, True,
                               [K.r, QT.r], [Sb_.r])
                            act(Pt.t[:, c0:TT], Sb_.t[:, c0:TT], AF.Exp, [], [Sb_.r, Pt.r])
                            if jd >= 0:
                                P.op("pool", lambda e, Pt=Pt, c0=c0: e.memset(Pt.t[64:128, c0:c0 + 64], 0.0), [], [Pt.r])
                            pts[kb] = (Pt, c0)

                        def pv(kb, V=V, Ob=Ob):
                            Pt, c0 = pts.pop(kb)
                            mm(Ob.t[:, c0:TT], V.t[:, kb, :], Pt.t[:, c0:TT], kb == 0, kb == nkb - 1, [V.r, Pt.r], [Ob.r])

                        LA = 2
                        for kb in range(min(LA, nkb)):
                            qk(kb)
                        for kb in range(nkb):
                            if kb + LA < nkb:
                                qk(kb + LA)
                            pv(kb)
                        P.op("dve", lambda e, Ob=Ob: e.reciprocal(rec.t[:, :], Ob.t[64:128, :]), [], [Ob.r, rec.r])
                        tt("dve", OT.t[:, h, :], Ob.t[0:64, :], rec.t[:, :], ALU.mult, [rec.r], [Ob.r, OT.r])
                    for m in range(8):
                        bk = PS[m % 2]
                        for h in range(H):
                            mm(bk.t[:, :], ow.t[0:64, h, m * 128:(m + 1) * 128], OT.t[0:64, h, :], h == 0, h == H - 1,
                               [ow.r, OT.r], [bk.r])
                        cp("act", msb.t[:, m, :], bk.t[:, :], [], [bk.r, msb.r])
                    rms_stats(sq, msb.t[:], 8, TT, D, NORM_EPS, rstd, [msb.r], PS[2])
                    for c in range(8):
                        stt("dve", msb.t[:, c, :], msb.t[:, c, :], vD("ng%d_1" % layer, c), rstd.t[:, :], ALU.mult, ALU.mult,
                            [rstd.r, vt.r], [msb.r])
                    tt("pool", msb.t[:], msb.t[:], xt.t[:], ALU.add, [xt.r], [msb.r])
                    P.dma(xtile(dst, t0, TT), msb.t[:], reads=[msb.r], writes=rx(t0, TT))
                P.barrier()

        P.barrier()
        cur = xT
        for layer in range(depth):
            if layer < NA:
                rwkv_layer(layer, layer, cur, xs)
            else:
                if layer == NA:
                    kv_prep(xs)
                mla_layer(layer - NA, layer, xs, xs)
            cur = xs
            ffn_layer(layer, xs, y if layer == depth - 1 else xs)
        P.barrier()
        P.emit()
        nc._n_ops = P.n
    return nc


NVT = [0, 0, 0]
_CACHE = {}


def prep_inputs(inputs):
    inp = {k: np.asarray(v) for k, v in inputs.items()}
    B, T, _ = inp["x"].shape
    vt, nd, nf = _pack_tables(inp)
    NVT[0], NVT[1], NVT[2] = vt.shape[1], nd, nf
    f32 = lambda a: np.ascontiguousarray(a, dtype=np.float32)
    kd = inp["kv_w_down"]
    kds = np.concatenate([kd[:, 272:288], kd[:, 256:272]], 1)
    ku = inp["kv_w_up"].reshape(KVL, H, 128)
    qu = inp["q_w_up"].reshape(2, QL, H, 96)
    qus = np.concatenate([qu[..., 80:96], qu[..., 64:80]], -1).reshape(2, QL, H * 32)
    shared = {
        "vt": vt, "cst": _consts(), "rope": _rope(T),
        "ffn_w_in": f32(inp["ffn_w_in"]), "ffn_w_out": f32(inp["ffn_w_out"]),
        "a_w_rkv": f32(inp["a_w_rkv"]), "a_w1": f32(inp["a_w1"]), "a_w2": f32(inp["a_w2"]),
        "a_a1": f32(inp["a_a1"]), "a_a2": f32(inp["a_a2"]), "a_g1": f32(inp["a_g1"]), "a_g2": f32(inp["a_g2"]),
        "a_w_o": f32(inp["a_w_o"]), "kv_w_down": f32(kd), "kv_w_down_sw": f32(kds),
        "kv_w_up_k": f32(ku[:, :, 0:64].reshape(KVL, H * 64)), "kv_w_up_v": f32(ku[:, :, 64:128].reshape(KVL, H * 64)),
        "q_w_down": f32(inp["q_w_down"]), "q_w_up": f32(inp["q_w_up"]), "q_w_up_sw": f32(qus),
        "o_w": f32(inp["o_w"]),
    }
    maps = []
    for b in range(B):
        m = dict(shared)
        m["xT"] = f32(inp["x"][b].T)
        maps.append(m)
    return maps, B, T


def kernel(**inputs):
    maps, B, T = prep_inputs(inputs)
    key = (T, DEPTH)
    if key not in _CACHE:
        _CACHE[key] = build(T)
    nc = _CACHE[key]
    res = run_bass_kernel_spmd(nc, maps, core_ids=list(range(B)))
    out = np.stack([np.asarray(r["y"]).T for r in res.results], 0)
    return np.ascontiguousarray(out.astype(np.float32))
```

```python
import math
from contextlib import ExitStack
import numpy as np
import concourse.bass as bass
import concourse.mybir as mybir
from concourse.bass_utils import run_bass_kernel_spmd

F32 = mybir.dt.float32
BF16 = mybir.dt.bfloat16
AF = mybir.ActivationFunctionType
ALU = mybir.AluOpType

D = 1024
DEPTH = 4
NA = 2
H = 16
FF = 2816
NF = FF // 128
QL = 384
KVL = 256
LNX_EPS = 64e-5
NORM_EPS = 1e-6
SCALE = 1.0 / math.sqrt(96.0)

COMPUTE = ("pe", "act", "dve", "pool")
SEM_LIMIT = 30000
NDMA_SEM = 24


class Res:
    __slots__ = ("name", "w", "r")

    def __init__(self, name=""):
        self.name = name
        self.w = None
        self.r = {}


class Tl:
    __slots__ = ("t", "r")

    def __init__(self, t, r):
        self.t = t
        self.r = r


class Prog:
    def __init__(self, nc, stack):
        self.nc = nc
        self.stack = stack
        self.streams = {e: [] for e in COMPUTE + ("sp",)}
        self.sems = {}
        self.cnt = {}
        for e in COMPUTE:
            self.sems[e] = [self._newsem(e + "0")]
            self.cnt[e] = (0, 0)
        self.dma_sems = [self._newsem("dma%d" % i) for i in range(NDMA_SEM)]
        self.dma_cnt = [0] * NDMA_SEM
        self.dma_i = 0
        self.seen = {e: {} for e in self.streams}
        self.n = 0

    def _newsem(self, name):
        return self.stack.enter_context(self.nc.semaphore(name))

    def _semof(self, key, idx):
        if isinstance(key, tuple):
            return self.dma_sems[key[1]]
        return self.sems[key][idx]

    @staticmethod
    def _need(waits, tok):
        if tok is None:
            return
        key, idx, val = tok
        cur = waits.get(key)
        if cur is None or (idx, val) > cur:
            waits[key] = (idx, val)

    def _collect(self, q, reads, writes, eng):
        waits = {}
        for r in reads:
            self._need(waits, r.w)
        for w in writes:
            self._need(waits, w.w)
            for k, (i, v) in w.r.items():
                if k == eng:
                    continue
                self._need(waits, (k, i, v))
        if eng == "pe":
            waits.pop("pe", None)
        out = []
        for key, (idx, val) in waits.items():
            s = self.seen[q].get(key)
            if s is not None and s >= (idx, val):
                continue
            self.seen[q][key] = (idx, val)
            out.append((self._semof(key, idx), val))
        return out

    def op(self, eng, fn, reads=(), writes=()):
        waits = self._collect(eng, reads, writes, eng)
        idx, val = self.cnt[eng]
        if val >= SEM_LIMIT:
            idx += 1
            val = 0
            self.sems[eng].append(self._newsem("%s%d" % (eng, idx)))
        val += 1
        self.cnt[eng] = (idx, val)
        self.streams[eng].append((waits, fn, (self.sems[eng][idx], 1)))
        tok = (eng, idx, val)
        for r in reads:
            r.r[eng] = (idx, val)
        for w in writes:
            w.w = tok
            w.r = {}
        self.n += 1
        return tok

    def dma(self, out, in_, reads=(), writes=(), q="sp"):
        waits = self._collect(q, reads, writes, None)
        j = self.dma_i % NDMA_SEM
        self.dma_i += 1
        self.dma_cnt[j] += 16
        val = self.dma_cnt[j]
        key = ("dma", j)
        self.streams[q].append(
            (waits, lambda e: e.dma_start(out=out, in_=in_), (self.dma_sems[j], 16)))
        tok = (key, 0, val)
        for r in reads:
            r.r[key] = (0, val)
        for w in writes:
            w.w = tok
            w.r = {}
        self.n += 1
        return tok

    def barrier(self):
        for q in self.streams:
            waits = []
            for e in COMPUTE:
                if e == q:
                    continue
                idx, val = self.cnt[e]
                if val == 0:
                    continue
                sn = self.seen[q].get(e)
                if sn is not None and sn >= (idx, val):
                    continue
                self.seen[q][e] = (idx, val)
                waits.append((self.sems[e][idx], val))
            for j in range(NDMA_SEM):
                val = self.dma_cnt[j]
                if val == 0:
                    continue
                key = ("dma", j)
                sn = self.seen[q].get(key)
                if sn is not None and sn >= (0, val):
                    continue
                self.seen[q][key] = (0, val)
                waits.append((self.dma_sems[j], val))
            self.streams[q].append((waits, None, None))

    def emit(self):
        nc = self.nc
        with nc.Block() as block:
            def run(stream):
                def body(e):
                    for waits, fn, inc in stream:
                        for s, v in waits:
                            e.wait_ge(s, v)
                        if fn is not None:
                            ins = fn(e)
                            if inc is not None:
                                ins.then_inc(inc[0], inc[1])
                return body
            block.tensor(run(self.streams["pe"]))
            block.scalar(run(self.streams["act"]))
            block.vector(run(self.streams["dve"]))
            block.gpsimd(run(self.streams["pool"]))
            block.sync(run(self.streams["sp"]))


VD = {}


def _pack_tables(inp):
    vecs = []

    def add(name, v):
        VD[name] = len(vecs)
        vecs.append(np.asarray(v, np.float32).reshape(D))

    for l in range(DEPTH):
        for j in range(4):
            add("ng%d_%d" % (l, j), inp["norm_g"][l, j])
    for i in range(NA):
        for n in range(6):
            add("mu%d_%d" % (i, n), inp["a_mu"][i, n])
        for nm in ("w0", "a0", "k_k", "k_a", "r_k", "lnx_w", "lnx_b"):
            add("%s%d" % (nm, i), inp["a_" + nm][i])
    add("kvng", inp["kv_norm_g"])
    tabD = np.stack(vecs, 0).reshape(len(vecs), 8, 128).transpose(2, 0, 1).reshape(128, -1)
    fv = []
    for l in range(DEPTH):
        for j in range(3):
            fv.append(inp["ffn_conv_w"][l, j])
        fv.append(inp["ffn_conv_b"][l])
    tabF = np.stack(fv, 0).reshape(len(fv), NF, 128).transpose(2, 0, 1).reshape(128, -1)
    qg = np.asarray(inp["q_norm_g"]).reshape(2, 3, 128).transpose(2, 0, 1).reshape(128, 6)
    kg = np.asarray(inp["kv_a_norm_g"]).reshape(2, 128).T
    vt = np.ascontiguousarray(np.concatenate([tabD, tabF, qg, kg], 1).astype(np.float32))
    return vt, tabD.shape[1], tabF.shape[1]


def _consts():
    p = np.arange(128)
    ident = np.eye(128, dtype=np.float32)
    bd = (p[:, None] // 64 == p[None, :] // 64).astype(np.float32)
    su = (p[:, None] < p[None, :]).astype(np.float32)
    ui = (p[:, None] <= p[None, :]).astype(np.float32)
    low = (p[None, :] < p[:, None]).astype(np.float32)
    ones = np.ones((128, 128), np.float32)
    return np.ascontiguousarray(np.concatenate([ident, bd, su, ui, su, ui, low, ones], 1))


C_ID, C_BD, C_M4, C_LOW, C_ONE = 0, 128, 256, 768, 896
NCST = 1024


def _rope(T):
    inv = 1.0 / (10000.0 ** (np.arange(0, 32, 2, dtype=np.float32) / 32.0))
    ang = np.arange(T, dtype=np.float32)[:, None] * inv[None, :].astype(np.float32)
    cos = np.cos(ang).astype(np.float32).T
    sin = np.sin(ang).astype(np.float32).T
    tab = np.zeros((128, 2, T), np.float32)
    tab[64:80, 0] = cos
    tab[80:96, 0] = cos
    tab[64:80, 1] = -sin
    tab[80:96, 1] = sin
    return tab


def build(T, depth=DEPTH, dbg=False):
    nc = bass.Bass("TRN2", target_bir_lowering=False)
    NT5 = T // 512
    NT2 = T // 256

    def din(name, shape):
        return nc.dram_tensor(name, list(shape), F32, kind="ExternalInput").ap()

    xT = din("xT", [D, T])
    vt_d = din("vt", [128, NVT[0]])
    cst_d = din("cst", [128, NCST])
    rope_d = din("rope", [128, 2, T])
    w_in_d = din("ffn_w_in", [DEPTH, D, 2 * FF])
    w_out_d = din("ffn_w_out", [DEPTH, FF, D])
    wrkv_d = din("a_w_rkv", [NA, 3, D, D])
    w1_d = din("a_w1", [NA, D, 64])
    w2_d = din("a_w2", [NA, 64, D])
    a1_d = din("a_a1", [NA, D, 64])
    a2_d = din("a_a2", [NA, 64, D])
    g1_d = din("a_g1", [NA, D, 160])
    g2_d = din("a_g2", [NA, 160, D])
    wo_d = din("a_w_o", [NA, D, D])
    kd_d = din("kv_w_down", [D, 288])
    kds_d = din("kv_w_down_sw", [D, 32])
    kuk_d = din("kv_w_up_k", [KVL, H * 64])
    kuv_d = din("kv_w_up_v", [KVL, H * 64])
    qd_d = din("q_w_down", [2, D, QL])
    qu_d = din("q_w_up", [2, QL, H * 96])
    qus_d = din("q_w_up_sw", [2, QL, H * 32])
    ow_d = din("o_w", [2, D, D])
    y = nc.dram_tensor("y", [D, T], F32, kind="ExternalOutput").ap()
    xs = nc.dram_tensor("xs", [D, T], F32).ap()
    hscr = nc.dram_tensor("hscr", [T // 512, 128, NF, 512], BF16).ap()
    kscr = nc.dram_tensor("kscr", [H, 96, T], BF16).ap()
    vscr = nc.dram_tensor("vscr", [H, T, 128], BF16).ap()

    Rx = [Res("x%d" % i) for i in range(NT2)]
    Rh = [Res("h%d" % i) for i in range(NT5)]
    Rkv = [Res("kv%d" % i) for i in range(NT5)]
    Rin = Res("in")

    with ExitStack() as top:
        P = Prog(nc, top)
        top.enter_context(nc.allow_low_precision("bf16 matmul operands, fp32 accumulate"))

        uid = [0]

        def sbt(st, name, shape, dt):
            uid[0] += 1
            nm = "sb%d_%s" % (uid[0], name)
            return Tl(st.enter_context(nc.sbuf_tensor(nm, list(shape), dt)), Res(nm))

        PS = [Tl(top.enter_context(nc.psum_tensor("ps%d" % i, [128, 512], F32)), Res("ps%d" % i))
              for i in range(8)]
        vt = sbt(top, "vt", [128, NVT[0]], F32)
        cst = sbt(top, "cst", [128, NCST], F32)
        onesb = sbt(top, "onesb", [128, 128], BF16)
        stg = [sbt(top, "stg%d" % i, [128, 1024], F32) for i in range(2)]
        stg_i = [0]
        P.dma(vt.t[:], vt_d, writes=[vt.r])
        P.dma(cst.t[:], cst_d, writes=[cst.r])
        P.op("dve", lambda e: e.tensor_copy(onesb.t[:], cst.t[:, C_ONE:C_ONE + 128]), [cst.r], [onesb.r])
        identb_t = sbt(top, "identb", [128, 128], BF16)
        P.op("dve", lambda e: e.tensor_copy(identb_t.t[:], cst.t[:, C_ID:C_ID + 128]), [cst.r], [identb_t.r])
        identb = identb_t.t[:, :]
        bdb_t = sbt(top, "bdb", [128, 128], BF16)
        P.op("dve", lambda e: e.tensor_copy(bdb_t.t[:], cst.t[:, C_BD:C_BD + 128]), [cst.r], [bdb_t.r])
        bdb = bdb_t.t[:, :]

        ident = cst.t[:, C_ID:C_ID + 128]
        bdones = cst.t[:, C_BD:C_BD + 128]
        mask4 = cst.t[:, C_M4:C_M4 + 512]
        lowm = cst.t[:, C_LOW:C_LOW + 128]
        ones = cst.t[:, C_ONE:C_ONE + 128]

        def vD(name, c):
            i = VD[name] * 8 + c
            return vt.t[:, i:i + 1]

        def vF(l, j, f):
            i = NVT[1] + (l * 4 + j) * NF + f
            return vt.t[:, i:i + 1]

        def vQ(j, c):
            i = NVT[1] + NVT[2] + j * 3 + c
            return vt.t[:, i:i + 1]

        def vK(c):
            i = NVT[1] + NVT[2] + 6 + c
            return vt.t[:, i:i + 1]

        def mm(out, lhsT, rhs, start, stop, reads, writes):
            P.op("pe", lambda e: e.matmul(out, lhsT, rhs, start=start, stop=stop), reads, writes)

        def act(out, in_, func, reads, writes, bias=None, scale=None):
            kw = {}
            if bias is not None:
                kw["bias"] = bias
            if scale is not None:
                kw["scale"] = scale
            P.op("act", lambda e: e.activation(out, in_, func, **kw), reads, writes)

        def tt(eng, out, in0, in1, op, reads, writes):
            P.op(eng, lambda e: e.tensor_tensor(out, in0, in1, op), reads, writes)

        def ts(eng, out, in0, s1, s2, op0, op1, reads, writes):
            if s2 is None:
                P.op(eng, lambda e: e.tensor_scalar(out, in0, s1, None, op0), reads, writes)
            else:
                P.op(eng, lambda e: e.tensor_scalar(out, in0, s1, s2, op0, op1), reads, writes)

        def stt(eng, out, in0, sc, in1, op0, op1, reads, writes):
            P.op("dve", lambda e: e.scalar_tensor_tensor(out, in0, sc, in1, op0, op1), reads, writes)

        def cp(eng, out, in_, reads, writes):
            if eng == "act":
                act(out, in_, AF.Copy, reads, writes)
            else:
                P.op(eng, lambda e: e.tensor_copy(out, in_), reads, writes)

        rr = [0]

        def ew():
            rr[0] += 1
            return "pool" if rr[0] % 3 == 0 else "dve"

        def load_w(view, res, src, rows, cols):
            c0 = 0
            while c0 < cols:
                cw = min(1024, cols - c0)
                s = stg[stg_i[0] % len(stg)]
                stg_i[0] += 1
                P.dma(s.t[0:rows, 0:cw], src[:, c0:c0 + cw], reads=[Rin], writes=[s.r])
                eng = "pool" if stg_i[0] % 2 == 0 else "dve"
                cp(eng, view(rows, c0, cw), s.t[0:rows, 0:cw], [s.r], [res])
                c0 += cw

        def load_wk(tile, src, nk, cols, rows_last=128):
            for k in range(nk):
                rows = rows_last if k == nk - 1 else 128
                load_w(lambda r, c0, cw, k=k: tile.t[0:r, k, c0:c0 + cw], tile.r,
                       src[k * 128:k * 128 + rows, :], rows, cols)

        def rms_stats(st_sq, src_ap, nch, TT, Dn, eps, rstd, src_reads, bank):
            act(st_sq.t[:, 0:nch, 0:TT], src_ap, AF.Square, src_reads, [st_sq.r])
            for c in range(nch):
                mm(bank.t[:, 0:TT], onesb.t[:, :], st_sq.t[:, c, 0:TT], c == 0, c == nch - 1,
                   [st_sq.r, onesb.r], [bank.r])
            act(rstd.t[:, 0:TT], bank.t[:, 0:TT], AF.Ln, [], [bank.r, rstd.r], bias=eps, scale=1.0 / Dn)
            act(rstd.t[:, 0:TT], rstd.t[:, 0:TT], AF.Exp, [], [rstd.r], scale=-0.5)

        def xtile(ap, t0, TT):
            return ap.rearrange("(c p) t -> p c t", p=128)[:, :, t0:t0 + TT]

        def rx(t0, TT):
            return Rx[t0 // 256:(t0 + TT) // 256]

        def run_jobs(gens, width, stagger):
            active = []
            it = iter(gens)
            steps0 = 0
            done = False
            while True:
                while not done and len(active) < width and (len(active) == 0 or steps0 >= stagger):
                    g = next(it, None)
                    if g is None:
                        done = True
                        break
                    active.append(g)
                    if len(active) == 1:
                        steps0 = 0
                if not active:
                    break
                for g in list(active):
                    try:
                        next(g)
                    except StopIteration:
                        active.remove(g)
                        steps0 = stagger
                steps0 += 1

        def rwkv_layer(i, layer, src, dst):
            TT = 256
            with ExitStack() as st:
                wr = sbt(st, "wr", [128, 8, D], BF16)
                wk = sbt(st, "wk", [128, 8, D], BF16)
                wv = sbt(st, "wv", [128, 8, D], BF16)
                wo = sbt(st, "wo", [128, 8, D], BF16)
                w1 = sbt(st, "w1", [128, 8, 64], BF16)
                a1 = sbt(st, "a1", [128, 8, 64], BF16)
                g1 = sbt(st, "g1", [128, 8, 160], BF16)
                w2 = sbt(st, "w2", [128, 1, D], BF16)
                a2 = sbt(st, "a2", [128, 1, D], BF16)
                g2 = sbt(st, "g2", [128, 2, D], BF16)
                for tl, srcw in ((wr, wrkv_d[i, 0]), (wk, wrkv_d[i, 1]), (wv, wrkv_d[i, 2]), (wo, wo_d[i])):
                    load_wk(tl, srcw, 8, D)
                load_wk(w1, w1_d[i], 8, 64)
                load_wk(a1, a1_d[i], 8, 64)
                load_wk(g1, g1_d[i], 8, 160)
                load_wk(w2, w2_d[i], 1, D, rows_last=64)
                load_wk(a2, a2_d[i], 1, D, rows_last=64)
                load_wk(g2, g2_d[i], 2, D, rows_last=32)

                xt = sbt(st, "xt", [128, 8, TT], F32)
                hb = sbt(st, "hb", [128, 8, TT + 1], F32)
                xsn = [sbt(st, "xs%d" % n, [128, 8, TT], BF16) for n in range(6)]
                rstd = sbt(st, "rstd", [128, TT], F32)
                tw = sbt(st, "tw", [64, TT], BF16)
                ta = sbt(st, "ta", [64, TT], BF16)
                tg = sbt(st, "tg", [128, 2, TT], BF16)
                z = sbt(st, "z", [128, 8, TT], BF16)
                sq = z
                msb = sbt(st, "msb", [128, 8, TT], F32)
                xx = msb
                S = sbt(st, "S", [128, 8, 64], F32)
                Sb = sbt(st, "Sb", [128, 8, 64], BF16)
                names = "r k v kk a lw g rn kmod b cum cex ecum bonus d".split()
                sets = []
                for k in range(2):
                    J = dict(
                        cb={nm: sbt(st, "c%d_%s" % (k, nm), [128, TT], F32) for nm in names},
                        vb=sbt(st, "vb%d" % k, [128, TT], BF16),
                        sqb=sbt(st, "sqb%d" % k, [128, TT], BF16),
                        AR=sbt(st, "AR%d" % k, [128, 2 * TT], BF16),
                        KB=sbt(st, "KB%d" % k, [128, 2 * TT], BF16),
                        TM=sbt(st, "TM%d" % k, [128, 2, 3, 128], BF16),
                        bA=PS[4 * k], bB=PS[4 * k + 1], hd=[])
                    for hh in range(2):
                        J["hd"].append(dict(
                            AMs=sbt(st, "AMs%d_%d" % (k, hh), [128, 512], BF16),
                            Ls=sbt(st, "Ls%d_%d" % (k, hh), [128, 128], BF16),
                            LM=[sbt(st, "LM%d_%d_%d" % (k, hh, q), [128, 256], BF16) for q in range(2)],
                            Tt=[sbt(st, "Tt%d_%d_%d" % (k, hh, q), [128, 128], BF16) for q in range(2)],
                            Xs=sbt(st, "Xs%d_%d" % (k, hh), [128, 64], BF16),
                            Us=sbt(st, "Us%d_%d" % (k, hh), [128, 64], BF16),
                            tmpS=sbt(st, "tmpS%d_%d" % (k, hh), [128, 64], F32),
                            WK=PS[4 * k + 2 + hh]))
                    sets.append(J)
                SR = [Res("S%d" % c) for c in range(8)]
                SbR = [Res("Sb%d" % c) for c in range(8)]
                P.op("pool", lambda e: e.memset(S.t[:], 0.0), [], SR)
                P.op("pool", lambda e: e.memset(Sb.t[:], 0.0), [], SbR)
                P.op("pool", lambda e: e.memset(hb.t[:, :, 0:1], 0.0), [], [hb.r])

                def job(c, J):
                    B = J["cb"]
                    vb, AR, KB, TM, bA, bB, hd = J["vb"], J["AR"], J["KB"], J["TM"], J["bA"], J["bB"], J["hd"]
                    sqb = J["sqb"]
                    cs = slice(c * 128, (c + 1) * 128)
                    for kc in range(8):
                        mm(bA.t[:, 0:TT], wr.t[:, kc, cs], xsn[0].t[:, kc, :], kc == 0, kc == 7, [wr.r, xsn[0].r], [bA.r])
                    for kc in range(8):
                        mm(bA.t[:, TT:2 * TT], wk.t[:, kc, cs], xsn[1].t[:, kc, :], kc == 0, kc == 7, [wk.r, xsn[1].r], [bA.r])
                    for kc in range(8):
                        mm(bB.t[:, 0:TT], wv.t[:, kc, cs], xsn[2].t[:, kc, :], kc == 0, kc == 7, [wv.r, xsn[2].r], [bB.r])
                    mm(bB.t[:, TT:2 * TT], w2.t[0:64, 0, cs], tw.t[:], True, True, [w2.r, tw.r], [bB.r])
                    yield
                    cp("act", B["r"].t[:], bA.t[:, 0:TT], [], [bA.r, B["r"].r])
                    ts("dve", B["kk"].t[:], bA.t[:, TT:2 * TT], vD("k_k%d" % i, c), None, ALU.mult, None, [vt.r], [bA.r, B["kk"].r])
                    cp("act", B["k"].t[:], bA.t[:, TT:2 * TT], [], [bA.r, B["k"].r])
                    yield
                    cp("dve", B["v"].t[:], bB.t[:, 0:TT], [], [bB.r, B["v"].r])
                    cp("act", vb.t[:], bB.t[:, 0:TT], [], [bB.r, vb.r])
                    act(B["lw"].t[:], bB.t[:, TT:2 * TT], AF.Sigmoid, [vt.r], [bB.r, B["lw"].r], bias=vD("w0%d" % i, c))
                    ts("pool", B["lw"].t[:], B["lw"].t[:], -math.exp(-0.5), None, ALU.mult, None, [], [B["lw"].r])
                    yield
                    mm(bA.t[:, 0:TT], a2.t[0:64, 0, cs], ta.t[:], True, True, [a2.r, ta.r], [bA.r])
                    mm(bA.t[:, TT:2 * TT], g2.t[:, 0, cs], tg.t[:, 0, :], True, False, [g2.r, tg.r], [bA.r])
                    mm(bA.t[:, TT:2 * TT], g2.t[0:32, 1, cs], tg.t[0:32, 1, :], False, True, [g2.r, tg.r], [bA.r])
                    act(B["a"].t[:], bA.t[:, 0:TT], AF.Sigmoid, [vt.r], [bA.r, B["a"].r], bias=vD("a0%d" % i, c))
                    cp("act", B["g"].t[:], bA.t[:, TT:2 * TT], [], [bA.r, B["g"].r])
                    yield
                    act(sqb.t[:], B["kk"].t[:], AF.Square, [B["kk"].r], [sqb.r])
                    mm(bB.t[:, 0:TT], bdb, sqb.t[:], True, True, [bdb_t.r, sqb.r], [bB.r])
                    ts("dve", B["rn"].t[:], bB.t[:, 0:TT], 1e-24, None, ALU.max, None, [], [bB.r, B["rn"].r])
                    act(B["rn"].t[:], B["rn"].t[:], AF.Ln, [], [B["rn"].r])
                    act(B["rn"].t[:], B["rn"].t[:], AF.Exp, [], [B["rn"].r], scale=-0.5)
                    tt("dve", B["kk"].t[:], B["kk"].t[:], B["rn"].t[:], ALU.mult, [B["rn"].r], [B["kk"].r])
                    yield
                    ts("pool", B["kmod"].t[:], B["a"].t[:], -1.0, vD("k_a%d" % i, c), ALU.add, ALU.mult, [B["a"].r, vt.r], [B["kmod"].r])
                    stt("dve", B["kmod"].t[:], B["kmod"].t[:], 1.0, B["k"].t[:], ALU.add, ALU.mult, [B["k"].r], [B["kmod"].r])
                    tt("pool", B["b"].t[:], B["kk"].t[:], B["a"].t[:], ALU.mult, [B["kk"].r, B["a"].r], [B["b"].r])
                    for ch in range(2):
                        sl = slice(ch * 128, (ch + 1) * 128)
                        P.op("dve", lambda e, sl=sl: e.tensor_tensor_scan(
                            B["cum"].t[:, sl], ones, B["lw"].t[:, sl], 0.0, ALU.mult, ALU.add), [cst.r, B["lw"].r], [B["cum"].r])
                    tt("pool", B["cex"].t[:], B["cum"].t[:], B["lw"].t[:], ALU.subtract, [B["cum"].r, B["lw"].r], [B["cex"].r])
                    act(B["ecum"].t[:], B["cum"].t[:], AF.Exp, [B["cum"].r], [B["ecum"].r])
                    act(B["cum"].t[:], B["cum"].t[:], AF.Exp, [], [B["cum"].r], scale=-1.0)
                    act(B["cex"].t[:], B["cex"].t[:], AF.Exp, [], [B["cex"].r])
                    yield
                    for ch in range(2):
                        sl = slice(ch * 128, (ch + 1) * 128)
                        o = ch * 256
                        stt("dve", AR.t[:, o:o + 128], B["kk"].t[:, sl], -1.0, B["cex"].t[:, sl], ALU.mult, ALU.mult,
                            [B["kk"].r, B["cex"].r], [AR.r])
                        tt("pool", AR.t[:, o + 128:o + 256], B["r"].t[:, sl], B["ecum"].t[:, sl], ALU.mult, [B["r"].r, B["ecum"].r], [AR.r])
                        tt("dve", KB.t[:, o:o + 128], B["kmod"].t[:, sl], B["cum"].t[:, sl], ALU.mult, [B["kmod"].r, B["cum"].r], [KB.r])
                        tt("pool", KB.t[:, o + 128:o + 256], B["b"].t[:, sl], B["cum"].t[:, sl], ALU.mult, [B["b"].r, B["cum"].r], [KB.r])
                    yield
                    stt("dve", sqb.t[:], B["r"].t[:], vD("r_k%d" % i, c), B["kmod"].t[:], ALU.mult, ALU.mult,
                        [B["r"].r, B["kmod"].r, vt.r], [sqb.r])
                    mm(bB.t[:, TT:2 * TT], bdb, sqb.t[:], True, True, [bdb_t.r, sqb.r], [bB.r])
                    tt("dve", B["bonus"].t[:], bB.t[:, TT:2 * TT], B["v"].t[:], ALU.mult, [B["v"].r], [bB.r, B["bonus"].r])
                    for ch in range(2):
                        sl = slice(ch * 128, (ch + 1) * 128)
                        o = ch * 256
                        mm(bA.t[:, ch * 128:ch * 128 + 128], vb.t[:, sl], identb, True, True, [vb.r, identb_t.r], [bA.r])
                        mm(bA.t[:, 256 + ch * 128:256 + ch * 128 + 128], KB.t[:, o:o + 128], identb, True, True, [KB.r, identb_t.r], [bA.r])
                        mm(bB.t[:, ch * 128:ch * 128 + 128], KB.t[:, o + 128:o + 256], identb, True, True, [KB.r, identb_t.r], [bB.r])
                    yield
                    for ch in range(2):
                        cp("act", TM.t[:, ch, 0, :], bA.t[:, ch * 128:ch * 128 + 128], [], [bA.r, TM.r])
                        cp("dve", TM.t[:, ch, 1, :], bA.t[:, 256 + ch * 128:256 + ch * 128 + 128], [], [bA.r, TM.r])
                        cp("act", TM.t[:, ch, 2, :], bB.t[:, ch * 128:ch * 128 + 128], [], [bB.r, TM.r])
                    yield
                    for ch in range(2):
                        o = ch * 256
                        for hh in range(2):
                            p = slice(hh * 64, hh * 64 + 64)
                            Hh = hd[hh]
                            WK = Hh["WK"]
                            mm(WK.t[:, 0:256], KB.t[p, o:o + 128], AR.t[p, o:o + 256], True, True, [KB.r, AR.r], [WK.r])
                            mm(WK.t[:, 256:512], KB.t[p, o + 128:o + 256], AR.t[p, o:o + 256], True, True, [KB.r, AR.r], [WK.r])
                            TB = bA if hh == 0 else bB
                            mm(TB.t[:, 128:256], AR.t[p, o:o + 128], KB.t[p, o + 128:o + 256], True, True, [KB.r, AR.r], [TB.r])
                            yield
                        for hh in range(2):
                            Hh = hd[hh]
                            WK = Hh["WK"]
                            TB = bA if hh == 0 else bB
                            tt("dve", Hh["AMs"].t[:], WK.t[:], mask4, ALU.mult, [cst.r], [WK.r, Hh["AMs"].r])
                            tt("dve", Hh["Ls"].t[:], TB.t[:, 128:256], lowm, ALU.mult, [cst.r], [TB.r, Hh["Ls"].r])
                            tt("pool", Hh["Tt"][0].t[:], Hh["AMs"].t[:, 256:384], identb, ALU.add, [Hh["AMs"].r, identb_t.r], [Hh["Tt"][0].r])
                            yield
                        for lv in range(1, 7):
                            for hh in range(2):
                                Hh = hd[hh]
                                WK = Hh["WK"]
                                if lv == 1:
                                    Lp, Mp, rd = Hh["Ls"].t[:], Hh["AMs"].t[:, 256:384], [Hh["Ls"].r, Hh["AMs"].r]
                                else:
                                    pl = Hh["LM"][(lv - 1) % 2]
                                    Lp, Mp, rd = pl.t[:, 0:128], pl.t[:, 128:256], [pl.r]
                                nl = Hh["LM"][lv % 2]
                                mm(WK.t[:, 128:256], Mp, Lp, True, True, rd, [WK.r])
                                if lv < 6:
                                    mm(WK.t[:, 256:384], Lp, Mp, True, True, rd, [WK.r])
                                    cp("act", nl.t[:, 0:256], WK.t[:, 128:384], [], [WK.r, nl.r])
                                else:
                                    cp("act", nl.t[:, 0:128], WK.t[:, 128:256], [], [WK.r, nl.r])
                                yield
                            for hh in range(2):
                                Hh = hd[hh]
                                WK = Hh["WK"]
                                nl = Hh["LM"][lv % 2]
                                Tc = Hh["Tt"][(lv - 1) % 2]
                                Tn = Hh["Tt"][lv % 2]
                                TB = bA if hh == 0 else bB
                                mm(TB.t[:, 0:128], nl.t[:, 0:128], Tc.t[:], True, True, [nl.r, Tc.r], [TB.r])
                                tt("dve", Tn.t[:], TB.t[:, 0:128], Tc.t[:], ALU.add, [Tc.r], [TB.r, Tn.r])
                                yield
                        for hh in range(2):
                            p = slice(hh * 64, hh * 64 + 64)
                            fs = slice(hh * 64, hh * 64 + 64)
                            Hh = hd[hh]
                            WK = Hh["WK"]
                            mm(WK.t[:, 0:64], AR.t[p, o:o + 128], Sb.t[p, c, :], True, False, [AR.r, SbR[c]], [WK.r])
                            mm(WK.t[:, 0:64], Hh["AMs"].t[:, 0:128], TM.t[:, ch, 0, fs], False, True, [Hh["AMs"].r, TM.r], [WK.r])
                            cp("act", Hh["Xs"].t[:], WK.t[:, 0:64], [], [WK.r, Hh["Xs"].r])
                            yield
                        for hh in range(2):
                            Hh = hd[hh]
                            WK = Hh["WK"]
                            Tf = Hh["Tt"][0]
                            mm(WK.t[:, 64:128], Tf.t[:], Hh["Xs"].t[:], True, True, [Tf.r, Hh["Xs"].r], [WK.r])
                            cp("act", Hh["Us"].t[:], WK.t[:, 64:128], [], [WK.r, Hh["Us"].r])
                            yield
                        for hh in range(2):
                            p = slice(hh * 64, hh * 64 + 64)
                            fs = slice(hh * 64, hh * 64 + 64)
                            Hh = hd[hh]
                            WK = Hh["WK"]
                            yo = WK.t[p, 128:256]
                            mm(yo, Sb.t[p, c, :], AR.t[p, o + 128:o + 256], True, False, [SbR[c], AR.r], [WK.r])
                            mm(yo, Hh["Us"].t[:], Hh["AMs"].t[:, 384:512], False, False, [Hh["Us"].r, Hh["AMs"].r], [WK.r])
                            mm(yo, TM.t[:, ch, 0, fs], Hh["AMs"].t[:, 128:256], False, True, [TM.r, Hh["AMs"].r], [WK.r])
                            so = WK.t[p, 256:320]
                            mm(so, TM.t[:, ch, 2, fs], Hh["Us"].t[:], True, False, [TM.r, Hh["Us"].r], [WK.r])
                            mm(so, TM.t[:, ch, 1, fs], TM.t[:, ch, 0, fs], False, True, [TM.r], [WK.r])
                            yield
                        for hh in range(2):
                            p = slice(hh * 64, hh * 64 + 64)
                            Hh = hd[hh]
                            WK = Hh["WK"]
                            yo = WK.t[p, 128:256]
                            so = WK.t[p, 256:320]
                            cp("act", B["d"].t[p, ch * 128:ch * 128 + 128], yo, [], [WK.r, B["d"].r])
                            tt("dve", Hh["tmpS"].t[p, :], so, S.t[p, c, :], ALU.add, [SR[c]], [WK.r, Hh["tmpS"].r])
                            wc = B["ecum"].t[p, ch * 128 + 127:ch * 128 + 128]
                            ts("dve", S.t[p, c, :], Hh["tmpS"].t[p, :], wc, None, ALU.mult, None, [Hh["tmpS"].r, B["ecum"].r], [SR[c]])
                            ts("pool", Sb.t[p, c, :], Hh["tmpS"].t[p, :], wc, None, ALU.mult, None, [Hh["tmpS"].r, B["ecum"].r], [SbR[c]])
                            yield
                    cp("pool", sqb.t[:], B["d"].t[:], [B["d"].r], [sqb.r])
                    mm(bB.t[:, 0:TT], bdb, sqb.t[:], True, True, [bdb_t.r, sqb.r], [bB.r])
                    stt("dve", B["d"].t[:], bB.t[:, 0:TT], -1.0 / 64, B["d"].t[:], ALU.mult, ALU.add, [], [bB.r, B["d"].r])
                    act(sqb.t[:], B["d"].t[:], AF.Square, [B["d"].r], [sqb.r])
                    mm(bB.t[:, TT:2 * TT], bdb, sqb.t[:], True, True, [bdb_t.r, sqb.r], [bB.r])
                    act(B["rn"].t[:], bB.t[:, TT:2 * TT], AF.Ln, [], [bB.r, B["rn"].r], bias=LNX_EPS, scale=1.0 / 64)
                    act(B["rn"].t[:], B["rn"].t[:], AF.Exp, [], [B["rn"].r], scale=-0.5)
                    yield
                    stt("dve", B["d"].t[:], B["d"].t[:], vD("lnx_w%d" % i, c), B["rn"].t[:], ALU.mult, ALU.mult, [B["rn"].r, vt.r], [B["d"].r])
                    stt("dve", B["d"].t[:], B["d"].t[:], vD("lnx_b%d" % i, c), B["bonus"].t[:], ALU.add, ALU.add, [B["bonus"].r, vt.r], [B["d"].r])
                    tt("pool", z.t[:, c, :], B["d"].t[:], B["g"].t[:], ALU.mult, [B["d"].r, B["g"].r], [z.r])
                    yield

                for it in range(T // TT):
                    t0 = it * TT
                    P.dma(xt.t[:], xtile(src, t0, TT), reads=rx(t0, TT), writes=[xt.r])
                    rms_stats(sq, xt.t[:], 8, TT, D, NORM_EPS, rstd, [xt.r], PS[3])
                    for c in range(8):
                        stt("dve", hb.t[:, c, 1:TT + 1], xt.t[:, c, :], vD("ng%d_0" % layer, c), rstd.t[:, :],
                            ALU.mult, ALU.mult, [xt.r, rstd.r, vt.r], [hb.r])
                    tt("pool", xx.t[:], hb.t[:, :, 0:TT], hb.t[:, :, 1:TT + 1], ALU.subtract, [hb.r], [xx.r])
                    for n in range(6):
                        for c in range(8):
                            stt("dve", xsn[n].t[:, c, :], xx.t[:, c, :], vD("mu%d_%d" % (i, n), c),
                                hb.t[:, c, 1:TT + 1], ALU.mult, ALU.add, [xx.r, hb.r, vt.r], [xsn[n].r])
                    for kc in range(8):
                        mm(PS[0].t[0:64, 0:TT], w1.t[:, kc, :], xsn[3].t[:, kc, :], kc == 0, kc == 7, [w1.r, xsn[3].r], [PS[0].r])
                    act(tw.t[:], PS[0].t[0:64, 0:TT], AF.Tanh, [], [PS[0].r, tw.r])
                    for kc in range(8):
                        mm(PS[1].t[0:64, 0:TT], a1.t[:, kc, :], xsn[4].t[:, kc, :], kc == 0, kc == 7, [a1.r, xsn[4].r], [PS[1].r])
                    cp("act", ta.t[:], PS[1].t[0:64, 0:TT], [], [PS[1].r, ta.r])
                    for kc in range(8):
                        mm(PS[2].t[:, 0:TT], g1.t[:, kc, 0:128], xsn[5].t[:, kc, :], kc == 0, kc == 7, [g1.r, xsn[5].r], [PS[2].r])
                    for kc in range(8):
                        mm(PS[2].t[0:32, TT:2 * TT], g1.t[:, kc, 128:160], xsn[5].t[:, kc, :], kc == 0, kc == 7, [g1.r, xsn[5].r], [PS[2].r])
                    act(tg.t[:, 0, :], PS[2].t[:, 0:TT], AF.Sigmoid, [], [PS[2].r, tg.r])
                    act(tg.t[0:32, 1, :], PS[2].t[0:32, TT:2 * TT], AF.Sigmoid, [], [PS[2].r, tg.r])

                    run_jobs((job(c, sets[c % 2]) for c in range(8)), 2, 38)

                    for m in range(8):
                        bk = PS[m % 4]
                        for kc in range(8):
                            mm(bk.t[:, 0:TT], wo.t[:, kc, m * 128:(m + 1) * 128], z.t[:, kc, :], kc == 0, kc == 7, [wo.r, z.r], [bk.r])
                        cp("act", msb.t[:, m, :], bk.t[:, 0:TT], [], [bk.r, msb.r])
                    rms_stats(sq, msb.t[:], 8, TT, D, NORM_EPS, rstd, [msb.r], PS[4])
                    for c in range(8):
                        stt("dve", msb.t[:, c, :], msb.t[:, c, :], vD("ng%d_1" % layer, c), rstd.t[:, :], ALU.mult, ALU.mult,
                            [rstd.r, vt.r], [msb.r])
                    tt("pool", msb.t[:], msb.t[:], xt.t[:], ALU.add, [xt.r], [msb.r])
                    P.dma(xtile(dst, t0, TT), msb.t[:], reads=[msb.r], writes=rx(t0, TT))
                    cp("pool", hb.t[:, :, 0:1], hb.t[:, :, TT:TT + 1], [], [hb.r])
                P.barrier()

        def ffn_layer(layer, src, dst):
            TT = 512
            with ExitStack() as st:
                win = sbt(st, "win", [128, 8, 2 * FF], BF16)
                stg.extend([sbt(st, "stgx%d" % k, [128, 1024], F32) for k in range(4)])
                load_wk(win, w_in_d[layer], 8, 2 * FF)
                del stg[2:]
                xts = [sbt(st, "xt%d" % k, [128, 8, TT], F32) for k in range(2)]
                xns = [sbt(st, "xn%d" % k, [128, 8, TT], BF16) for k in range(2)]
                sq = sbt(st, "sq", [128, 8, TT], BF16)
                rstd = sbt(st, "rstd", [128, TT], F32)
                carry = sbt(st, "carry", [128, NF, 2], F32)
                gbuf = [sbt(st, "gbuf%d" % k, [128, TT + 2], F32) for k in range(2)]
                acc = [sbt(st, "acc%d" % k, [128, TT], F32) for k in range(2)]
                gl = [sbt(st, "gl%d" % k, [128, TT], F32) for k in range(2)]
                hT = [sbt(st, "hT%d" % k, [128, TT], BF16) for k in range(2)]
                P.op("pool", lambda e: e.memset(carry.t[:], 0.0), [], [carry.r])

                def norm_in(it):
                    t0 = it * TT
                    xt, xn = xts[it % 2], xns[it % 2]
                    P.dma(xt.t[:], xtile(src, t0, TT), reads=rx(t0, TT), writes=[xt.r])
                    rms_stats(sq, xt.t[:], 8, TT, D, NORM_EPS, rstd, [xt.r], PS[7])
                    for c in range(8):
                        stt("dve", xn.t[:, c, :], xt.t[:, c, :], vD("ng%d_2" % layer, c), rstd.t[:, :], ALU.mult, ALU.mult,
                            [xt.r, rstd.r, vt.r], [xn.r])

                norm_in(0)
                for it in range(T // TT):
                    t0 = it * TT
                    xn = xns[it % 2]
                    for f in range(NF):
                        if f == 12 and it + 1 < T // TT:
                            norm_in(it + 1)
                        k2 = f % 2
                        gb, ub = PS[k2], PS[2 + k2]
                        for kc in range(8):
                            mm(gb.t[:, :], win.t[:, kc, f * 128:(f + 1) * 128], xn.t[:, kc, :], kc == 0, kc == 7,
                               [win.r, xn.r], [gb.r])
                        for kc in range(8):
                            mm(ub.t[:, :], win.t[:, kc, FF + f * 128:FF + (f + 1) * 128], xn.t[:, kc, :], kc == 0, kc == 7,
                               [win.r, xn.r], [ub.r])
                        G = gbuf[k2]
                        cp("pool", G.t[:, 0:2], carry.t[:, f, :], [carry.r], [G.r])
                        cp("act", G.t[:, 2:TT + 2], gb.t[:, :], [], [gb.r, G.r])
                        cp("pool", carry.t[:, f, :], G.t[:, TT:TT + 2], [G.r], [carry.r])
                        A = acc[k2]
                        ts("dve", A.t[:], G.t[:, 2:TT + 2], vF(layer, 2, f), vF(layer, 3, f), ALU.mult, ALU.add,
                           [G.r, vt.r], [A.r])
                        stt("dve", A.t[:], G.t[:, 1:TT + 1], vF(layer, 1, f), A.t[:], ALU.mult, ALU.add, [G.r, vt.r], [A.r])
                        stt("dve", A.t[:], G.t[:, 0:TT], vF(layer, 0, f), A.t[:], ALU.mult, ALU.add, [G.r, vt.r], [A.r])
                        act(gl[k2].t[:], A.t[:], AF.Gelu_apprx_tanh, [A.r], [gl[k2].r])
                        tt("dve", hT[k2].t[:], ub.t[:, :], gl[k2].t[:], ALU.mult, [gl[k2].r], [ub.r, hT[k2].r])
                        P.dma(hscr[it, :, f, :], hT[k2].t[:], reads=[hT[k2].r], writes=[Rh[it]])
                P.barrier()
            with ExitStack() as st:
                wout = sbt(st, "wout", [128, NF, D], BF16)
                stg.extend([sbt(st, "stgy%d" % k, [128, 1024], F32) for k in range(4)])
                load_wk(wout, w_out_d[layer], NF, D)
                del stg[2:]
                xt = sbt(st, "xt", [128, 8, TT], F32)
                ht = [sbt(st, "ht%d" % k, [128, NF, TT], BF16) for k in range(2)]
                msb = sbt(st, "msb", [128, 8, TT], F32)
                sq = sbt(st, "sq", [128, 8, TT], BF16)
                rstd = sbt(st, "rstd", [128, TT], F32)
                for it in range(T // TT):
                    t0 = it * TT
                    hh = ht[it % 2]
                    P.dma(hh.t[:], hscr[it], reads=[Rh[it]], writes=[hh.r])
                    P.dma(xt.t[:], xtile(src, t0, TT), reads=rx(t0, TT), writes=[xt.r])
                    for m in range(8):
                        bk = PS[m % 4]
                        for f in range(NF):
                            mm(bk.t[:, :], wout.t[:, f, m * 128:(m + 1) * 128], hh.t[:, f, :], f == 0, f == NF - 1,
                               [wout.r, hh.r], [bk.r])
                        cp("act", msb.t[:, m, :], bk.t[:, :], [], [bk.r, msb.r])
                    rms_stats(sq, msb.t[:], 8, TT, D, NORM_EPS, rstd, [msb.r], PS[7])
                    for c in range(8):
                        stt("dve", msb.t[:, c, :], msb.t[:, c, :], vD("ng%d_3" % layer, c), rstd.t[:, :], ALU.mult, ALU.mult,
                            [rstd.r, vt.r], [msb.r])
                    tt("pool", msb.t[:], msb.t[:], xt.t[:], ALU.add, [xt.r], [msb.r])
                    P.dma(xtile(dst, t0, TT), msb.t[:], reads=[msb.r], writes=rx(t0, TT))
                P.barrier()

        def kv_prep(src):
            TT = 512
            with ExitStack() as st:
                kd = sbt(st, "kd", [128, 8, 288], BF16)
                kds = sbt(st, "kds", [128, 8, 32], BF16)
                kuk = sbt(st, "kuk", [128, 2, H * 64], BF16)
                kuv = sbt(st, "kuv", [128, 2, H * 64], BF16)
                load_wk(kd, kd_d, 8, 288)
                load_wk(kds, kds_d, 8, 32)
                load_wk(kuk, kuk_d, 2, H * 64)
                load_wk(kuv, kuv_d, 2, H * 64)
                xt = sbt(st, "xt", [128, 8, TT], F32)
                xn = sbt(st, "xn", [128, 8, TT], BF16)
                sq = sbt(st, "sq", [128, 8, TT], BF16)
                rstd = sbt(st, "rstd", [128, TT], F32)
                ckv = sbt(st, "ckv", [128, 2, TT], F32)
                ckvn = sbt(st, "ckvn", [128, 2, TT], BF16)
                rp = sbt(st, "rp", [128, 2, TT], F32)
                t1 = sbt(st, "t1", [128, TT], F32)
                t2 = sbt(st, "t2", [128, TT], F32)
                kr = sbt(st, "kr", [128, TT], BF16)
                KT = [sbt(st, "KT%d" % k, [128, TT], BF16) for k in range(2)]
                Vt = [sbt(st, "Vt%d" % k, [128, H, 128], BF16) for k in range(2)]
                for k in range(2):
                    P.op("pool", lambda e, k=k: e.memset(Vt[k].t[:, :, 64:128], 1.0), [], [Vt[k].r])
                R = slice(64, 96)
                for it in range(T // TT):
                    t0 = it * TT
                    P.dma(xt.t[:], xtile(src, t0, TT), reads=rx(t0, TT), writes=[xt.r])
                    P.dma(rp.t[R, :, :], rope_d[R, :, t0:t0 + TT], reads=[Rin], writes=[rp.r])
                    rms_stats(sq, xt.t[:], 8, TT, D, NORM_EPS, rstd, [xt.r], PS[7])
                    for c in range(8):
                        stt(ew(), xn.t[:, c, :], xt.t[:, c, :], vD("kvng", c), rstd.t[:, :], ALU.mult, ALU.mult,
                            [xt.r, rstd.r, vt.r], [xn.r])
                    for j in range(2):
                        for kc in range(8):
                            mm(PS[j].t[:, :], kd.t[:, kc, j * 128:(j + 1) * 128], xn.t[:, kc, :], kc == 0, kc == 7,
                               [kd.r, xn.r], [PS[j].r])
                        cp("act", ckv.t[:, j, :], PS[j].t[:, :], [], [PS[j].r, ckv.r])
                    for kc in range(8):
                        mm(PS[2].t[R, :], kd.t[:, kc, 256:288], xn.t[:, kc, :], kc == 0, kc == 7, [kd.r, xn.r], [PS[2].r])
                    for kc in range(8):
                        mm(PS[3].t[R, :], kds.t[:, kc, :], xn.t[:, kc, :], kc == 0, kc == 7, [kds.r, xn.r], [PS[3].r])
                    tt("dve", t1.t[R, :], PS[2].t[R, :], rp.t[R, 0, :], ALU.mult, [rp.r], [PS[2].r, t1.r])
                    tt("dve", t2.t[R, :], PS[3].t[R, :], rp.t[R, 1, :], ALU.mult, [rp.r], [PS[3].r, t2.r])
                    tt("dve", kr.t[R, :], t1.t[R, :], t2.t[R, :], ALU.add, [t1.r, t2.r], [kr.r])
                    rms_stats(sq, ckv.t[:], 2, TT, KVL, NORM_EPS, rstd, [ckv.r], PS[7])
                    for c in range(2):
                        stt("dve", ckvn.t[:, c, :], ckv.t[:, c, :], vK(c), rstd.t[:, :], ALU.mult, ALU.mult,
                            [ckv.r, rstd.r, vt.r], [ckvn.r])
                    for h in range(H):
                        K = KT[h % 2]
                        bk = PS[h % 2]
                        for kc in range(2):
                            mm(bk.t[0:64, :], kuk.t[:, kc, h * 64:(h + 1) * 64], ckvn.t[:, kc, :], kc == 0, kc == 1,
                               [kuk.r, ckvn.r], [bk.r])
                        cp("act", K.t[0:64, :], bk.t[0:64, :], [], [bk.r, K.r])
                        cp("pool", K.t[R, :], kr.t[R, :], [kr.r], [K.r])
                        P.dma(kscr[h, :, t0:t0 + TT], K.t[0:96, :], reads=[K.r], writes=[Rkv[it]])
                    for tb in range(TT // 128):
                        V = Vt[tb % 2]
                        for hf in range(2):
                            bk = PS[4 + hf]
                            for kc in range(2):
                                mm(bk.t[:, :], ckvn.t[:, kc, tb * 128:(tb + 1) * 128], kuv.t[:, kc, hf * 512:(hf + 1) * 512],
                                   kc == 0, kc == 1, [kuv.r, ckvn.r], [bk.r])
                            cp("act" if hf == 0 else "dve", V.t[:, hf * 8:(hf + 1) * 8, 0:64],
                               bk.t[:, :].rearrange("p (h d) -> p h d", d=64), [], [bk.r, V.r])
                        P.dma(vscr[:, t0 + tb * 128:t0 + (tb + 1) * 128, :].rearrange("h t d -> t h d"), V.t[:],
                              reads=[V.r], writes=[Rkv[it]])
                P.barrier()

        def mla_layer(j, layer, src, dst):
            TT = 512
            with ExitStack() as st:
                qd = sbt(st, "qd", [128, 8, QL], BF16)
                qu = sbt(st, "qu", [128, 3, H * 96], BF16)
                qus = sbt(st, "qus", [128, 3, H * 32], BF16)
                ow = sbt(st, "ow", [64, H, D], BF16)
                load_wk(qd, qd_d[j], 8, QL)
                load_wk(qu, qu_d[j], 3, H * 96)
                load_wk(qus, qus_d[j], 3, H * 32)
                for h in range(H):
                    load_w(lambda r, c0, cw, h=h: ow.t[0:r, h, c0:c0 + cw], ow.r, ow_d[j, h * 64:(h + 1) * 64, :], 64, D)
                xt = sbt(st, "xt", [128, 8, TT], F32)
                xn = sbt(st, "xn", [128, 8, TT], BF16)
                sq = sbt(st, "sq", [128, 8, TT], BF16)
                rstd = sbt(st, "rstd", [128, TT], F32)
                cq = sbt(st, "cq", [128, 3, TT], F32)
                cqn = sbt(st, "cqn", [128, 3, TT], BF16)
                rp = sbt(st, "rp", [128, 2, TT], F32)
                t1 = sbt(st, "t1", [128, TT], F32)
                t2 = sbt(st, "t2", [128, TT], F32)
                QT = sbt(st, "QT", [128, H, TT], BF16)
                OT = sbt(st, "OT", [64, H, TT], BF16)
                KT = [sbt(st, "KT%d" % k, [128, T], BF16) for k in range(2)]
                VT = [sbt(st, "VT%d" % k, [128, T // 128, 128], BF16) for k in range(2)]
                PT = [sbt(st, "PT%d" % k, [128, TT], BF16) for k in range(3)]
                rec = sbt(st, "rec", [64, TT], F32)
                msb = sbt(st, "msb", [128, 8, TT], F32)
                R = slice(64, 96)
                pi = 0
                for it in range(T // TT):
                    t0 = it * TT
                    P.dma(xt.t[:], xtile(src, t0, TT), reads=rx(t0, TT), writes=[xt.r])
                    P.dma(rp.t[R, :, :], rope_d[R, :, t0:t0 + TT], reads=[Rin], writes=[rp.r])
                    rms_stats(sq, xt.t[:], 8, TT, D, NORM_EPS, rstd, [xt.r], PS[2])
                    for c in range(8):
                        stt(ew(), xn.t[:, c, :], xt.t[:, c, :], vD("ng%d_0" % layer, c), rstd.t[:, :], ALU.mult, ALU.mult,
                            [xt.r, rstd.r, vt.r], [xn.r])
                    for m in range(3):
                        bk = PS[m % 2]
                        for kc in range(8):
                            mm(bk.t[:, :], qd.t[:, kc, m * 128:(m + 1) * 128], xn.t[:, kc, :], kc == 0, kc == 7,
                               [qd.r, xn.r], [bk.r])
                        cp("act", cq.t[:, m, :], bk.t[:, :], [], [bk.r, cq.r])
                    rms_stats(sq, cq.t[:], 3, TT, QL, NORM_EPS, rstd, [cq.r], PS[2])
                    for c in range(3):
                        stt("dve", cqn.t[:, c, :], cq.t[:, c, :], vQ(j, c), rstd.t[:, :], ALU.mult, ALU.mult,
                            [cq.r, rstd.r, vt.r], [cqn.r])
                    for h in range(H):
                        A = PS[h % 2]
                        Bk = PS[2]
                        for kc in range(3):
                            mm(A.t[0:96, :], qu.t[:, kc, h * 96:(h + 1) * 96], cqn.t[:, kc, :], kc == 0, kc == 2,
                               [qu.r, cqn.r], [A.r])
                        for kc in range(3):
                            mm(Bk.t[R, :], qus.t[:, kc, h * 32:(h + 1) * 32], cqn.t[:, kc, :], kc == 0, kc == 2,
                               [qus.r, cqn.r], [Bk.r])
                        act(QT.t[0:64, h, :], A.t[0:64, :], AF.Copy, [], [A.r, QT.r], scale=SCALE)
                        stt("dve", t1.t[R, :], A.t[R, :], SCALE, rp.t[R, 0, :], ALU.mult, ALU.mult, [rp.r], [A.r, t1.r])
                        stt("dve", t2.t[R, :], Bk.t[R, :], SCALE, rp.t[R, 1, :], ALU.mult, ALU.mult, [rp.r], [Bk.r, t2.r])
                        tt("dve", QT.t[R, h, :], t1.t[R, :], t2.t[R, :], ALU.add, [t1.r, t2.r], [QT.r])
                    nkb = (it + 1) * 4
                    nk = nkb * 128
                    for h in range(H):
                        K = KT[h % 2]
                        V = VT[h % 2]
                        P.dma(K.t[0:96, 0:nk], kscr[h, :, 0:nk], reads=Rkv[0:it + 1], writes=[K.r])
                        P.dma(V.t[:, 0:nkb, :], vscr[h, 0:nk, :].rearrange("(kb p) d -> p kb d", p=128),
                              reads=Rkv[0:it + 1], writes=[V.r])
                        Ob = PS[6 + h % 2]
                        pts = {}

                        def qk(kb, h=h, K=K):
                            nonlocal pi
                            jd = kb - it * 4
                            c0 = max(jd, 0) * 128
                            Sb_ = PS[3 + pi % 3]
                            Pt = PT[pi % 3]
                            pi += 1
                            mm(Sb_.t[:, c0:TT], K.t[0:96, kb * 128:(kb + 1) * 128], QT.t[0:96, h, c0:TT], True, True,
                               [K.r, QT.r], [Sb_.r])
                            act(Pt.t[:, c0:TT], Sb_.t[:, c0:TT], AF.Exp, [], [Sb_.r, Pt.r])
                            if jd >= 0:
                                P.op("pool", lambda e, Pt=Pt, c0=c0: e.memset(Pt.t[64:128, c0:c0 + 64], 0.0), [], [Pt.r])
                            pts[kb] = (Pt, c0)

                        def pv(kb, V=V, Ob=Ob):
                            Pt, c0 = pts.pop(kb)
                            mm(Ob.t[:, c0:TT], V.t[:, kb, :], Pt.t[:, c0:TT], kb == 0, kb == nkb - 1, [V.r, Pt.r], [Ob.r])

                        LA = 2
                        for kb in range(min(LA, nkb)):
                            qk(kb)
                        for kb in range(nkb):
                            if kb + LA < nkb:
                                qk(kb + LA)
                            pv(kb)
                        P.op("dve", lambda e, Ob=Ob: e.reciprocal(rec.t[:, :], Ob.t[64:128, :]), [], [Ob.r, rec.r])
                        tt("dve", OT.t[:, h, :], Ob.t[0:64, :], rec.t[:, :], ALU.mult, [rec.r], [Ob.r, OT.r])
                    for m in range(8):
                        bk = PS[m % 2]
                        for h in range(H):
                            mm(bk.t[:, :], ow.t[0:64, h, m * 128:(m + 1) * 128], OT.t[0:64, h, :], h == 0, h == H - 1,
                               [ow.r, OT.r], [bk.r])
                        cp("act", msb.t[:, m, :], bk.t[:, :], [], [bk.r, msb.r])
                    rms_stats(sq, msb.t[:], 8, TT, D, NORM_EPS, rstd, [msb.r], PS[2])
                    for c in range(8):
                        stt("dve", msb.t[:, c, :], msb.t[:, c, :], vD("ng%d_1" % layer, c), rstd.t[:, :], ALU.mult, ALU.mult,
                            [rstd.r, vt.r], [msb.r])
                    tt("pool", msb.t[:], msb.t[:], xt.t[:], ALU.add, [xt.r], [msb.r])
                    P.dma(xtile(dst, t0, TT), msb.t[:], reads=[msb.r], writes=rx(t0, TT))
                P.barrier()

        P.barrier()
        cur = xT
        for layer in range(depth):
            if layer < NA:
                rwkv_layer(layer, layer, cur, xs)
            else:
                if layer == NA:
                    kv_prep(xs)
                mla_layer(layer - NA, layer, xs, xs)
            cur = xs
            ffn_layer(layer, xs, y if layer == depth - 1 else xs)
        P.barrier()
        P.emit()
        nc._n_ops = P.n
    return nc


NVT = [0, 0, 0]
_CACHE = {}


def prep_inputs(inputs):
    inp = {k: np.asarray(v) for k, v in inputs.items()}
    B, T, _ = inp["x"].shape
    vt, nd, nf = _pack_tables(inp)
    NVT[0], NVT[1], NVT[2] = vt.shape[1], nd, nf
    f32 = lambda a: np.ascontiguousarray(a, dtype=np.float32)
    kd = inp["kv_w_down"]
    kds = np.concatenate([kd[:, 272:288], kd[:, 256:272]], 1)
    ku = inp["kv_w_up"].reshape(KVL, H, 128)
    qu = inp["q_w_up"].reshape(2, QL, H, 96)
    qus = np.concatenate([qu[..., 80:96], qu[..., 64:80]], -1).reshape(2, QL, H * 32)
    shared = {
        "vt": vt, "cst": _consts(), "rope": _rope(T),
        "ffn_w_in": f32(inp["ffn_w_in"]), "ffn_w_out": f32(inp["ffn_w_out"]),
        "a_w_rkv": f32(inp["a_w_rkv"]), "a_w1": f32(inp["a_w1"]), "a_w2": f32(inp["a_w2"]),
        "a_a1": f32(inp["a_a1"]), "a_a2": f32(inp["a_a2"]), "a_g1": f32(inp["a_g1"]), "a_g2": f32(inp["a_g2"]),
        "a_w_o": f32(inp["a_w_o"]), "kv_w_down": f32(kd), "kv_w_down_sw": f32(kds),
        "kv_w_up_k": f32(ku[:, :, 0:64].reshape(KVL, H * 64)), "kv_w_up_v": f32(ku[:, :, 64:128].reshape(KVL, H * 64)),
        "q_w_down": f32(inp["q_w_down"]), "q_w_up": f32(inp["q_w_up"]), "q_w_up_sw": f32(qus),
        "o_w": f32(inp["o_w"]),
    }
    maps = []
    for b in range(B):
        m = dict(shared)
        m["xT"] = f32(inp["x"][b].T)
        maps.append(m)
    return maps, B, T


def kernel(**inputs):
    maps, B, T = prep_inputs(inputs)
    key = (T, DEPTH)
    if key not in _CACHE:
        _CACHE[key] = build(T)
    nc = _CACHE[key]
    res = run_bass_kernel_spmd(nc, maps, core_ids=list(range(B)))
    out = np.stack([np.asarray(r["y"]).T for r in res.results], 0)
    return np.ascontiguousarray(out.astype(np.float32))
```

```python
import math
from contextlib import ExitStack
import numpy as np
import concourse.bass as bass
import concourse.mybir as mybir
from concourse.bass_utils import run_bass_kernel_spmd

F32 = mybir.dt.float32
BF16 = mybir.dt.bfloat16
AF = mybir.ActivationFunctionType
ALU = mybir.AluOpType

D = 1024
DEPTH = 4
NA = 2
H = 16
FF = 2816
NF = FF // 128
QL = 384
KVL = 256
LNX_EPS = 64e-5
NORM_EPS = 1e-6
SCALE = 1.0 / math.sqrt(96.0)

COMPUTE = ("pe", "act", "dve", "pool")
SEM_LIMIT = 30000
NDMA_SEM = 24


class Res:
    __slots__ = ("name", "w", "r")

    def __init__(self, name=""):
        self.name = name
        self.w = None
        self.r = {}


class Tl:
    __slots__ = ("t", "r")

    def __init__(self, t, r):
        self.t = t
        self.r = r


class Prog:
    def __init__(self, nc, stack):
        self.nc = nc
        self.stack = stack
        self.streams = {e: [] for e in COMPUTE + ("sp",)}
        self.sems = {}
        self.cnt = {}
        for e in COMPUTE:
            self.sems[e] = [self._newsem(e + "0")]
            self.cnt[e] = (0, 0)
        self.dma_sems = [self._newsem("dma%d" % i) for i in range(NDMA_SEM)]
        self.dma_cnt = [0] * NDMA_SEM
        self.dma_i = 0
        self.seen = {e: {} for e in self.streams}
        self.n = 0

    def _newsem(self, name):
        return self.stack.enter_context(self.nc.semaphore(name))

    def _semof(self, key, idx):
        if isinstance(key, tuple):
            return self.dma_sems[key[1]]
        return self.sems[key][idx]

    @staticmethod
    def _need(waits, tok):
        if tok is None:
            return
        key, idx, val = tok
        cur = waits.get(key)
        if cur is None or (idx, val) > cur:
            waits[key] = (idx, val)

    def _collect(self, q, reads, writes, eng):
        waits = {}
        for r in reads:
            self._need(waits, r.w)
        for w in writes:
            self._need(waits, w.w)
            for k, (i, v) in w.r.items():
                if k == eng:
                    continue
                self._need(waits, (k, i, v))
        if eng == "pe":
            waits.pop("pe", None)
        out = []
        for key, (idx, val) in waits.items():
            s = self.seen[q].get(key)
            if s is not None and s >= (idx, val):
                continue
            self.seen[q][key] = (idx, val)
            out.append((self._semof(key, idx), val))
        return out

    def op(self, eng, fn, reads=(), writes=()):
        waits = self._collect(eng, reads, writes, eng)
        idx, val = self.cnt[eng]
        if val >= SEM_LIMIT:
            idx += 1
            val = 0
            self.sems[eng].append(self._newsem("%s%d" % (eng, idx)))
        val += 1
        self.cnt[eng] = (idx, val)
        self.streams[eng].append((waits, fn, (self.sems[eng][idx], 1)))
        tok = (eng, idx, val)
        for r in reads:
            r.r[eng] = (idx, val)
        for w in writes:
            w.w = tok
            w.r = {}
        self.n += 1
        return tok

    def dma(self, out, in_, reads=(), writes=(), q="sp"):
        waits = self._collect(q, reads, writes, None)
        j = self.dma_i % NDMA_SEM
        self.dma_i += 1
        self.dma_cnt[j] += 16
        val = self.dma_cnt[j]
        key = ("dma", j)
        self.streams[q].append(
            (waits, lambda e: e.dma_start(out=out, in_=in_), (self.dma_sems[j], 16)))
        tok = (key, 0, val)
        for r in reads:
            r.r[key] = (0, val)
        for w in writes:
            w.w = tok
            w.r = {}
        self.n += 1
        return tok

    def barrier(self):
        for q in self.streams:
            waits = []
            for e in COMPUTE:
                if e == q:
                    continue
                idx, val = self.cnt[e]
                if val == 0:
                    continue
                sn = self.seen[q].get(e)
                if sn is not None and sn >= (idx, val):
                    continue
                self.seen[q][e] = (idx, val)
                waits.append((self.sems[e][idx], val))
            for j in range(NDMA_SEM):
                val = self.dma_cnt[j]
                if val == 0:
                    continue
                key = ("dma", j)
                sn = self.seen[q].get(key)
                if sn is not None and sn >= (0, val):
                    continue
                self.seen[q][key] = (0, val)
                waits.append((self.dma_sems[j], val))
            self.streams[q].append((waits, None, None))

    def emit(self):
        nc = self.nc
        with nc.Block() as block:
            def run(stream):
                def body(e):
                    for waits, fn, inc in stream:
                        for s, v in waits:
                            e.wait_ge(s, v)
                        if fn is not None:
                            ins = fn(e)
                            if inc is not None:
                                ins.then_inc(inc[0], inc[1])
                return body
            block.tensor(run(self.streams["pe"]))
            block.scalar(run(self.streams["act"]))
            block.vector(run(self.streams["dve"]))
            block.gpsimd(run(self.streams["pool"]))
            block.sync(run(self.streams["sp"]))


VD = {}


def _pack_tables(inp):
    vecs = []

    def add(name, v):
        VD[name] = len(vecs)
        vecs.append(np.asarray(v, np.float32).reshape(D))

    for l in range(DEPTH):
        for j in range(4):
            add("ng%d_%d" % (l, j), inp["norm_g"][l, j])
    for i in range(NA):
        for n in range(6):
            add("mu%d_%d" % (i, n), inp["a_mu"][i, n])
        for nm in ("w0", "a0", "k_k", "k_a", "r_k", "lnx_w", "lnx_b"):
            add("%s%d" % (nm, i), inp["a_" + nm][i])
    add("kvng", inp["kv_norm_g"])
    tabD = np.stack(vecs, 0).reshape(len(vecs), 8, 128).transpose(2, 0, 1).reshape(128, -1)
    fv = []
    for l in range(DEPTH):
        for j in range(3):
            fv.append(inp["ffn_conv_w"][l, j])
        fv.append(inp["ffn_conv_b"][l])
    tabF = np.stack(fv, 0).reshape(len(fv), NF, 128).transpose(2, 0, 1).reshape(128, -1)
    qg = np.asarray(inp["q_norm_g"]).reshape(2, 3, 128).transpose(2, 0, 1).reshape(128, 6)
    kg = np.asarray(inp["kv_a_norm_g"]).reshape(2, 128).T
    vt = np.ascontiguousarray(np.concatenate([tabD, tabF, qg, kg], 1).astype(np.float32))
    return vt, tabD.shape[1], tabF.shape[1]


def _consts():
    p = np.arange(128)
    ident = np.eye(128, dtype=np.float32)
    bd = (p[:, None] // 64 == p[None, :] // 64).astype(np.float32)
    su = (p[:, None] < p[None, :]).astype(np.float32)
    ui = (p[:, None] <= p[None, :]).astype(np.float32)
    low = (p[None, :] < p[:, None]).astype(np.float32)
    ones = np.ones((128, 128), np.float32)
    return np.ascontiguousarray(np.concatenate([ident, bd, su, ui, su, ui, low, ones], 1))


C_ID, C_BD, C_M4, C_LOW, C_ONE = 0, 128, 256, 768, 896
NCST = 1024


def _rope(T):
    inv = 1.0 / (10000.0 ** (np.arange(0, 32, 2, dtype=np.float32) / 32.0))
    ang = np.arange(T, dtype=np.float32)[:, None] * inv[None, :].astype(np.float32)
    cos = np.cos(ang).astype(np.float32).T
    sin = np.sin(ang).astype(np.float32).T
    tab = np.zeros((128, 2, T), np.float32)
    tab[64:80, 0] = cos
    tab[80:96, 0] = cos
    tab[64:80, 1] = -sin
    tab[80:96, 1] = sin
    return tab


def build(T, depth=DEPTH, dbg=False):
    nc = bass.Bass("TRN2", target_bir_lowering=False)
    NT5 = T // 512
    NT2 = T // 256

    def din(name, shape):
        return nc.dram_tensor(name, list(shape), F32, kind="ExternalInput").ap()

    xT = din("xT", [D, T])
    vt_d = din("vt", [128, NVT[0]])
    cst_d = din("cst", [128, NCST])
    rope_d = din("rope", [128, 2, T])
    w_in_d = din("ffn_w_in", [DEPTH, D, 2 * FF])
    w_out_d = din("ffn_w_out", [DEPTH, FF, D])
    wrkv_d = din("a_w_rkv", [NA, 3, D, D])
    w1_d = din("a_w1", [NA, D, 64])
    w2_d = din("a_w2", [NA, 64, D])
    a1_d = din("a_a1", [NA, D, 64])
    a2_d = din("a_a2", [NA, 64, D])
    g1_d = din("a_g1", [NA, D, 160])
    g2_d = din("a_g2", [NA, 160, D])
    wo_d = din("a_w_o", [NA, D, D])
    kd_d = din("kv_w_down", [D, 288])
    kds_d = din("kv_w_down_sw", [D, 32])
    kuk_d = din("kv_w_up_k", [KVL, H * 64])
    kuv_d = din("kv_w_up_v", [KVL, H * 64])
    qd_d = din("q_w_down", [2, D, QL])
    qu_d = din("q_w_up", [2, QL, H * 96])
    qus_d = din("q_w_up_sw", [2, QL, H * 32])
    ow_d = din("o_w", [2, D, D])
    y = nc.dram_tensor("y", [D, T], F32, kind="ExternalOutput").ap()
    xs = nc.dram_tensor("xs", [D, T], F32).ap()
    hscr = nc.dram_tensor("hscr", [T // 512, 128, NF, 512], BF16).ap()
    kscr = nc.dram_tensor("kscr", [H, 96, T], BF16).ap()
    vscr = nc.dram_tensor("vscr", [H, T, 128], BF16).ap()

    Rx = [Res("x%d" % i) for i in range(NT2)]
    Rh = [Res("h%d" % i) for i in range(NT5)]
    Rkv = [Res("kv%d" % i) for i in range(NT5)]
    Rin = Res("in")

    with ExitStack() as top:
        P = Prog(nc, top)
        top.enter_context(nc.allow_low_precision("bf16 matmul operands, fp32 accumulate"))

        uid = [0]

        def sbt(st, name, shape, dt):
            uid[0] += 1
            nm = "sb%d_%s" % (uid[0], name)
            return Tl(st.enter_context(nc.sbuf_tensor(nm, list(shape), dt)), Res(nm))

        PS = [Tl(top.enter_context(nc.psum_tensor("ps%d" % i, [128, 512], F32)), Res("ps%d" % i))
              for i in range(8)]
        vt = sbt(top, "vt", [128, NVT[0]], F32)
        cst = sbt(top, "cst", [128, NCST], F32)
        onesb = sbt(top, "onesb", [128, 128], BF16)
        stg = [sbt(top, "stg%d" % i, [128, 512], F32) for i in range(2)]
        stg_i = [0]
        P.dma(vt.t[:], vt_d, writes=[vt.r])
        P.dma(cst.t[:], cst_d, writes=[cst.r])
        P.op("dve", lambda e: e.tensor_copy(onesb.t[:], cst.t[:, C_ONE:C_ONE + 128]), [cst.r], [onesb.r])
        identb_t = sbt(top, "identb", [128, 128], BF16)
        P.op("dve", lambda e: e.tensor_copy(identb_t.t[:], cst.t[:, C_ID:C_ID + 128]), [cst.r], [identb_t.r])
        identb = identb_t.t[:, :]
        bdb_t = sbt(top, "bdb", [128, 128], BF16)
        P.op("dve", lambda e: e.tensor_copy(bdb_t.t[:], cst.t[:, C_BD:C_BD + 128]), [cst.r], [bdb_t.r])
        bdb = bdb_t.t[:, :]

        ident = cst.t[:, C_ID:C_ID + 128]
        bdones = cst.t[:, C_BD:C_BD + 128]
        mask4 = cst.t[:, C_M4:C_M4 + 512]
        lowm = cst.t[:, C_LOW:C_LOW + 128]
        ones = cst.t[:, C_ONE:C_ONE + 128]

        def vD(name, c):
            i = VD[name] * 8 + c
            return vt.t[:, i:i + 1]

        def vF(l, j, f):
            i = NVT[1] + (l * 4 + j) * NF + f
            return vt.t[:, i:i + 1]

        def vQ(j, c):
            i = NVT[1] + NVT[2] + j * 3 + c
            return vt.t[:, i:i + 1]

        def vK(c):
            i = NVT[1] + NVT[2] + 6 + c
            return vt.t[:, i:i + 1]

        def mm(out, lhsT, rhs, start, stop, reads, writes):
            P.op("pe", lambda e: e.matmul(out, lhsT, rhs, start=start, stop=stop), reads, writes)

        def act(out, in_, func, reads, writes, bias=None, scale=None):
            kw = {}
            if bias is not None:
                kw["bias"] = bias
            if scale is not None:
                kw["scale"] = scale
            P.op("act", lambda e: e.activation(out, in_, func, **kw), reads, writes)

        def tt(eng, out, in0, in1, op, reads, writes):
            P.op(eng, lambda e: e.tensor_tensor(out, in0, in1, op), reads, writes)

        def ts(eng, out, in0, s1, s2, op0, op1, reads, writes):
            if s2 is None:
                P.op(eng, lambda e: e.tensor_scalar(out, in0, s1, None, op0), reads, writes)
            else:
                P.op(eng, lambda e: e.tensor_scalar(out, in0, s1, s2, op0, op1), reads, writes)

        def stt(eng, out, in0, sc, in1, op0, op1, reads, writes):
            P.op("dve", lambda e: e.scalar_tensor_tensor(out, in0, sc, in1, op0, op1), reads, writes)

        def cp(eng, out, in_, reads, writes):
            if eng == "act":
                act(out, in_, AF.Copy, reads, writes)
            else:
                P.op(eng, lambda e: e.tensor_copy(out, in_), reads, writes)

        rr = [0]

        def ew():
            rr[0] += 1
            return "pool" if rr[0] % 3 == 0 else "dve"

        def load_w(view, res, src, rows, cols):
            c0 = 0
            while c0 < cols:
                cw = min(512, cols - c0)
                s = stg[stg_i[0] % len(stg)]
                stg_i[0] += 1
                P.dma(s.t[0:rows, 0:cw], src[:, c0:c0 + cw], reads=[Rin], writes=[s.r])
                eng = "pool" if stg_i[0] % 2 == 0 else "dve"
                cp(eng, view(rows, c0, cw), s.t[0:rows, 0:cw], [s.r], [res])
                c0 += cw

        def load_wk(tile, src, nk, cols, rows_last=128):
            for k in range(nk):
                rows = rows_last if k == nk - 1 else 128
                load_w(lambda r, c0, cw, k=k: tile.t[0:r, k, c0:c0 + cw], tile.r,
                       src[k * 128:k * 128 + rows, :], rows, cols)

        def rms_stats(st_sq, src_ap, nch, TT, Dn, eps, rstd, src_reads, bank):
            act(st_sq.t[:, 0:nch, 0:TT], src_ap, AF.Square, src_reads, [st_sq.r])
            for c in range(nch):
                mm(bank.t[:, 0:TT], onesb.t[:, :], st_sq.t[:, c, 0:TT], c == 0, c == nch - 1,
                   [st_sq.r, onesb.r], [bank.r])
            act(rstd.t[:, 0:TT], bank.t[:, 0:TT], AF.Ln, [], [bank.r, rstd.r], bias=eps, scale=1.0 / Dn)
            act(rstd.t[:, 0:TT], rstd.t[:, 0:TT], AF.Exp, [], [rstd.r], scale=-0.5)

        def xtile(ap, t0, TT):
            return ap.rearrange("(c p) t -> p c t", p=128)[:, :, t0:t0 + TT]

        def rx(t0, TT):
            return Rx[t0 // 256:(t0 + TT) // 256]

        def run_jobs(gens, width, stagger):
            active = []
            it = iter(gens)
            steps0 = 0
            done = False
            while True:
                while not done and len(active) < width and (len(active) == 0 or steps0 >= stagger):
                    g = next(it, None)
                    if g is None:
                        done = True
                        break
                    active.append(g)
                    if len(active) == 1:
                        steps0 = 0
                if not active:
                    break
                for g in list(active):
                    try:
                        next(g)
                    except StopIteration:
                        active.remove(g)
                        steps0 = stagger
                steps0 += 1

        def rwkv_layer(i, layer, src, dst):
            TT = 256
            with ExitStack() as st:
                wr = sbt(st, "wr", [128, 8, D], BF16)
                wk = sbt(st, "wk", [128, 8, D], BF16)
                wv = sbt(st, "wv", [128, 8, D], BF16)
                wo = sbt(st, "wo", [128, 8, D], BF16)
                w1 = sbt(st, "w1", [128, 8, 64], BF16)
                a1 = sbt(st, "a1", [128, 8, 64], BF16)
                g1 = sbt(st, "g1", [128, 8, 160], BF16)
                w2 = sbt(st, "w2", [128, 1, D], BF16)
                a2 = sbt(st, "a2", [128, 1, D], BF16)
                g2 = sbt(st, "g2", [128, 2, D], BF16)
                for tl, srcw in ((wr, wrkv_d[i, 0]), (wk, wrkv_d[i, 1]), (wv, wrkv_d[i, 2]), (wo, wo_d[i])):
                    load_wk(tl, srcw, 8, D)
                load_wk(w1, w1_d[i], 8, 64)
                load_wk(a1, a1_d[i], 8, 64)
                load_wk(g1, g1_d[i], 8, 160)
                load_wk(w2, w2_d[i], 1, D, rows_last=64)
                load_wk(a2, a2_d[i], 1, D, rows_last=64)
                load_wk(g2, g2_d[i], 2, D, rows_last=32)

                xts = [sbt(st, "xt%d" % k, [128, 8, TT], F32) for k in range(2)]
                hb = sbt(st, "hb", [128, 8, TT + 1], F32)
                xsn = [sbt(st, "xs%d" % n, [128, 8, TT], BF16) for n in range(6)]
                rstd = sbt(st, "rstd", [128, TT], F32)
                tw = sbt(st, "tw", [64, TT], BF16)
                ta = sbt(st, "ta", [64, TT], BF16)
                tg = sbt(st, "tg", [128, 2, TT], BF16)
                z = sbt(st, "z", [128, 8, TT], BF16)
                sq = sbt(st, "sq", [128, 8, TT], BF16)
                msb = sbt(st, "msb", [128, 8, TT], F32)
                xx = msb
                S = sbt(st, "S", [128, 8, 64], F32)
                Sb = sbt(st, "Sb", [128, 8, 64], BF16)
                names = "r k v kk a lw rn kmod b cum cex ecum d".split()
                sets = []
                for k in range(2):
                    J = dict(
                        cb={nm: sbt(st, "c%d_%s" % (k, nm), [128, TT], F32) for nm in names},
                        tmpS=sbt(st, "tmpS%d" % k, [128, 64], F32),
                        vb=sbt(st, "vb%d" % k, [128, TT], BF16),
                        sqb=sbt(st, "sqb%d" % k, [128, TT], BF16),
                        AR=sbt(st, "AR%d" % k, [128, 2 * TT], BF16),
                        KB=sbt(st, "KB%d" % k, [128, 2 * TT], BF16),
                        TM=sbt(st, "TM%d" % k, [128, 2, 3, 128], BF16),
                        bA=PS[4 * k], bB=PS[4 * k + 1], hd=[])
                    for hh in range(2):
                        J["hd"].append(dict(
                            AMs=sbt(st, "AMs%d_%d" % (k, hh), [128, 512], BF16),
                            Ls=sbt(st, "Ls%d_%d" % (k, hh), [128, 128], BF16),
                            LM=[sbt(st, "LM%d_%d_%d" % (k, hh, q), [128, 256], BF16) for q in range(2)],
                            Tt=[sbt(st, "Tt%d_%d_%d" % (k, hh, q), [128, 128], BF16) for q in range(2)],
                            Xs=sbt(st, "Xs%d_%d" % (k, hh), [128, 64], BF16),
                            Us=sbt(st, "Us%d_%d" % (k, hh), [128, 64], BF16),
                            WK=PS[4 * k + 2 + hh]))
                    J["cb"]["g"] = sbt(st, "c%d_g" % k, [128, TT], BF16)
                    J["cb"]["bonus"] = sbt(st, "c%d_bonus" % k, [128, TT], BF16)
                    for hh in range(2):
                        J["hd"][hh]["tmpS"] = J["tmpS"]
                    sets.append(J)
                SR = [Res("S%d" % c) for c in range(8)]
                SbR = [Res("Sb%d" % c) for c in range(8)]
                P.op("pool", lambda e: e.memset(S.t[:], 0.0), [], SR)
                P.op("pool", lambda e: e.memset(Sb.t[:], 0.0), [], SbR)
                P.op("pool", lambda e: e.memset(hb.t[:, :, 0:1], 0.0), [], [hb.r])

                def job(c, J):
                    B = J["cb"]
                    vb, AR, KB, TM, bA, bB, hd = J["vb"], J["AR"], J["KB"], J["TM"], J["bA"], J["bB"], J["hd"]
                    sqb = J["sqb"]
                    cs = slice(c * 128, (c + 1) * 128)
                    for kc in range(8):
                        mm(bA.t[:, 0:TT], wr.t[:, kc, cs], xsn[0].t[:, kc, :], kc == 0, kc == 7, [wr.r, xsn[0].r], [bA.r])
                    for kc in range(8):
                        mm(bA.t[:, TT:2 * TT], wk.t[:, kc, cs], xsn[1].t[:, kc, :], kc == 0, kc == 7, [wk.r, xsn[1].r], [bA.r])
                    for kc in range(8):
                        mm(bB.t[:, 0:TT], wv.t[:, kc, cs], xsn[2].t[:, kc, :], kc == 0, kc == 7, [wv.r, xsn[2].r], [bB.r])
                    mm(bB.t[:, TT:2 * TT], w2.t[0:64, 0, cs], tw.t[:], True, True, [w2.r, tw.r], [bB.r])
                    yield
                    cp("act", B["r"].t[:], bA.t[:, 0:TT], [], [bA.r, B["r"].r])
                    ts("dve", B["kk"].t[:], bA.t[:, TT:2 * TT], vD("k_k%d" % i, c), None, ALU.mult, None, [vt.r], [bA.r, B["kk"].r])
                    cp("act", B["k"].t[:], bA.t[:, TT:2 * TT], [], [bA.r, B["k"].r])
                    yield
                    cp("dve", B["v"].t[:], bB.t[:, 0:TT], [], [bB.r, B["v"].r])
                    cp("act", vb.t[:], bB.t[:, 0:TT], [], [bB.r, vb.r])
                    act(B["lw"].t[:], bB.t[:, TT:2 * TT], AF.Sigmoid, [vt.r], [bB.r, B["lw"].r], bias=vD("w0%d" % i, c))
                    ts("pool", B["lw"].t[:], B["lw"].t[:], -math.exp(-0.5), None, ALU.mult, None, [], [B["lw"].r])
                    yield
                    mm(bA.t[:, 0:TT], a2.t[0:64, 0, cs], ta.t[:], True, True, [a2.r, ta.r], [bA.r])
                    mm(bA.t[:, TT:2 * TT], g2.t[:, 0, cs], tg.t[:, 0, :], True, False, [g2.r, tg.r], [bA.r])
                    mm(bA.t[:, TT:2 * TT], g2.t[0:32, 1, cs], tg.t[0:32, 1, :], False, True, [g2.r, tg.r], [bA.r])
                    act(B["a"].t[:], bA.t[:, 0:TT], AF.Sigmoid, [vt.r], [bA.r, B["a"].r], bias=vD("a0%d" % i, c))
                    cp("act", B["g"].t[:], bA.t[:, TT:2 * TT], [], [bA.r, B["g"].r])
                    yield
                    act(sqb.t[:], B["kk"].t[:], AF.Square, [B["kk"].r], [sqb.r])
                    mm(bB.t[:, 0:TT], bdb, sqb.t[:], True, True, [bdb_t.r, sqb.r], [bB.r])
                    ts("dve", B["rn"].t[:], bB.t[:, 0:TT], 1e-24, None, ALU.max, None, [], [bB.r, B["rn"].r])
                    act(B["rn"].t[:], B["rn"].t[:], AF.Ln, [], [B["rn"].r])
                    act(B["rn"].t[:], B["rn"].t[:], AF.Exp, [], [B["rn"].r], scale=-0.5)
                    tt("dve", B["kk"].t[:], B["kk"].t[:], B["rn"].t[:], ALU.mult, [B["rn"].r], [B["kk"].r])
                    yield
                    ts("pool", B["kmod"].t[:], B["a"].t[:], -1.0, vD("k_a%d" % i, c), ALU.add, ALU.mult, [B["a"].r, vt.r], [B["kmod"].r])
                    stt("dve", B["kmod"].t[:], B["kmod"].t[:], 1.0, B["k"].t[:], ALU.add, ALU.mult, [B["k"].r], [B["kmod"].r])
                    tt("pool", B["b"].t[:], B["kk"].t[:], B["a"].t[:], ALU.mult, [B["kk"].r, B["a"].r], [B["b"].r])
                    for ch in range(2):
                        sl = slice(ch * 128, (ch + 1) * 128)
                        P.op("dve", lambda e, sl=sl: e.tensor_tensor_scan(
                            B["cum"].t[:, sl], ones, B["lw"].t[:, sl], 0.0, ALU.mult, ALU.add), [cst.r, B["lw"].r], [B["cum"].r])
                    tt("pool", B["cex"].t[:], B["cum"].t[:], B["lw"].t[:], ALU.subtract, [B["cum"].r, B["lw"].r], [B["cex"].r])
                    act(B["ecum"].t[:], B["cum"].t[:], AF.Exp, [B["cum"].r], [B["ecum"].r])
                    act(B["cum"].t[:], B["cum"].t[:], AF.Exp, [], [B["cum"].r], scale=-1.0)
                    act(B["cex"].t[:], B["cex"].t[:], AF.Exp, [], [B["cex"].r])
                    yield
                    for ch in range(2):
                        sl = slice(ch * 128, (ch + 1) * 128)
                        o = ch * 256
                        stt("dve", AR.t[:, o:o + 128], B["kk"].t[:, sl], -1.0, B["cex"].t[:, sl], ALU.mult, ALU.mult,
                            [B["kk"].r, B["cex"].r], [AR.r])
                        tt("pool", AR.t[:, o + 128:o + 256], B["r"].t[:, sl], B["ecum"].t[:, sl], ALU.mult, [B["r"].r, B["ecum"].r], [AR.r])
                        tt("dve", KB.t[:, o:o + 128], B["kmod"].t[:, sl], B["cum"].t[:, sl], ALU.mult, [B["kmod"].r, B["cum"].r], [KB.r])
                        tt("pool", KB.t[:, o + 128:o + 256], B["b"].t[:, sl], B["cum"].t[:, sl], ALU.mult, [B["b"].r, B["cum"].r], [KB.r])
                    yield
                    stt("dve", sqb.t[:], B["r"].t[:], vD("r_k%d" % i, c), B["kmod"].t[:], ALU.mult, ALU.mult,
                        [B["r"].r, B["kmod"].r, vt.r], [sqb.r])
                    mm(bB.t[:, TT:2 * TT], bdb, sqb.t[:], True, True, [bdb_t.r, sqb.r], [bB.r])
                    tt("dve", B["bonus"].t[:], bB.t[:, TT:2 * TT], B["v"].t[:], ALU.mult, [B["v"].r], [bB.r, B["bonus"].r])
                    for ch in range(2):
                        sl = slice(ch * 128, (ch + 1) * 128)
                        o = ch * 256
                        mm(bA.t[:, ch * 128:ch * 128 + 128], vb.t[:, sl], identb, True, True, [vb.r, identb_t.r], [bA.r])
                        mm(bA.t[:, 256 + ch * 128:256 + ch * 128 + 128], KB.t[:, o:o + 128], identb, True, True, [KB.r, identb_t.r], [bA.r])
                        mm(bB.t[:, ch * 128:ch * 128 + 128], KB.t[:, o + 128:o + 256], identb, True, True, [KB.r, identb_t.r], [bB.r])
                    yield
                    for ch in range(2):
                        cp("act", TM.t[:, ch, 0, :], bA.t[:, ch * 128:ch * 128 + 128], [], [bA.r, TM.r])
                        cp("dve", TM.t[:, ch, 1, :], bA.t[:, 256 + ch * 128:256 + ch * 128 + 128], [], [bA.r, TM.r])
                        cp("act", TM.t[:, ch, 2, :], bB.t[:, ch * 128:ch * 128 + 128], [], [bB.r, TM.r])
                    yield
                    for ch in range(2):
                        o = ch * 256
                        for hh in range(2):
                            p = slice(hh * 64, hh * 64 + 64)
                            Hh = hd[hh]
                            WK = Hh["WK"]
                            mm(WK.t[:, 0:256], KB.t[p, o:o + 128], AR.t[p, o:o + 256], True, True, [KB.r, AR.r], [WK.r])
                            mm(WK.t[:, 256:512], KB.t[p, o + 128:o + 256], AR.t[p, o:o + 256], True, True, [KB.r, AR.r], [WK.r])
                            TB = bA if hh == 0 else bB
                            mm(TB.t[:, 128:256], AR.t[p, o:o + 128], KB.t[p, o + 128:o + 256], True, True, [KB.r, AR.r], [TB.r])
                            yield
                        for hh in range(2):
                            Hh = hd[hh]
                            WK = Hh["WK"]
                            TB = bA if hh == 0 else bB
                            tt("dve", Hh["AMs"].t[:], WK.t[:], mask4, ALU.mult, [cst.r], [WK.r, Hh["AMs"].r])
                            tt("dve", Hh["Ls"].t[:], TB.t[:, 128:256], lowm, ALU.mult, [cst.r], [TB.r, Hh["Ls"].r])
                            tt("pool", Hh["Tt"][0].t[:], Hh["AMs"].t[:, 256:384], identb, ALU.add, [Hh["AMs"].r, identb_t.r], [Hh["Tt"][0].r])
                            yield
                        for lv in range(1, 7):
                            for hh in range(2):
                                Hh = hd[hh]
                                WK = Hh["WK"]
                                if lv == 1:
                                    Lp, Mp, rd = Hh["Ls"].t[:], Hh["AMs"].t[:, 256:384], [Hh["Ls"].r, Hh["AMs"].r]
                                else:
                                    pl = Hh["LM"][(lv - 1) % 2]
                                    Lp, Mp, rd = pl.t[:, 0:128], pl.t[:, 128:256], [pl.r]
                                nl = Hh["LM"][lv % 2]
                                mm(WK.t[:, 128:256], Mp, Lp, True, True, rd, [WK.r])
                                if lv < 6:
                                    mm(WK.t[:, 256:384], Lp, Mp, True, True, rd, [WK.r])
                                    cp("act", nl.t[:, 0:256], WK.t[:, 128:384], [], [WK.r, nl.r])
                                else:
                                    cp("act", nl.t[:, 0:128], WK.t[:, 128:256], [], [WK.r, nl.r])
                                yield
                            for hh in range(2):
                                Hh = hd[hh]
                                WK = Hh["WK"]
                                nl = Hh["LM"][lv % 2]
                                Tc = Hh["Tt"][(lv - 1) % 2]
                                Tn = Hh["Tt"][lv % 2]
                                TB = bA if hh == 0 else bB
                                mm(TB.t[:, 0:128], nl.t[:, 0:128], Tc.t[:], True, True, [nl.r, Tc.r], [TB.r])
                                tt("dve", Tn.t[:], TB.t[:, 0:128], Tc.t[:], ALU.add, [Tc.r], [TB.r, Tn.r])
                                yield
                        for hh in range(2):
                            p = slice(hh * 64, hh * 64 + 64)
                            fs = slice(hh * 64, hh * 64 + 64)
                            Hh = hd[hh]
                            WK = Hh["WK"]
                            mm(WK.t[:, 0:64], AR.t[p, o:o + 128], Sb.t[p, c, :], True, False, [AR.r, SbR[c]], [WK.r])
                            mm(WK.t[:, 0:64], Hh["AMs"].t[:, 0:128], TM.t[:, ch, 0, fs], False, True, [Hh["AMs"].r, TM.r], [WK.r])
                            cp("act", Hh["Xs"].t[:], WK.t[:, 0:64], [], [WK.r, Hh["Xs"].r])
                            yield
                        for hh in range(2):
                            Hh = hd[hh]
                            WK = Hh["WK"]
                            Tf = Hh["Tt"][0]
                            mm(WK.t[:, 64:128], Tf.t[:], Hh["Xs"].t[:], True, True, [Tf.r, Hh["Xs"].r], [WK.r])
                            cp("act", Hh["Us"].t[:], WK.t[:, 64:128], [], [WK.r, Hh["Us"].r])
                            yield
                        for hh in range(2):
                            p = slice(hh * 64, hh * 64 + 64)
                            fs = slice(hh * 64, hh * 64 + 64)
                            Hh = hd[hh]
                            WK = Hh["WK"]
                            yo = WK.t[p, 128:256]
                            mm(yo, Sb.t[p, c, :], AR.t[p, o + 128:o + 256], True, False, [SbR[c], AR.r], [WK.r])
                            mm(yo, Hh["Us"].t[:], Hh["AMs"].t[:, 384:512], False, False, [Hh["Us"].r, Hh["AMs"].r], [WK.r])
                            mm(yo, TM.t[:, ch, 0, fs], Hh["AMs"].t[:, 128:256], False, True, [TM.r, Hh["AMs"].r], [WK.r])
                            so = WK.t[p, 256:320]
                            mm(so, TM.t[:, ch, 2, fs], Hh["Us"].t[:], True, False, [TM.r, Hh["Us"].r], [WK.r])
                            mm(so, TM.t[:, ch, 1, fs], TM.t[:, ch, 0, fs], False, True, [TM.r], [WK.r])
                            yield
                        for hh in range(2):
                            p = slice(hh * 64, hh * 64 + 64)
                            Hh = hd[hh]
                            WK = Hh["WK"]
                            yo = WK.t[p, 128:256]
                            so = WK.t[p, 256:320]
                            cp("act", B["d"].t[p, ch * 128:ch * 128 + 128], yo, [], [WK.r, B["d"].r])
                            tt("dve", Hh["tmpS"].t[p, :], so, S.t[p, c, :], ALU.add, [SR[c]], [WK.r, Hh["tmpS"].r])
                            wc = B["ecum"].t[p, ch * 128 + 127:ch * 128 + 128]
                            ts("dve", S.t[p, c, :], Hh["tmpS"].t[p, :], wc, None, ALU.mult, None, [Hh["tmpS"].r, B["ecum"].r], [SR[c]])
                            ts("pool", Sb.t[p, c, :], Hh["tmpS"].t[p, :], wc, None, ALU.mult, None, [Hh["tmpS"].r, B["ecum"].r], [SbR[c]])
                            yield
                    cp("pool", sqb.t[:], B["d"].t[:], [B["d"].r], [sqb.r])
                    mm(bB.t[:, 0:TT], bdb, sqb.t[:], True, True, [bdb_t.r, sqb.r], [bB.r])
                    stt("dve", B["d"].t[:], bB.t[:, 0:TT], -1.0 / 64, B["d"].t[:], ALU.mult, ALU.add, [], [bB.r, B["d"].r])
                    act(sqb.t[:], B["d"].t[:], AF.Square, [B["d"].r], [sqb.r])
                    mm(bB.t[:, TT:2 * TT], bdb, sqb.t[:], True, True, [bdb_t.r, sqb.r], [bB.r])
                    act(B["rn"].t[:], bB.t[:, TT:2 * TT], AF.Ln, [], [bB.r, B["rn"].r], bias=LNX_EPS, scale=1.0 / 64)
                    act(B["rn"].t[:], B["rn"].t[:], AF.Exp, [], [B["rn"].r], scale=-0.5)
                    yield
                    stt("dve", B["d"].t[:], B["d"].t[:], vD("lnx_w%d" % i, c), B["rn"].t[:], ALU.mult, ALU.mult, [B["rn"].r, vt.r], [B["d"].r])
                    stt("dve", B["d"].t[:], B["d"].t[:], vD("lnx_b%d" % i, c), B["bonus"].t[:], ALU.add, ALU.add, [B["bonus"].r, vt.r], [B["d"].r])
                    tt("pool", z.t[:, c, :], B["d"].t[:], B["g"].t[:], ALU.mult, [B["d"].r, B["g"].r], [z.r])
                    yield

                def pre1(it):
                    t0 = it * TT
                    xt = xts[it % 2]
                    P.dma(xt.t[:], xtile(src, t0, TT), reads=rx(t0, TT), writes=[xt.r])
                    rms_stats(sq, xt.t[:], 8, TT, D, NORM_EPS, rstd, [xt.r], PS[3])
                    for c in range(8):
                        stt("dve", hb.t[:, c, 1:TT + 1], xt.t[:, c, :], vD("ng%d_0" % layer, c), rstd.t[:, :],
                            ALU.mult, ALU.mult, [xt.r, rstd.r, vt.r], [hb.r])
                    tt("pool", xx.t[:], hb.t[:, :, 0:TT], hb.t[:, :, 1:TT + 1], ALU.subtract, [hb.r], [xx.r])
                    for n in range(6):
                        for c in range(8):
                            stt("dve", xsn[n].t[:, c, :], xx.t[:, c, :], vD("mu%d_%d" % (i, n), c),
                                hb.t[:, c, 1:TT + 1], ALU.mult, ALU.add, [xx.r, hb.r, vt.r], [xsn[n].r])
                    cp("pool", hb.t[:, :, 0:1], hb.t[:, :, TT:TT + 1], [], [hb.r])

                def pre2(it):
                    for kc in range(8):
                        mm(PS[0].t[0:64, 0:TT], w1.t[:, kc, :], xsn[3].t[:, kc, :], kc == 0, kc == 7, [w1.r, xsn[3].r], [PS[0].r])
                    act(tw.t[:], PS[0].t[0:64, 0:TT], AF.Tanh, [], [PS[0].r, tw.r])
                    for kc in range(8):
                        mm(PS[1].t[0:64, 0:TT], a1.t[:, kc, :], xsn[4].t[:, kc, :], kc == 0, kc == 7, [a1.r, xsn[4].r], [PS[1].r])
                    cp("act", ta.t[:], PS[1].t[0:64, 0:TT], [], [PS[1].r, ta.r])
                    for kc in range(8):
                        mm(PS[2].t[:, 0:TT], g1.t[:, kc, 0:128], xsn[5].t[:, kc, :], kc == 0, kc == 7, [g1.r, xsn[5].r], [PS[2].r])
                    for kc in range(8):
                        mm(PS[2].t[0:32, TT:2 * TT], g1.t[:, kc, 128:160], xsn[5].t[:, kc, :], kc == 0, kc == 7, [g1.r, xsn[5].r], [PS[2].r])
                    act(tg.t[:, 0, :], PS[2].t[:, 0:TT], AF.Sigmoid, [], [PS[2].r, tg.r])
                    act(tg.t[0:32, 1, :], PS[2].t[0:32, TT:2 * TT], AF.Sigmoid, [], [PS[2].r, tg.r])

                def post(it):
                    t0 = it * TT
                    xt = xts[it % 2]
                    for m in range(8):
                        bk = PS[4 + m % 4]
                        for kc in range(8):
                            mm(bk.t[:, 0:TT], wo.t[:, kc, m * 128:(m + 1) * 128], z.t[:, kc, :], kc == 0, kc == 7, [wo.r, z.r], [bk.r])
                        cp("act", msb.t[:, m, :], bk.t[:, 0:TT], [], [bk.r, msb.r])
                    rms_stats(sq, msb.t[:], 8, TT, D, NORM_EPS, rstd, [msb.r], PS[3])
                    for c in range(8):
                        stt("dve", msb.t[:, c, :], msb.t[:, c, :], vD("ng%d_1" % layer, c), rstd.t[:, :], ALU.mult, ALU.mult,
                            [rstd.r, vt.r], [msb.r])
                    tt("pool", msb.t[:], msb.t[:], xt.t[:], ALU.add, [xt.r], [msb.r])
                    P.dma(xtile(dst, t0, TT), msb.t[:], reads=[msb.r], writes=rx(t0, TT))

                NT = T // TT
                pre1(0)
                pre2(0)
                for it in range(NT):
                    run_jobs((job(c, sets[c % 2]) for c in range(8)), 2, 38)
                    if it + 1 < NT:
                        pre1(it + 1)
                    post(it)
                    if it + 1 < NT:
                        pre2(it + 1)
                P.barrier()

        def ffn_layer(layer, src, dst):
            TT = 512
            with ExitStack() as st:
                win = sbt(st, "win", [128, 8, 2 * FF], BF16)
                stg.extend([sbt(st, "stgx%d" % k, [128, 1024], F32) for k in range(4)])
                load_wk(win, w_in_d[layer], 8, 2 * FF)
                del stg[2:]
                xts = [sbt(st, "xt%d" % k, [128, 8, TT], F32) for k in range(2)]
                xns = [sbt(st, "xn%d" % k, [128, 8, TT], BF16) for k in range(2)]
                sq = sbt(st, "sq", [128, 8, TT], BF16)
                rstd = sbt(st, "rstd", [128, TT], F32)
                carry = sbt(st, "carry", [128, NF, 2], F32)
                gbuf = [sbt(st, "gbuf%d" % k, [128, TT + 2], F32) for k in range(2)]
                acc = [sbt(st, "acc%d" % k, [128, TT], F32) for k in range(2)]
                gl = [sbt(st, "gl%d" % k, [128, TT], F32) for k in range(2)]
                hT = [sbt(st, "hT%d" % k, [128, TT], BF16) for k in range(2)]
                P.op("pool", lambda e: e.memset(carry.t[:], 0.0), [], [carry.r])

                def norm_in(it):
                    t0 = it * TT
                    xt, xn = xts[it % 2], xns[it % 2]
                    P.dma(xt.t[:], xtile(src, t0, TT), reads=rx(t0, TT), writes=[xt.r])
                    rms_stats(sq, xt.t[:], 8, TT, D, NORM_EPS, rstd, [xt.r], PS[7])
                    for c in range(8):
                        stt("dve", xn.t[:, c, :], xt.t[:, c, :], vD("ng%d_2" % layer, c), rstd.t[:, :], ALU.mult, ALU.mult,
                            [xt.r, rstd.r, vt.r], [xn.r])

                norm_in(0)
                for it in range(T // TT):
                    t0 = it * TT
                    xn = xns[it % 2]
                    for f in range(NF):
                        if f == 12 and it + 1 < T // TT:
                            norm_in(it + 1)
                        k2 = f % 2
                        gb, ub = PS[k2], PS[2 + k2]
                        for kc in range(8):
                            mm(gb.t[:, :], win.t[:, kc, f * 128:(f + 1) * 128], xn.t[:, kc, :], kc == 0, kc == 7,
                               [win.r, xn.r], [gb.r])
                        for kc in range(8):
                            mm(ub.t[:, :], win.t[:, kc, FF + f * 128:FF + (f + 1) * 128], xn.t[:, kc, :], kc == 0, kc == 7,
                               [win.r, xn.r], [ub.r])
                        G = gbuf[k2]
                        cp("pool", G.t[:, 0:2], carry.t[:, f, :], [carry.r], [G.r])
                        cp("act", G.t[:, 2:TT + 2], gb.t[:, :], [], [gb.r, G.r])
                        cp("pool", carry.t[:, f, :], G.t[:, TT:TT + 2], [G.r], [carry.r])
                        A = acc[k2]
                        ts("dve", A.t[:], G.t[:, 2:TT + 2], vF(layer, 2, f), vF(layer, 3, f), ALU.mult, ALU.add,
                           [G.r, vt.r], [A.r])
                        stt("dve", A.t[:], G.t[:, 1:TT + 1], vF(layer, 1, f), A.t[:], ALU.mult, ALU.add, [G.r, vt.r], [A.r])
                        stt("dve", A.t[:], G.t[:, 0:TT], vF(layer, 0, f), A.t[:], ALU.mult, ALU.add, [G.r, vt.r], [A.r])
                        act(gl[k2].t[:], A.t[:], AF.Gelu_apprx_tanh, [A.r], [gl[k2].r])
                        tt("dve", hT[k2].t[:], ub.t[:, :], gl[k2].t[:], ALU.mult, [gl[k2].r], [ub.r, hT[k2].r])
                        P.dma(hscr[it, :, f, :], hT[k2].t[:], reads=[hT[k2].r], writes=[Rh[it]])
                P.barrier()
            with ExitStack() as st:
                wout = sbt(st, "wout", [128, NF, D], BF16)
                stg.extend([sbt(st, "stgy%d" % k, [128, 1024], F32) for k in range(4)])
                load_wk(wout, w_out_d[layer], NF, D)
                del stg[2:]
                xt = sbt(st, "xt", [128, 8, TT], F32)
                ht = [sbt(st, "ht%d" % k, [128, NF, TT], BF16) for k in range(2)]
                msb = sbt(st, "msb", [128, 8, TT], F32)
                sq = sbt(st, "sq", [128, 8, TT], BF16)
                rstd = sbt(st, "rstd", [128, TT], F32)
                for it in range(T // TT):
                    t0 = it * TT
                    hh = ht[it % 2]
                    P.dma(hh.t[:], hscr[it], reads=[Rh[it]], writes=[hh.r])
                    P.dma(xt.t[:], xtile(src, t0, TT), reads=rx(t0, TT), writes=[xt.r])
                    for m in range(8):
                        bk = PS[m % 4]
                        for f in range(NF):
                            mm(bk.t[:, :], wout.t[:, f, m * 128:(m + 1) * 128], hh.t[:, f, :], f == 0, f == NF - 1,
                               [wout.r, hh.r], [bk.r])
                        cp("act", msb.t[:, m, :], bk.t[:, :], [], [bk.r, msb.r])
                    rms_stats(sq, msb.t[:], 8, TT, D, NORM_EPS, rstd, [msb.r], PS[7])
                    for c in range(8):
                        stt("dve", msb.t[:, c, :], msb.t[:, c, :], vD("ng%d_3" % layer, c), rstd.t[:, :], ALU.mult, ALU.mult,
                            [rstd.r, vt.r], [msb.r])
                    tt("pool", msb.t[:], msb.t[:], xt.t[:], ALU.add, [xt.r], [msb.r])
                    P.dma(xtile(dst, t0, TT), msb.t[:], reads=[msb.r], writes=rx(t0, TT))
                P.barrier()

        def kv_prep(src):
            TT = 512
            with ExitStack() as st:
                kd = sbt(st, "kd", [128, 8, 288], BF16)
                kds = sbt(st, "kds", [128, 8, 32], BF16)
                kuk = sbt(st, "kuk", [128, 2, H * 64], BF16)
                kuv = sbt(st, "kuv", [128, 2, H * 64], BF16)
                load_wk(kd, kd_d, 8, 288)
                load_wk(kds, kds_d, 8, 32)
                load_wk(kuk, kuk_d, 2, H * 64)
                load_wk(kuv, kuv_d, 2, H * 64)
                xt = sbt(st, "xt", [128, 8, TT], F32)
                xn = sbt(st, "xn", [128, 8, TT], BF16)
                sq = sbt(st, "sq", [128, 8, TT], BF16)
                rstd = sbt(st, "rstd", [128, TT], F32)
                ckv = sbt(st, "ckv", [128, 2, TT], F32)
                ckvn = sbt(st, "ckvn", [128, 2, TT], BF16)
                rp = sbt(st, "rp", [128, 2, TT], F32)
                t1 = sbt(st, "t1", [128, TT], F32)
                t2 = sbt(st, "t2", [128, TT], F32)
                kr = sbt(st, "kr", [128, TT], BF16)
                KT = [sbt(st, "KT%d" % k, [128, TT], BF16) for k in range(2)]
                Vt = [sbt(st, "Vt%d" % k, [128, H, 128], BF16) for k in range(2)]
                for k in range(2):
                    P.op("pool", lambda e, k=k: e.memset(Vt[k].t[:, :, 64:128], 1.0), [], [Vt[k].r])
                R = slice(64, 96)
                for it in range(T // TT):
                    t0 = it * TT
                    P.dma(xt.t[:], xtile(src, t0, TT), reads=rx(t0, TT), writes=[xt.r])
                    P.dma(rp.t[R, :, :], rope_d[R, :, t0:t0 + TT], reads=[Rin], writes=[rp.r])
                    rms_stats(sq, xt.t[:], 8, TT, D, NORM_EPS, rstd, [xt.r], PS[7])
                    for c in range(8):
                        stt(ew(), xn.t[:, c, :], xt.t[:, c, :], vD("kvng", c), rstd.t[:, :], ALU.mult, ALU.mult,
                            [xt.r, rstd.r, vt.r], [xn.r])
                    for j in range(2):
                        for kc in range(8):
                            mm(PS[j].t[:, :], kd.t[:, kc, j * 128:(j + 1) * 128], xn.t[:, kc, :], kc == 0, kc == 7,
                               [kd.r, xn.r], [PS[j].r])
                        cp("act", ckv.t[:, j, :], PS[j].t[:, :], [], [PS[j].r, ckv.r])
                    for kc in range(8):
                        mm(PS[2].t[R, :], kd.t[:, kc, 256:288], xn.t[:, kc, :], kc == 0, kc == 7, [kd.r, xn.r], [PS[2].r])
                    for kc in range(8):
                        mm(PS[3].t[R, :], kds.t[:, kc, :], xn.t[:, kc, :], kc == 0, kc == 7, [kds.r, xn.r], [PS[3].r])
                    tt("dve", t1.t[R, :], PS[2].t[R, :], rp.t[R, 0, :], ALU.mult, [rp.r], [PS[2].r, t1.r])
                    tt("dve", t2.t[R, :], PS[3].t[R, :], rp.t[R, 1, :], ALU.mult, [rp.r], [PS[3].r, t2.r])
                    tt("dve", kr.t[R, :], t1.t[R, :], t2.t[R, :], ALU.add, [t1.r, t2.r], [kr.r])
                    rms_stats(sq, ckv.t[:], 2, TT, KVL, NORM_EPS, rstd, [ckv.r], PS[7])
                    for c in range(2):
                        stt("dve", ckvn.t[:, c, :], ckv.t[:, c, :], vK(c), rstd.t[:, :], ALU.mult, ALU.mult,
                            [ckv.r, rstd.r, vt.r], [ckvn.r])
                    for h in range(H):
                        K = KT[h % 2]
                        bk = PS[h % 2]
                        for kc in range(2):
                            mm(bk.t[0:64, :], kuk.t[:, kc, h * 64:(h + 1) * 64], ckvn.t[:, kc, :], kc == 0, kc == 1,
                               [kuk.r, ckvn.r], [bk.r])
                        cp("act", K.t[0:64, :], bk.t[0:64, :], [], [bk.r, K.r])
                        cp("pool", K.t[R, :], kr.t[R, :], [kr.r], [K.r])
                        P.dma(kscr[h, :, t0:t0 + TT], K.t[0:96, :], reads=[K.r], writes=[Rkv[it]])
                    for tb in range(TT // 128):
                        V = Vt[tb % 2]
                        for hf in range(2):
                            bk = PS[4 + hf]
                            for kc in range(2):
                                mm(bk.t[:, :], ckvn.t[:, kc, tb * 128:(tb + 1) * 128], kuv.t[:, kc, hf * 512:(hf + 1) * 512],
                                   kc == 0, kc == 1, [kuv.r, ckvn.r], [bk.r])
                            cp("act" if hf == 0 else "dve", V.t[:, hf * 8:(hf + 1) * 8, 0:64],
                               bk.t[:, :].rearrange("p (h d) -> p h d", d=64), [], [bk.r, V.r])
                        P.dma(vscr[:, t0 + tb * 128:t0 + (tb + 1) * 128, :].rearrange("h t d -> t h d"), V.t[:],
                              reads=[V.r], writes=[Rkv[it]])
                P.barrier()

        def mla_layer(j, layer, src, dst):
            TT = 512
            with ExitStack() as st:
                qd = sbt(st, "qd", [128, 8, QL], BF16)
                qu = sbt(st, "qu", [128, 3, H * 96], BF16)
                qus = sbt(st, "qus", [128, 3, H * 32], BF16)
                ow = sbt(st, "ow", [64, H, D], BF16)
                load_wk(qd, qd_d[j], 8, QL)
                load_wk(qu, qu_d[j], 3, H * 96)
                load_wk(qus, qus_d[j], 3, H * 32)
                for h in range(H):
                    load_w(lambda r, c0, cw, h=h: ow.t[0:r, h, c0:c0 + cw], ow.r, ow_d[j, h * 64:(h + 1) * 64, :], 64, D)
                xt = sbt(st, "xt", [128, 8, TT], F32)
                xn = sbt(st, "xn", [128, 8, TT], BF16)
                sq = sbt(st, "sq", [128, 8, TT], BF16)
                rstd = sbt(st, "rstd", [128, TT], F32)
                cq = sbt(st, "cq", [128, 3, TT], F32)
                cqn = sbt(st, "cqn", [128, 3, TT], BF16)
                rp = sbt(st, "rp", [128, 2, TT], F32)
                t1 = sbt(st, "t1", [128, TT], F32)
                t2 = sbt(st, "t2", [128, TT], F32)
                QT = sbt(st, "QT", [128, H, TT], BF16)
                OT = sbt(st, "OT", [64, H, TT], BF16)
                KT = [sbt(st, "KT%d" % k, [128, T], BF16) for k in range(2)]
                VT = [sbt(st, "VT%d" % k, [128, T // 128, 128], BF16) for k in range(2)]
                PT = [sbt(st, "PT%d" % k, [128, TT], BF16) for k in range(3)]
                rec = sbt(st, "rec", [64, TT], F32)
                msb = sbt(st, "msb", [128, 8, TT], F32)
                R = slice(64, 96)
                pi = 0
                for it in range(T // TT):
                    t0 = it * TT
                    P.dma(xt.t[:], xtile(src, t0, TT), reads=rx(t0, TT), writes=[xt.r])
                    P.dma(rp.t[R, :, :], rope_d[R, :, t0:t0 + TT], reads=[Rin], writes=[rp.r])
                    rms_stats(sq, xt.t[:], 8, TT, D, NORM_EPS, rstd, [xt.r], PS[2])
                    for c in range(8):
                        stt(ew(), xn.t[:, c, :], xt.t[:, c, :], vD("ng%d_0" % layer, c), rstd.t[:, :], ALU.mult, ALU.mult,
                            [xt.r, rstd.r, vt.r], [xn.r])
                    for m in range(3):
                        bk = PS[m % 2]
                        for kc in range(8):
                            mm(bk.t[:, :], qd.t[:, kc, m * 128:(m + 1) * 128], xn.t[:, kc, :], kc == 0, kc == 7,
                               [qd.r, xn.r], [bk.r])
                        cp("act", cq.t[:, m, :], bk.t[:, :], [], [bk.r, cq.r])
                    rms_stats(sq, cq.t[:], 3, TT, QL, NORM_EPS, rstd, [cq.r], PS[2])
                    for c in range(3):
                        stt("dve", cqn.t[:, c, :], cq.t[:, c, :], vQ(j, c), rstd.t[:, :], ALU.mult, ALU.mult,
                            [cq.r, rstd.r, vt.r], [cqn.r])
                    for h in range(H):
                        A = PS[h % 2]
                        Bk = PS[2]
                        for kc in range(3):
                            mm(A.t[0:96, :], qu.t[:, kc, h * 96:(h + 1) * 96], cqn.t[:, kc, :], kc == 0, kc == 2,
                               [qu.r, cqn.r], [A.r])
                        for kc in range(3):
                            mm(Bk.t[R, :], qus.t[:, kc, h * 32:(h + 1) * 32], cqn.t[:, kc, :], kc == 0, kc == 2,
                               [qus.r, cqn.r], [Bk.r])
                        act(QT.t[0:64, h, :], A.t[0:64, :], AF.Copy, [], [A.r, QT.r], scale=SCALE)
                        stt("dve", t1.t[R, :], A.t[R, :], SCALE, rp.t[R, 0, :], ALU.mult, ALU.mult, [rp.r], [A.r, t1.r])
                        stt("dve", t2.t[R, :], Bk.t[R, :], SCALE, rp.t[R, 1, :], ALU.mult, ALU.mult, [rp.r], [Bk.r, t2.r])
                        tt("dve", QT.t[R, h, :], t1.t[R, :], t2.t[R, :], ALU.add, [t1.r, t2.r], [QT.r])
                    nkb = (it + 1) * 4
                    nk = nkb * 128
                    for h in range(H):
                        K = KT[h % 2]
                        V = VT[h % 2]
                        P.dma(K.t[0:96, 0:nk], kscr[h, :, 0:nk], reads=Rkv[0:it + 1], writes=[K.r])
                        P.dma(V.t[:, 0:nkb, :], vscr[h, 0:nk, :].rearrange("(kb p) d -> p kb d", p=128),
                              reads=Rkv[0:it + 1], writes=[V.r])
                        Ob = PS[6 + h % 2]
                        pts = {}

                        def qk(kb, h=h, K=K):
                            nonlocal pi
                            jd = kb - it * 4
                            c0 = max(jd, 0) * 128
                            Sb_ = PS[3 + pi % 3]
                            Pt = PT[pi % 3]
                            pi += 1
                            mm(Sb_.t[:, c0:TT], K.t[0:96, kb * 128:(kb + 1) * 128], QT.t[0:96, h, c0:TT], True, True,
                               [K.r, QT.r], [Sb_.r])
                            act(Pt.t[:, c0:TT], Sb_.t[:, c0:TT], AF.Exp, [], [Sb_.r, Pt.r])
                            if jd >= 0:
                                P.op("pool", lambda e, Pt=Pt, c0=c0: e.memset(Pt.t[64:128, c0:c0 + 64], 0.0), [], [Pt.r])
                            pts[kb] = (Pt, c0)

                        def pv(kb, V=V, Ob=Ob):
                            Pt, c0 = pts.pop(kb)
                            mm(Ob.t[:, c0:TT], V.t[:, kb, :], Pt.t[:, c0:TT], kb == 0, kb == nkb - 1, [V.r, Pt.r], [Ob.r])

                        LA = 2
                        for kb in range(min(LA, nkb)):
                            qk(kb)
                        for kb in range(nkb):
                            if kb + LA < nkb:
                                qk(kb + LA)
                            pv(kb)
                        P.op("dve", lambda e, Ob=Ob: e.reciprocal(rec.t[:, :], Ob.t[64:128, :]), [], [Ob.r, rec.r])
                        tt("dve", OT.t[:, h, :], Ob.t[0:64, :], rec.t[:, :], ALU.mult, [rec.r], [Ob.r, OT.r])
                    for m in range(8):
                        bk = PS[m % 2]
                        for h in range(H):
                            mm(bk.t[:, :], ow.t[0:64, h, m * 128:(m + 1) * 128], OT.t[0:64, h, :], h == 0, h == H - 1,
                               [ow.r, OT.r], [bk.r])
                        cp("act", msb.t[:, m, :], bk.t[:, :], [], [bk.r, msb.r])
                    rms_stats(sq, msb.t[:], 8, TT, D, NORM_EPS, rstd, [msb.r], PS[2])
                    for c in range(8):
                        stt("dve", msb.t[:, c, :], msb.t[:, c, :], vD("ng%d_1" % layer, c), rstd.t[:, :], ALU.mult, ALU.mult,
                            [rstd.r, vt.r], [msb.r])
                    tt("pool", msb.t[:], msb.t[:], xt.t[:], ALU.add, [xt.r], [msb.r])
                    P.dma(xtile(dst, t0, TT), msb.t[:], reads=[msb.r], writes=rx(t0, TT))
                P.barrier()

        P.barrier()
        cur = xT
        for layer in range(depth):
            if layer < NA:
                rwkv_layer(layer, layer, cur, xs)
            else:
                if layer == NA:
                    kv_prep(xs)
                mla_layer(layer - NA, layer, xs, xs)
            cur = xs
            ffn_layer(layer, xs, y if layer == depth - 1 else xs)
        P.barrier()
        P.emit()
        nc._n_ops = P.n
    return nc


NVT = [0, 0, 0]
_CACHE = {}


def prep_inputs(inputs):
    inp = {k: np.asarray(v) for k, v in inputs.items()}
    B, T, _ = inp["x"].shape
    vt, nd, nf = _pack_tables(inp)
    NVT[0], NVT[1], NVT[2] = vt.shape[1], nd, nf
    f32 = lambda a: np.ascontiguousarray(a, dtype=np.float32)
    kd = inp["kv_w_down"]
    kds = np.concatenate([kd[:, 272:288], kd[:, 256:272]], 1)
    ku = inp["kv_w_up"].reshape(KVL, H, 128)
    qu = inp["q_w_up"].reshape(2, QL, H, 96)
    qus = np.concatenate([qu[..., 80:96], qu[..., 64:80]], -1).reshape(2, QL, H * 32)
    shared = {
        "vt": vt, "cst": _consts(), "rope": _rope(T),
        "ffn_w_in": f32(inp["ffn_w_in"]), "ffn_w_out": f32(inp["ffn_w_out"]),
        "a_w_rkv": f32(inp["a_w_rkv"]), "a_w1": f32(inp["a_w1"]), "a_w2": f32(inp["a_w2"]),
        "a_a1": f32(inp["a_a1"]), "a_a2": f32(inp["a_a2"]), "a_g1": f32(inp["a_g1"]), "a_g2": f32(inp["a_g2"]),
        "a_w_o": f32(inp["a_w_o"]), "kv_w_down": f32(kd), "kv_w_down_sw": f32(kds),
        "kv_w_up_k": f32(ku[:, :, 0:64].reshape(KVL, H * 64)), "kv_w_up_v": f32(ku[:, :, 64:128].reshape(KVL, H * 64)),
        "q_w_down": f32(inp["q_w_down"]), "q_w_up": f32(inp["q_w_up"]), "q_w_up_sw": f32(qus),
        "o_w": f32(inp["o_w"]),
    }
    maps = []
    for b in range(B):
        m = dict(shared)
        m["xT"] = f32(inp["x"][b].T)
        maps.append(m)
    return maps, B, T


def kernel(**inputs):
    maps, B, T = prep_inputs(inputs)
    key = (T, DEPTH)
    if key not in _CACHE:
        _CACHE[key] = build(T)
    nc = _CACHE[key]
    res = run_bass_kernel_spmd(nc, maps, core_ids=list(range(B)))
    out = np.stack([np.asarray(r["y"]).T for r in res.results], 0)
    return np.ascontiguousarray(out.astype(np.float32))
```
